# Optimizing a Trainium2 kernel written in Bass

```python
import numpy as np
import jax
import jax.numpy as jnp
from jax import lax

D_MODEL = 1024
BATCH = 16
SEQ = 4096
DEPTH = 2

PLE_DIM = 256
DN_ALPHA = (2 * DEPTH) ** 0.25
DN_BETA = (8 * DEPTH) ** -0.25
LN_EPS = 1e-5
ROPE_THETA = 10000.0

RW_WIDTH = D_MODEL // 2
RW_HEAD = 64
RW_HEADS = RW_WIDTH // RW_HEAD
RW_DECAY_LORA = 64
RW_AAA_LORA = 64
RW_GATE_LORA = 128
RW_GN_EPS = RW_HEAD * 1e-5
RW_SPLITS = (RW_WIDTH, RW_WIDTH, RW_WIDTH, RW_DECAY_LORA, RW_AAA_LORA, RW_GATE_LORA)
RW_COLS = sum(RW_SPLITS)

NSA_WIDTH = D_MODEL - RW_WIDTH
NSA_HEAD = 64
NSA_HEADS = NSA_WIDTH // NSA_HEAD
NSA_KV_HEADS = 2
NSA_GROUP = NSA_HEADS // NSA_KV_HEADS
NSA_KV = NSA_KV_HEADS * NSA_HEAD
CMP_LEN = 32
CMP_STRIDE = 16
CMP_HIDDEN = 128
SEL_LEN = 64
SEL_TOP = 16
WINDOW = 512
NSA_QBLOCK = 32
NSA_SPLITS = (NSA_WIDTH,) + (NSA_KV,) * 6 + (3 * NSA_HEADS,)
NSA_COLS = sum(NSA_SPLITS)
EV_COLS = RW_COLS + NSA_COLS

HG_HEAD = 128
HG_HEADS = D_MODEL // HG_HEAD
HG_CHUNK = 16
HG_SPLITS = (D_MODEL, D_MODEL, D_MODEL, D_MODEL)
HG_COLS = sum(HG_SPLITS)

MOE_GROUPS = 4
MOE_PER_GROUP = 8
MOE_EXPERTS = MOE_GROUPS * MOE_PER_GROUP
MOE_TOPK = 2
MOE_HIDDEN = 512
MOE_BLOCK = 256

N_EVEN = (DEPTH + 1) // 2
N_ODD = DEPTH // 2

kernel_name = 'hybrid_rwkv7_nsa_hgrn2_hmoe'


def _split(z, sizes):
    cuts = [int(c) for c in np.cumsum(sizes)[:-1]]
    return jnp.split(z, cuts, axis=-1)


def _layer_norm(x, g, b):
    xf = x.astype(jnp.float32)
    xc = xf - jnp.mean(xf, -1, keepdims=True)
    var = jnp.mean(xc * xc, -1, keepdims=True)
    return (xc * lax.rsqrt(var + LN_EPS) * g + b).astype(x.dtype)


def _rope_tables(positions, dim):
    inv = (1.0 / (ROPE_THETA ** (np.arange(0, dim, 2, dtype=np.float32) / dim))).astype(np.float32)
    ang = positions.astype(jnp.float32)[..., None] * inv
    return jnp.cos(ang)[:, :, None, :], jnp.sin(ang)[:, :, None, :]


def _apply_rope(x, cos, sin):
    half = x.shape[-1] // 2
    xf = x.astype(jnp.float32)
    x1, x2 = xf[..., :half], xf[..., half:]
    return jnp.concatenate([x1 * cos - x2 * sin, x2 * cos + x1 * sin], -1).astype(x.dtype)


def _masked_softmax(s, mask):
    s = jnp.where(mask, s.astype(jnp.float32), -jnp.inf)
    m = jnp.max(s, axis=-1, keepdims=True)
    m = jnp.where(jnp.isfinite(m), m, 0.0)
    e = jnp.exp(s - m)
    return e / jnp.maximum(jnp.sum(e, -1, keepdims=True), 1e-30)


def _rwkv7_scan(r, w, k, v, kk, a):
    B, T, H, N = r.shape

    def step(S, inp):
        r_t, w_t, k_t, v_t, kk_t, a_t = inp
        sa = jnp.einsum('bhvk,bhk->bhv', S, kk_t)
        S = S * w_t[:, :, None, :] - sa[..., None] * (kk_t * a_t)[:, :, None, :] + v_t[..., None] * k_t[:, :, None, :]
        return S, jnp.einsum('bhvk,bhk->bhv', S, r_t)

    xs = tuple(jnp.moveaxis(t, 1, 0) for t in (r, w, k, v, kk, a))
    _, o = lax.scan(step, jnp.zeros((B, H, N, N), jnp.float32), xs)
    return jnp.moveaxis(o, 0, 1)


def _rwkv7_group(z, mu, w0, w2, a0, a2, g2, k_k, k_a, r_k, gn_g, gn_b):
    B, T, _ = z.shape
    z = z.astype(jnp.float32)
    z_prev = jnp.pad(z, ((0, 0), (1, 0), (0, 0)))[:, :-1]
    z = z + mu * (z_prev - z)
    r, k, v, wd, ad, gd = _split(z, RW_SPLITS)
    w = -jax.nn.softplus(-(w0 + jnp.tanh(wd) @ w2)) - 0.5
    decay = jnp.exp(-jnp.exp(w))
    a = jax.nn.sigmoid(a0 + ad @ a2)
    g = jax.nn.sigmoid(gd) @ g2
    heads = lambda t: t.reshape(B, T, RW_HEADS, RW_HEAD)
    kk = heads(k * k_k)
    kk = kk / jnp.maximum(jnp.sqrt(jnp.sum(kk * kk, -1, keepdims=True)), 1e-12)
    k = k * (1.0 + (a - 1.0) * k_a)
    r, k, v, a = heads(r), heads(k), heads(v), heads(a)
    o = _rwkv7_scan(r, heads(decay), k, v, kk, a)
    oc = o - jnp.mean(o, -1, keepdims=True)
    o = oc * lax.rsqrt(jnp.mean(oc * oc, -1, keepdims=True) + RW_GN_EPS)
    o = o.reshape(B, T, RW_WIDTH) * gn_g + gn_b
    bonus = (jnp.sum(r * k * r_k, -1, keepdims=True) * v).reshape(B, T, RW_WIDTH)
    return (o + bonus) * g


def _cmp_sel_overlap(n_cmp, n_sel):
    cs = np.arange(n_cmp)[:, None] * CMP_STRIDE
    ss = np.arange(n_sel)[None, :] * SEL_LEN
    ov = np.clip(np.minimum(cs + CMP_LEN, ss + SEL_LEN) - np.maximum(cs, ss), 0, None)
    return (ov / CMP_LEN).astype(np.float32)


def _nsa_group(z, cos, sin, cmp_pos, cmp_w1, cmp_w2):
    B, T, _ = z.shape
    q, kc, vc, ksl, vsl, kwn, vwn, gl = _split(z, NSA_SPLITS)
    kv_heads = lambda t: t.reshape(B, T, NSA_KV_HEADS, NSA_HEAD)
    q = q.reshape(B, T, NSA_HEADS, NSA_HEAD)
    q_rot = _apply_rope(q, cos, sin).reshape(B, T, NSA_KV_HEADS, NSA_GROUP, NSA_HEAD)
    q_raw = q.reshape(B, T, NSA_KV_HEADS, NSA_GROUP, NSA_HEAD)
    ksl = _apply_rope(kv_heads(ksl), cos, sin)
    kwn = _apply_rope(kv_heads(kwn), cos, sin)
    vsl, vwn = kv_heads(vsl), kv_heads(vwn)
    gates = jax.nn.sigmoid(gl.astype(jnp.float32)).reshape(B, T, NSA_KV_HEADS, NSA_GROUP, 3)

    n_cmp = (T - CMP_LEN) // CMP_STRIDE + 1
    blk_idx = np.arange(n_cmp)[:, None] * CMP_STRIDE + np.arange(CMP_LEN)[None, :]
    blocks = jnp.stack([kv_heads(kc), kv_heads(vc)])[:, :, blk_idx]
    blocks = blocks + cmp_pos[:, None, None, :, None, :]
    blocks = jnp.moveaxis(blocks, 3, 4).reshape(2, B, n_cmp, NSA_KV_HEADS, CMP_LEN * NSA_HEAD)
    hid = jax.nn.gelu(jnp.einsum('zbnhi,zio->zbnho', blocks, cmp_w1))
    kv_cmp = jnp.einsum('zbnho,zod->zbnhd', hid, cmp_w2)
    k_cmp, v_cmp = kv_cmp[0], kv_cmp[1]
    cmp_last = np.arange(n_cmp) * CMP_STRIDE + CMP_LEN - 1

    n_sel = T // SEL_LEN
    n_top = min(SEL_TOP, n_sel)
    overlap = jnp.asarray(_cmp_sel_overlap(n_cmp, n_sel))
    sel_blocks = lambda t: jnp.moveaxis(t.reshape(B, n_sel, SEL_LEN, NSA_KV_HEADS, NSA_HEAD), 3, 1)
    k_blk, v_blk = sel_blocks(ksl), sel_blocks(vsl)
    gather = jax.vmap(jax.vmap(lambda blk, ix: blk[ix]))
    blk_ids = np.arange(n_sel)
    in_blk = np.arange(SEL_LEN)

    pad = ((0, 0), (WINDOW, 0), (0, 0), (0, 0))
    k_win, v_win = jnp.pad(kwn, pad), jnp.pad(vwn, pad)
    win_off = np.arange(WINDOW + NSA_QBLOCK) - WINDOW
    scale = NSA_HEAD ** -0.5

    def q_block(c):
        t0 = c * NSA_QBLOCK
        tq = t0 + jnp.arange(NSA_QBLOCK)
        sl = lambda t: lax.dynamic_slice_in_dim(t, t0, NSA_QBLOCK, axis=1)
        qc, qr, gc = sl(q_raw) * scale, sl(q_rot) * scale, sl(gates)
        s_c = jnp.einsum('bqhgd,bnhd->bhgqn', qc, k_cmp)
        p_c = _masked_softmax(s_c, cmp_last[None, :] <= tq[:, None])
        o_c = jnp.einsum('bhgqn,bnhd->bqhgd', p_c, v_cmp)
        imp = jnp.einsum('bhgqn,ns->bhqs', p_c, overlap)
        cur = (tq // SEL_LEN)[:, None]
        forced = (blk_ids[None, :] == 0) | (blk_ids[None, :] == cur) | (blk_ids[None, :] == cur - 1)
        imp = jnp.where(forced, jnp.inf, jnp.where(blk_ids[None, :] > cur, -jnp.inf, imp))
        _, sel = lax.top_k(imp, n_top)
        k_g = gather(k_blk, sel).reshape(B, NSA_KV_HEADS, NSA_QBLOCK, n_top * SEL_LEN, NSA_HEAD)
        v_g = gather(v_blk, sel).reshape(B, NSA_KV_HEADS, NSA_QBLOCK, n_top * SEL_LEN, NSA_HEAD)
        pos_s = (sel[..., None] * SEL_LEN + in_blk).reshape(B, NSA_KV_HEADS, NSA_QBLOCK, n_top * SEL_LEN)
        s_s = jnp.einsum('bqhgd,bhqkd->bhgqk', qr, k_g)
        p_s = _masked_softmax(s_s, (pos_s <= tq[:, None])[:, :, None])
        o_s = jnp.einsum('bhgqk,bhqkd->bqhgd', p_s, v_g)
        kw = lax.dynamic_slice_in_dim(k_win, t0, WINDOW + NSA_QBLOCK, axis=1)
        vw = lax.dynamic_slice_in_dim(v_win, t0, WINDOW + NSA_QBLOCK, axis=1)
        pos_w = t0 + win_off
        valid_w = (pos_w[None, :] >= 0) & (pos_w[None, :] <= tq[:, None]) & (pos_w[None, :] > tq[:, None] - WINDOW)
        s_w = jnp.einsum('bqhgd,bkhd->bhgqk', qr, kw)
        p_w = _masked_softmax(s_w, valid_w)
        o_w = jnp.einsum('bhgqk,bkhd->bqhgd', p_w, vw)
        return gc[..., 0:1] * o_c + gc[..., 1:2] * o_s + gc[..., 2:3] * o_w

    out = lax.map(q_block, jnp.arange(T // NSA_QBLOCK))
    return jnp.moveaxis(out, 0, 1).reshape(B, T, NSA_WIDTH)


def _hgrn2_chunkwise(q, k, v, logf):
    B, T, H, dk = q.shape
    dv = v.shape[-1]
    C = HG_CHUNK
    N = T // C
    to_chunks = lambda t: t.reshape(B, N, C, H, t.shape[-1]).transpose(0, 3, 1, 2, 4)
    q, k, v, logf = to_chunks(q), to_chunks(k), to_chunks(v), to_chunks(logf)
    b = jnp.cumsum(logf, axis=3)
    b_last = b[:, :, :, -1:, :]
    q_e = q * jnp.exp(b)
    k_e = k * jnp.exp(-b)
    k_tail = k * jnp.exp(b_last - b)
    causal = np.tril(np.ones((C, C), dtype=bool))
    A = jnp.where(causal, jnp.einsum('bhncd,bhnsd->bhncs', q_e, k_e), 0.0)
    o_intra = jnp.einsum('bhncs,bhnsv->bhncv', A, v)

    def step(S, inp):
        qn, kn, vn, dn = inp
        o = jnp.einsum('bhcd,bhdv->bhcv', qn, S)
        S = S * dn[..., None] + jnp.einsum('bhcd,bhcv->bhdv', kn, vn)
        return S, o

    xs = (jnp.moveaxis(q_e, 2, 0), jnp.moveaxis(k_tail, 2, 0), jnp.moveaxis(v, 2, 0),
          jnp.moveaxis(jnp.exp(b_last[:, :, :, 0]), 2, 0))
    _, o_inter = lax.scan(step, jnp.zeros((B, H, dk, dv), jnp.float32), xs)
    o = o_intra + jnp.moveaxis(o_inter, 0, 2)
    return o.transpose(0, 2, 3, 1, 4).reshape(B, T, H, dv)


def _hgrn2_mixer(x, w_in, w_out, lb, norm_g):
    B, T, _ = x.shape
    q, f, i, g = _split(x @ w_in, HG_SPLITS)
    heads = lambda t: t.astype(jnp.float32).reshape(B, T, HG_HEADS, HG_HEAD)
    lb = lb.reshape(HG_HEADS, HG_HEAD)
    f = heads(f)
    forget = lb + (1.0 - lb) * jax.nn.sigmoid(f)
    k = (1.0 - lb) * jax.nn.sigmoid(-f)
    o = _hgrn2_chunkwise(jax.nn.silu(heads(q)), k, heads(i), jnp.log(forget))
    o = o * lax.rsqrt(jnp.mean(o * o, -1, keepdims=True) + LN_EPS) * norm_g
    o = o.reshape(B, T, D_MODEL) * jax.nn.silu(g.astype(jnp.float32))
    return o.astype(x.dtype) @ w_out


def _hier_moe(h, w_rg, b_rg, w_re, b_re, w1, w3, w2):
    B, T, D = h.shape
    M = B * T
    A = M * MOE_TOPK
    xt = h.reshape(M, D)
    lg = (xt @ w_rg + b_rg).astype(jnp.float32)
    g_sel = jnp.argmax(lg, -1)
    p_grp = jnp.max(jax.nn.softmax(lg, -1), -1)
    le = (xt @ w_re + b_re).astype(jnp.float32).reshape(M, MOE_GROUPS, MOE_PER_GROUP)
    le = le[jnp.arange(M), g_sel]
    top_v, top_i = lax.top_k(le, MOE_TOPK)
    gate = (p_grp[:, None] * jax.nn.softmax(top_v, -1)).reshape(A)
    eid = (g_sel[:, None] * MOE_PER_GROUP + top_i).reshape(A)
    tok = jnp.repeat(jnp.arange(M), MOE_TOPK)
    order = jnp.argsort(eid)
    e_s, tok_s, gate_s = eid[order], tok[order], gate[order]
    counts = jnp.bincount(eid, length=MOE_EXPERTS)
    padded = (counts + MOE_BLOCK - 1) // MOE_BLOCK * MOE_BLOCK
    ends = jnp.cumsum(padded)
    start = ends - padded
    off = jnp.cumsum(counts) - counts
    dest = start[e_s] + jnp.arange(A) - off[e_s]
    P = A + MOE_EXPERTS * MOE_BLOCK
    n_blk = P // MOE_BLOCK
    xbuf = jnp.zeros((P, D), h.dtype).at[dest].set(xt[tok_s])
    blk_e = jnp.minimum(jnp.searchsorted(ends, jnp.arange(n_blk) * MOE_BLOCK, side='right'), MOE_EXPERTS - 1)

    def expert_block(args):
        xb, e = args
        return (jax.nn.silu(xb @ w1[e]) * (xb @ w3[e])) @ w2[e]

    ybuf = lax.map(expert_block, (xbuf.reshape(n_blk, MOE_BLOCK, D), blk_e)).reshape(P, D)
    y = jnp.zeros((M, D), jnp.float32).at[tok_s].add(gate_s[:, None] * ybuf[dest].astype(jnp.float32))
    return y.reshape(B, T, D).astype(h.dtype)


def setup_inputs(seed: int = 0) -> dict:
    key = jax.random.key(seed)
    keys = iter(jax.random.split(key, 40))

    def nrm(shape, scale):
        return jax.random.normal(next(keys), shape, jnp.float32) * scale

    def col_scale(spec):
        return jnp.asarray(np.concatenate([np.full(n, s, np.float32) for n, s in spec]))

    b = DN_BETA
    ev_spec = [(RW_WIDTH, 1.0), (RW_WIDTH, 1.0), (RW_WIDTH, b), (RW_DECAY_LORA, 1.0), (RW_AAA_LORA, 1.0),
               (RW_GATE_LORA, 1.0), (NSA_WIDTH, 1.0), (NSA_KV, 1.0), (NSA_KV, b), (NSA_KV, 1.0), (NSA_KV, b),
               (NSA_KV, 1.0), (NSA_KV, b), (3 * NSA_HEADS, 1.0)]
    hg_spec = [(D_MODEL, 1.0), (D_MODEL, 1.0), (D_MODEL, b), (D_MODEL, 1.0)]
    d_inv = D_MODEL ** -0.5
    return {
        'x': nrm((BATCH, SEQ, D_MODEL), 1.0),
        'p': nrm((DEPTH, BATCH, SEQ, PLE_DIM), 1.0),
        'positions': jnp.broadcast_to(jnp.arange(SEQ, dtype=jnp.int32)[None, :], (BATCH, SEQ)),
        'ev_w_in': nrm((N_EVEN, D_MODEL, EV_COLS), d_inv) * col_scale(ev_spec),
        'ev_w_out': nrm((N_EVEN, D_MODEL, D_MODEL), d_inv * b),
        'rw_mu': jax.random.uniform(next(keys), (N_EVEN, RW_COLS), jnp.float32, 0.0, 1.0),
        'rw_w0': jax.random.uniform(next(keys), (N_EVEN, RW_WIDTH), jnp.float32, -3.0, 1.0),
        'rw_w2': nrm((N_EVEN, RW_DECAY_LORA, RW_WIDTH), 0.5 * RW_DECAY_LORA ** -0.5),
        'rw_a0': nrm((N_EVEN, RW_WIDTH), 0.5),
        'rw_a2': nrm((N_EVEN, RW_AAA_LORA, RW_WIDTH), 0.5 * RW_AAA_LORA ** -0.5),
        'rw_g2': nrm((N_EVEN, RW_GATE_LORA, RW_WIDTH), RW_GATE_LORA ** -0.5),
        'rw_k_k': 0.85 + nrm((N_EVEN, RW_WIDTH), 0.05),
        'rw_k_a': 1.0 + nrm((N_EVEN, RW_WIDTH), 0.05),
        'rw_r_k': nrm((N_EVEN, RW_HEADS, RW_HEAD), 0.1),
        'rw_gn_g': 1.0 + nrm((N_EVEN, RW_WIDTH), 0.02),
        'rw_gn_b': nrm((N_EVEN, RW_WIDTH), 0.02),
        'nsa_cmp_pos': nrm((N_EVEN, 2, CMP_LEN, NSA_HEAD), 0.02),
        'nsa_cmp_w1': nrm((N_EVEN, 2, CMP_LEN * NSA_HEAD, CMP_HIDDEN), (CMP_LEN * NSA_HEAD) ** -0.5),
        'nsa_cmp_w2': nrm((N_EVEN, 2, CMP_HIDDEN, NSA_HEAD), CMP_HIDDEN ** -0.5),
        'od_w_in': nrm((N_ODD, D_MODEL, HG_COLS), d_inv) * col_scale(hg_spec),
        'od_w_out': nrm((N_ODD, D_MODEL, D_MODEL), d_inv * b),
        'hg_lb': nrm((DEPTH, D_MODEL), 0.1),
        'hg_norm_g': 1.0 + nrm((N_ODD, HG_HEAD), 0.02),
        'moe_w_rg': nrm((DEPTH, D_MODEL, MOE_GROUPS), d_inv),
        'moe_b_rg': nrm((DEPTH, MOE_GROUPS), 0.01),
        'moe_w_re': nrm((DEPTH, D_MODEL, MOE_EXPERTS), d_inv),
        'moe_b_re': nrm((DEPTH, MOE_EXPERTS), 0.01),
        'moe_w1': nrm((DEPTH, MOE_EXPERTS, D_MODEL, MOE_HIDDEN), d_inv),
        'moe_w3': nrm((DEPTH, MOE_EXPERTS, D_MODEL, MOE_HIDDEN), d_inv),
        'moe_w2': nrm((DEPTH, MOE_EXPERTS, MOE_HIDDEN, D_MODEL), MOE_HIDDEN ** -0.5 * b),
        'ln_g': 1.0 + nrm((DEPTH, 2, D_MODEL), 0.02),
        'ln_b': nrm((DEPTH, 2, D_MODEL), 0.02),
        'ple_w': nrm((DEPTH, PLE_DIM, D_MODEL), PLE_DIM ** -0.5),
        'ple_gate_w': nrm((DEPTH, D_MODEL, D_MODEL), d_inv),
    }


def reference(x, p, positions, ev_w_in, ev_w_out, rw_mu, rw_w0, rw_w2, rw_a0, rw_a2, rw_g2, rw_k_k, rw_k_a,
              rw_r_k, rw_gn_g, rw_gn_b, nsa_cmp_pos, nsa_cmp_w1, nsa_cmp_w2, od_w_in, od_w_out, hg_lb, hg_norm_g,
              moe_w_rg, moe_b_rg, moe_w_re, moe_b_re, moe_w1, moe_w3, moe_w2, ln_g, ln_b, ple_w, ple_gate_w):
    cos, sin = _rope_tables(positions, NSA_HEAD)
    lb_soft = jax.nn.softmax(hg_lb.astype(jnp.float32), axis=0)
    lb_all = jnp.cumsum(lb_soft, axis=0) - lb_soft[0:1]
    for li in range(DEPTH):
        j = li // 2
        if li % 2 == 0:
            z_rw, z_nsa = jnp.split(x @ ev_w_in[j], [RW_COLS], axis=-1)
            y_rw = _rwkv7_group(z_rw, rw_mu[j], rw_w0[j], rw_w2[j], rw_a0[j], rw_a2[j], rw_g2[j], rw_k_k[j],
                                rw_k_a[j], rw_r_k[j], rw_gn_g[j], rw_gn_b[j])
            y_nsa = _nsa_group(z_nsa, cos, sin, nsa_cmp_pos[j], nsa_cmp_w1[j], nsa_cmp_w2[j])
            mix = jnp.concatenate([y_rw.astype(x.dtype), y_nsa.astype(x.dtype)], -1) @ ev_w_out[j]
        else:
            mix = _hgrn2_mixer(x, od_w_in[j], od_w_out[j], lb_all[li], hg_norm_g[j])
        h = _layer_norm(DN_ALPHA * x + mix, ln_g[li, 0], ln_b[li, 0])
        ffn = _hier_moe(h, moe_w_rg[li], moe_b_rg[li], moe_w_re[li], moe_b_re[li], moe_w1[li], moe_w3[li], moe_w2[li])
        h = _layer_norm(DN_ALPHA * h + ffn, ln_g[li, 1], ln_b[li, 1])
        x = h + jax.nn.sigmoid(h @ ple_gate_w[li]) * (p[li] @ ple_w[li])
    return x
```

```python
from contextlib import ExitStack
import math, os
import numpy as np
import concourse.bass as bass
import concourse.mybir as mybir
from concourse.bass_utils import run_bass_kernel_spmd

F32 = mybir.dt.float32
BF16 = mybir.dt.bfloat16
I32 = mybir.dt.int32
U32 = mybir.dt.uint32
AF = mybir.ActivationFunctionType
ALU = mybir.AluOpType
AX = mybir.AxisListType

EPOCH = 12000
DMA_CAP = 1800


class _Op:
    __slots__ = ("eng", "fn", "reads", "writes", "dma", "semkey", "deps", "waits",
                 "inc", "sem", "val", "barrier", "seq")


class Prog:
    ENGS = ("pe", "act", "dve", "pool", "sp")

    def __init__(self, nc, same_engine_sync=True):
        self.nc = nc
        self.ops = []
        self.same_engine_sync = same_engine_sync
        self._sems = []
        self._keep = []

    def add(self, eng, fn, reads=(), writes=(), dma=False, semkey=None):
        o = _Op()
        o.eng = eng; o.fn = fn
        o.reads = tuple(reads); o.writes = tuple(writes)
        o.dma = dma; o.semkey = semkey
        o.deps = []; o.waits = []; o.inc = False; o.sem = None; o.val = 0
        o.barrier = False; o.seq = len(self.ops)
        if dma and semkey is None:
            raise ValueError("dma op needs semkey")
        self.ops.append(o)
        return o

    def barrier(self):
        for e in self.ENGS:
            o = self.add(e, None)
            o.barrier = True

    def dma(self, q, out, in_, reads, writes, semkey, **kw):
        return self.add(q, lambda e: e.dma_start(out=out, in_=in_, **kw), reads, writes,
                        dma=True, semkey=semkey)

    def mm(self, out, lhsT, rhs, start, stop, reads, writes):
        return self.add("pe", lambda e: e.matmul(out, lhsT, rhs, start=start, stop=stop),
                        reads, writes)

    def tr(self, out, in_, ident, reads, writes):
        return self.add("pe", lambda e: e.transpose(out, in_, ident), reads, writes)

    def _new_sem(self, name):
        h = self.nc.alloc_semaphore(name=name)
        self._sems.append(h)
        return h

    def finalize(self):
        ops = self.ops
        last_w = {}
        readers = {}
        last_of_eng = {}
        for o in ops:
            if o.barrier:
                continue
            deps = set()
            for k in o.reads:
                w = last_w.get(k)
                if w is not None:
                    deps.add(w)
            for k in o.writes:
                w = last_w.get(k)
                if w is not None:
                    deps.add(w)
                for r in readers.get(k, ()):
                    deps.add(r)
            deps.discard(o.seq)
            for d in deps:
                od = ops[d]
                if od.dma:
                    o.deps.append(d)
                elif od.eng != o.eng or o.dma:
                    o.deps.append(d)
                else:
                    if self.same_engine_sync and o.eng != "pe":
                        o.deps.append(d)
            for k in o.reads:
                readers.setdefault(k, []).append(o.seq)
            for k in o.writes:
                last_w[k] = o.seq
                readers[k] = []
        for o in ops:
            for d in o.deps:
                ops[d].inc = True
        eng_sem = {}
        dma_sem = {}
        last_inc_op = {}
        cur_last = {}
        for o in ops:
            if o.barrier:
                if o.eng == self.ENGS[0]:
                    for e, lo in cur_last.items():
                        lo.inc = True
                continue
            if not o.dma:
                cur_last[o.eng] = o
        waited = {e: {} for e in self.ENGS}
        released = set()
        all_dma_state = {}
        eng_state = {}
        nsem = 0
        for o in ops:
            if o.barrier:
                if o.eng == self.ENGS[-1]:
                    pass
                for e, (s, c) in eng_state.items():
                    if e != o.eng and c > 0:
                        o.waits.append((s, c))
                for k, (s, c) in all_dma_state.items():
                    if c > 0:
                        o.waits.append((s, c * 16))
                if o.eng == self.ENGS[-1]:
                    for v in dma_sem.values():
                        released.add(id(v))
                    dma_sem = {}
                continue
            for d in o.deps:
                od = ops[d]
                o.waits.append((od.sem, od.val))
            if o.dma:
                st = dma_sem.get(o.semkey)
                if st is None or st[1] >= DMA_CAP:
                    used = set(id(v) for v in dma_sem.values())
                    cands = [v for v in self._keep if id(v) not in used and id(v) in released and v[1] < DMA_CAP - 300]
                    if cands:
                        st = min(cands, key=lambda v: v[1])
                        released.discard(id(st))
                    else:
                        st = [self._new_sem("d%d" % nsem), 0]; nsem += 1
                        self._keep.append(st)
                    dma_sem[o.semkey] = st
                st[1] += 1
                o.sem = st[0]; o.val = st[1] * 16; o.inc = True
                all_dma_state[id(st)] = (st[0], st[1])
            elif o.inc:
                st = eng_sem.get(o.eng)
                if st is None or st[1] >= EPOCH:
                    st = [self._new_sem("e%d" % nsem), 0]; nsem += 1
                    eng_sem[o.eng] = st
                st[1] += 1
                o.sem = st[0]; o.val = st[1]
                eng_state[o.eng] = (st[0], st[1])
        for o in ops:
            w = waited[o.eng]
            best = {}
            for (s, v) in o.waits:
                key = id(s)
                if w.get(key, 0) >= v:
                    continue
                if key not in best or best[key][1] < v:
                    best[key] = (s, v)
            o.waits = list(best.values())
            for key, (s, v) in best.items():
                w[key] = v
        self.nsem = nsem
        return self

    def emit(self):
        nc = self.nc
        per = {e: [o for o in self.ops if o.eng == e] for e in self.ENGS}

        def run(engine, lst):
            for o in lst:
                for (s, v) in o.waits:
                    engine.wait_ge(s, v)
                if o.fn is None:
                    continue
                ins = o.fn(engine)
                if o.inc:
                    ins.then_inc(o.sem, 16 if o.dma else 1)

        with nc.Block() as block:
            @block.tensor
            def _(e):
                run(e, per["pe"])

            @block.scalar
            def _(e):
                run(e, per["act"])

            @block.vector
            def _(e):
                run(e, per["dve"])

            @block.gpsimd
            def _(e):
                run(e, per["pool"])

            @block.sync
            def _(e):
                run(e, per["sp"])


ALPHA = float((2 * 2) ** 0.25)
LN_EPS = 1e-5
BLK = 512
NEXP = 32


_UNIQ = [0]


def SB(es, nc, name, shape, dt=F32):
    _UNIQ[0] += 1
    return es.enter_context(nc.sbuf_tensor("s%d_%s" % (_UNIQ[0], name), list(shape), dt))


def PS(es, nc, name, shape, dt=F32):
    _UNIQ[0] += 1
    return es.enter_context(nc.psum_tensor("p%d_%s" % (_UNIQ[0], name), list(shape), dt))


def layer_norm_tile(P, u, out, g_t, b_t, st, mv, sd, rs, tag, eng2="pool"):
    ku, ko = tag + "u", tag + "o"
    P.add("dve", lambda e: e.bn_stats(st[:, 0, :], u[:, 0:512]), [ku], [tag + "st0"])
    P.add("dve", lambda e: e.bn_stats(st[:, 1, :], u[:, 512:1024]), [ku], [tag + "st1"])
    P.add("dve", lambda e: e.bn_aggr(mv[:], st[:].rearrange("p a b -> p (a b)")),
          [tag + "st0", tag + "st1"], [tag + "mv"])
    P.add("act", lambda e: e.activation(sd[:], mv[:, 1:2], AF.Sqrt, bias=LN_EPS, scale=1.0),
          [tag + "mv"], [tag + "sd"])
    P.add("dve", lambda e: e.reciprocal(rs[:], sd[:]), [tag + "sd"], [tag + "rs"])
    P.add("dve", lambda e: e.tensor_scalar(out, u, mv[:, 0:1], rs[:, 0:1], ALU.subtract, ALU.mult),
          [ku, tag + "mv", tag + "rs"], [ko])
    P.add(eng2, lambda e: e.tensor_tensor(out, out, g_t, ALU.mult), [ko, tag + "g"], [ko])
    P.add(eng2, lambda e: e.tensor_tensor(out, out, b_t, ALU.add), [ko, tag + "b"], [ko])


def phase_t1(P, nc, N, yT_d, x_d, wout_d, lng_d, lnb_d, wrg_d, wre_d, brg_d, bre_d,
             ident_d, tri_d, h1_d, R):
    NT = N // 128
    with ExitStack() as es:
        wout_b = SB(es, nc, "wout_b", [128, 8, 1024], BF16)
        wst = SB(es, nc, "wst", [128, 8, 512], F32)
        g_t = SB(es, nc, "g_t", [128, 1024]); b_t = SB(es, nc, "b_t", [128, 1024])
        wr = SB(es, nc, "wr", [128, 8, 36]); br = SB(es, nc, "br", [128, 36])
        ident = SB(es, nc, "ident", [128, 128]); tri = SB(es, nc, "tri", [128, 256])
        yt = [SB(es, nc, "yt%d" % i, [128, 8, 128]) for i in range(2)]
        ytb = [SB(es, nc, "ytb%d" % i, [128, 8, 128], BF16) for i in range(2)]
        xt = [SB(es, nc, "xt%d" % i, [128, 1024]) for i in range(2)]
        u = SB(es, nc, "u", [128, 1024]); h1 = [SB(es, nc, "h1_%d" % i, [128, 1024]) for i in range(2)]
        h1T = SB(es, nc, "h1T", [128, 8, 128])
        st = SB(es, nc, "st", [128, 2, 6]); mv = SB(es, nc, "mv", [128, 2])
        sd = SB(es, nc, "sd", [128, 1]); rs = SB(es, nc, "rs", [128, 1])
        lg = SB(es, nc, "lg", [128, 36]); sm = SB(es, nc, "sm", [128, 16])
        ohg = SB(es, nc, "ohg", [128, 4]); eg = SB(es, nc, "eg", [128, 4])
        le = SB(es, nc, "le", [128, 8]); top8 = SB(es, nc, "top8", [128, 8])
        m12 = SB(es, nc, "m12", [128, 2, 8]); t12 = SB(es, nc, "t12", [128, 32]); junk = SB(es, nc, "junk", [128, 32])
        psA = [PS(es, nc, "psA%d" % i, [128, 512]) for i in range(2)]
        psT = [PS(es, nc, "psT%d" % i, [128, 512]) for i in range(2)]
        psL = PS(es, nc, "psL", [128, 512]); psR = PS(es, nc, "psR", [128, 512])
        OH, rank, gates, base = R["OH"], R["rank"], R["gates"], R["base"]

        for hh in range(2):
            P.dma("sp", wst[:], wout_d[:, hh * 512:(hh + 1) * 512].rearrange("(c p) n -> p c n", p=128),
                  [], ["wst"], "wst")
            P.add("act", lambda e, hh=hh: e.activation(wout_b[:, :, hh * 512:(hh + 1) * 512], wst[:], AF.Copy),
                  ["wst"], ["wout_b"])
        P.dma("sp", g_t[:], lng_d.partition_broadcast(128), [], ["L1g"], "cst1")
        P.dma("sp", b_t[:], lnb_d.partition_broadcast(128), [], ["L1b"], "cst1")
        P.dma("pool", wr[:, :, 0:4], wrg_d.rearrange("(c p) e -> p c e", p=128), [], ["wr"], "cst2")
        P.dma("pool", wr[:, :, 4:36], wre_d.rearrange("(c p) e -> p c e", p=128), [], ["wr"], "cst2")
        P.dma("pool", br[:, 0:4], brg_d.partition_broadcast(128), [], ["br"], "cst2")
        P.dma("pool", br[:, 4:36], bre_d.partition_broadcast(128), [], ["br"], "cst2")
        P.dma("pool", ident[:], ident_d, [], ["ident"], "cst2")
        P.dma("pool", tri[:], tri_d, [], ["tri"], "cst2")
        P.add("dve", lambda e: e.memset(base[:], 0.0), [], ["base"])

        for i in range(NT):
            s = i % 2
            n0 = i * 128
            kyt, kytb, kxt, kh1 = "yt%d" % s, "ytb%d" % s, "xt%d" % s, "h1_%d" % s
            P.dma("sp", yt[s][:], yT_d[:, n0:n0 + 128].rearrange("(c p) n -> p c n", p=128), [], [kyt], kyt)
            P.dma("sp", xt[s][:], x_d[n0:n0 + 128, :], [], [kxt], kxt)
            P.add("act", lambda e, s=s: e.activation(ytb[s][:], yt[s][:], AF.Copy), [kyt], [kytb])
            for hh in range(2):
                for c in range(8):
                    P.mm(psA[hh][:], ytb[s][:, c, :], wout_b[:, c, hh * 512:(hh + 1) * 512], c == 0, c == 7,
                         [kytb, "wout_b"], ["psA%d" % hh])
                P.add("dve", lambda e, s=s, hh=hh: e.scalar_tensor_tensor(
                    u[:, hh * 512:(hh + 1) * 512], xt[s][:, hh * 512:(hh + 1) * 512], ALPHA, psA[hh][:],
                    ALU.mult, ALU.add), [kxt, "psA%d" % hh], ["L1u"])
            P.add("dve", lambda e: e.bn_stats(st[:, 0, :], u[:, 0:512]), ["L1u"], ["L1st0"])
            P.add("dve", lambda e: e.bn_stats(st[:, 1, :], u[:, 512:1024]), ["L1u"], ["L1st1"])
            P.add("dve", lambda e: e.bn_aggr(mv[:], st[:].rearrange("p a b -> p (a b)")), ["L1st0", "L1st1"], ["L1mv"])
            P.add("act", lambda e: e.activation(sd[:], mv[:, 1:2], AF.Sqrt, bias=LN_EPS, scale=1.0), ["L1mv"], ["L1sd"])
            P.add("dve", lambda e: e.reciprocal(rs[:], sd[:]), ["L1sd"], ["L1rs"])
            P.add("dve", lambda e, s=s: e.tensor_scalar(h1[s][:], u[:], mv[:, 0:1], rs[:, 0:1], ALU.subtract, ALU.mult),
                  ["L1u", "L1mv", "L1rs"], [kh1])
            P.add("pool", lambda e, s=s: e.tensor_tensor(h1[s][:], h1[s][:], g_t[:], ALU.mult), [kh1, "L1g"], [kh1])
            P.add("pool", lambda e, s=s: e.tensor_tensor(h1[s][:], h1[s][:], b_t[:], ALU.add), [kh1, "L1b"], [kh1])
            P.dma("sp", h1_d[n0:n0 + 128, :], h1[s][:], [kh1], [], "st" + kh1)
            for c in range(8):
                P.tr(psT[c // 4][:, (c % 4) * 128:(c % 4 + 1) * 128], h1[s][:, c * 128:(c + 1) * 128], ident[:],
                     [kh1, "ident"], ["psT%d" % (c // 4)])
            for hh in range(2):
                P.add("act", lambda e, hh=hh: e.activation(
                    h1T[:, hh * 4:(hh + 1) * 4, :], psT[hh][:].rearrange("p (c n) -> p c n", c=4), AF.Copy),
                    ["psT%d" % hh], ["h1T"])
            for c in range(8):
                P.mm(psL[:, 0:36], h1T[:, c, :], wr[:, c, :], c == 0, c == 7, ["h1T", "wr"], ["psL"])
            P.add("dve", lambda e: e.tensor_tensor(lg[:], psL[:, 0:36], br[:], ALU.add), ["psL", "br"], ["lg"])
            P.add("dve", lambda e: e.reduce_max(sm[:, 0:1], lg[:, 0:4], AX.X), ["lg"], ["sm0"])
            P.add("dve", lambda e: e.tensor_scalar(ohg[:], lg[:, 0:4], sm[:, 0:1], None, ALU.is_equal), ["lg", "sm0"], ["ohg"])
            P.add("dve", lambda e: e.tensor_scalar(sm[:, 1:2], sm[:, 0:1], -1.0, None, ALU.mult), ["sm0"], ["sm1"])
            P.add("act", lambda e: e.activation(eg[:], lg[:, 0:4], AF.Exp, bias=sm[:, 1:2], scale=1.0, accum_out=sm[:, 2:3]),
                  ["lg", "sm1"], ["sm2", "eg"])
            P.add("dve", lambda e: e.reciprocal(sm[:, 3:4], sm[:, 2:3]), ["sm2"], ["sm3"])
            P.add("dve", lambda e: e.tensor_scalar(le[:], lg[:, 4:12], ohg[:, 0:1], None, ALU.mult), ["lg", "ohg"], ["le"])
            for g in range(1, 4):
                P.add("dve", lambda e, g=g: e.scalar_tensor_tensor(le[:], lg[:, 4 + 8 * g:12 + 8 * g], ohg[:, g:g + 1], le[:],
                                                                   ALU.mult, ALU.add), ["lg", "ohg", "le"], ["le"])
            P.add("dve", lambda e: e.max(top8[:], le[:]), ["le"], ["top8"])
            P.add("dve", lambda e: e.tensor_scalar(m12[:, 0, :], le[:], top8[:, 0:1], None, ALU.is_equal), ["le", "top8"], ["m1"])
            P.add("dve", lambda e: e.tensor_scalar(m12[:, 1, :], le[:], top8[:, 1:2], None, ALU.is_equal), ["le", "top8"], ["m2"])
            P.add("dve", lambda e: e.tensor_tensor(sm[:, 4:5], top8[:, 1:2], top8[:, 0:1], ALU.subtract), ["top8"], ["sm4"])
            P.add("act", lambda e: e.activation(sm[:, 5:6], sm[:, 4:5], AF.Exp), ["sm4"], ["sm5"])
            P.add("dve", lambda e: e.tensor_scalar(sm[:, 6:7], sm[:, 5:6], 1.0, None, ALU.add), ["sm5"], ["sm6"])
            P.add("dve", lambda e: e.reciprocal(sm[:, 7:8], sm[:, 6:7]), ["sm6"], ["sm7"])
            P.add("dve", lambda e, i=i: e.tensor_tensor(gates[:, i, 0:1], sm[:, 7:8], sm[:, 3:4], ALU.mult), ["sm7", "sm3"], ["gates"])
            P.add("dve", lambda e, i=i: e.tensor_tensor(gates[:, i, 1:2], gates[:, i, 0:1], sm[:, 5:6], ALU.mult), ["gates", "sm5"], ["gates"])
            for j in range(2):
                for g in range(4):
                    P.add("dve", lambda e, i=i, j=j, g=g: e.tensor_scalar(
                        OH[:, i, j, 8 * g:8 * g + 8], m12[:, j, :], ohg[:, g:g + 1], None, ALU.mult),
                        ["m1", "m2", "ohg"], ["OH"])
            P.mm(psR[:, 0:32], tri[:, 0:128], OH[:, i, 0, :], True, True, ["tri", "OH"], ["psR"])
            P.mm(psR[:, 32:64], tri[:, 128:256], OH[:, i, 0, :], True, True, ["tri", "OH"], ["psR"])
            P.mm(psR[:, 64:96], tri[:, 0:128], OH[:, i, 1, :], True, True, ["tri", "OH"], ["psR"])
            P.mm(psR[:, 96:128], tri[:, 128:256], OH[:, i, 1, :], True, True, ["tri", "OH"], ["psR"])
            for j in range(2):
                P.add("dve", lambda e, j=j: e.tensor_tensor(t12[:], base[:], psR[:, 64 * j:64 * j + 32], ALU.add),
                      ["base", "psR"], ["t12"])
                P.add("dve", lambda e, i=i, j=j: e.tensor_tensor(junk[:], OH[:, i, j, :], t12[:], ALU.mult),
                      ["OH", "t12"], ["junk"])
                P.add("dve", lambda e, i=i, j=j: e.reduce_sum(rank[:, i, j:j + 1], junk[:], AX.X), ["junk"], ["rank"])
                P.add("dve", lambda e, j=j: e.tensor_tensor(base[:], base[:], psR[:, 64 * j + 32:64 * j + 64], ALU.add),
                      ["base", "psR"], ["base"])
    P.barrier()


def phase_t2(P, nc, N, h1_d, xbuf_d, thr_d, iop_d, R):
    NT = N // 128
    NBLK = (2 * N) // BLK + NEXP
    OH, rank, base = R["OH"], R["rank"], R["base"]
    dest_i, widx = R["dest_i"], R["widx"]
    sh = BLK.bit_length() - 1
    with ExitStack() as es:
        ci = SB(es, nc, "ci", [128, 32], I32); padded = SB(es, nc, "padded", [128, 32])
        ones = SB(es, nc, "ones32", [128, 32]); ends = SB(es, nc, "ends", [128, 32]); start = SB(es, nc, "start", [128, 32])
        tmp = SB(es, nc, "tmpd", [128, NT * 2, 32]); dsum = SB(es, nc, "dsum", [128, NT * 2])
        thr = SB(es, nc, "thr", [128, NBLK]); iop = SB(es, nc, "iop", [128, 1])
        cmp = SB(es, nc, "cmp", [128, NBLK, 32]); be = SB(es, nc, "be", [128, NBLK])
        hx = [SB(es, nc, "hx%d" % i, [128, 1024]) for i in range(2)]
        P.dma("sp", thr[:], thr_d.partition_broadcast(128), [], ["thr"], "c2a")
        P.dma("sp", iop[:], iop_d, [], ["iop"], "c2a")
        P.add("dve", lambda e: e.tensor_copy(ci[:], base[:]), ["base"], ["ci"])
        P.add("dve", lambda e: e.tensor_scalar(ci[:], ci[:], BLK - 1, None, ALU.add), ["ci"], ["ci"])
        P.add("dve", lambda e: e.tensor_scalar(ci[:], ci[:], sh, None, ALU.arith_shift_right), ["ci"], ["ci"])
        P.add("dve", lambda e: e.tensor_scalar(ci[:], ci[:], sh, None, ALU.logical_shift_left), ["ci"], ["ci"])
        P.add("dve", lambda e: e.tensor_copy(padded[:], ci[:]), ["ci"], ["padded"])
        P.add("dve", lambda e: e.memset(ones[:], 1.0), [], ["ones32"])
        P.add("dve", lambda e: e.tensor_tensor_scan(ends[:], ones[:], padded[:], 0.0, ALU.mult, ALU.add),
              ["ones32", "padded"], ["ends"])
        P.add("dve", lambda e: e.tensor_tensor(start[:], ends[:], padded[:], ALU.subtract), ["ends", "padded"], ["start"])
        P.add("dve", lambda e: e.tensor_tensor(
            tmp[:], OH[:].rearrange("p i j e -> p (i j) e"), start[:].unsqueeze(1).to_broadcast([128, NT * 2, 32]), ALU.mult),
            ["OH", "start"], ["tmpd"])
        P.add("dve", lambda e: e.reduce_sum(dsum[:], tmp[:], AX.X), ["tmpd"], ["dsum"])
        P.add("dve", lambda e: e.tensor_tensor(dsum[:], dsum[:], rank[:].rearrange("p i j -> p (i j)"), ALU.add),
              ["dsum", "rank"], ["dsum"])
        P.add("dve", lambda e: e.tensor_copy(dest_i[:], dsum[:]), ["dsum"], ["dest_i"])
        P.add("dve", lambda e: e.tensor_tensor(
            cmp[:], ends[:].unsqueeze(1).to_broadcast([128, NBLK, 32]), thr[:].unsqueeze(2).to_broadcast([128, NBLK, 32]),
            ALU.is_le), ["ends", "thr"], ["cmp"])
        P.add("dve", lambda e: e.reduce_sum(be[:], cmp[:], AX.X), ["cmp"], ["be"])
        P.add("dve", lambda e: e.tensor_scalar(be[:], be[:], float(NEXP - 1), 128.0, ALU.min, ALU.mult), ["be"], ["be"])
        P.add("dve", lambda e: e.tensor_scalar(be[:], be[:], iop[:, 0:1], None, ALU.add), ["be", "iop"], ["be"])
        P.add("dve", lambda e: e.tensor_copy(widx[:], be[:]), ["be"], ["widx"])
        for i in range(NT):
            s = i % 2
            k = "hx%d" % s
            P.dma("sp", hx[s][:], h1_d[i * 128:(i + 1) * 128, :], [], [k], k)
            for j in range(2):
                P.add("pool", lambda e, i=i, j=j, s=s: e.indirect_dma_start(
                    xbuf_d, bass.IndirectOffsetOnAxis(dest_i[:, 2 * i + j:2 * i + j + 1], 0), hx[s][:], None),
                    [k, "dest_i"], [], dma=True, semkey="sc%d_%d" % (s, j))
    P.barrier()


def phase_t3(P, nc, N, xbuf_d, ybuf_d, w1_d, w3_d, w2_d, ident_d, R):
    NBLK = (2 * N) // BLK + NEXP
    widx = R["widx"]
    NS = BLK // 128
    with ExitStack() as es:
        ident = SB(es, nc, "ident3", [128, 128])
        ws = [SB(es, nc, "ws%d" % i, [128, 4096]) for i in range(3)]
        w1b = [SB(es, nc, "w1b%d" % i, [128, 8, 4, 128], BF16) for i in range(2)]
        w3b = [SB(es, nc, "w3b%d" % i, [128, 8, 4, 128], BF16) for i in range(2)]
        w2b = [SB(es, nc, "w2b%d" % i, [128, 4, 1024], BF16) for i in range(2)]
        xb = SB(es, nc, "xb", [128, NS, 1024]); xbb = SB(es, nc, "xbb", [128, NS, 1024], BF16)
        identb = SB(es, nc, "identb3", [128, 128], BF16)
        xbT = SB(es, nc, "xbT", [128, 8, BLK], BF16)
        a1 = SB(es, nc, "a1", [128, BLK])
        hact = SB(es, nc, "hact", [128, 4, BLK], BF16)
        ysb = [SB(es, nc, "ysb%d" % i, [128, 1024]) for i in range(2)]
        psT = [PS(es, nc, "ps3T%d" % i, [128, 1024], BF16) for i in range(2)]
        psH1 = PS(es, nc, "psH1", [128, 512]); psH3 = PS(es, nc, "psH3", [128, 512])
        psY = [PS(es, nc, "psY%d" % i, [128, 512]) for i in range(2)]
        P.dma("sp", ident[:], ident_d, [], ["ident3"], "c3")
        P.add("act", lambda e: e.activation(identb[:], ident[:], AF.Copy), ["ident3"], ["identb3"])
        for b in range(NBLK):
            s = b % 2
            for wi, wd in enumerate((w1_d, w3_d, w2_d)):
                P.add("pool", lambda e, wi=wi, wd=wd, b=b: e.indirect_dma_start(
                    ws[wi][:], None, wd, bass.IndirectOffsetOnAxis(widx[:, b:b + 1], 0)),
                    ["widx"], ["ws%d" % wi], dma=True, semkey="ws%d" % wi)
            P.add("act", lambda e, s=s: e.activation(
                w1b[s][:], ws[0][:].rearrange("p (c q h) -> p c h q", c=8, q=128, h=4), AF.Copy), ["ws0"], ["w1b%d" % s])
            P.add("pool", lambda e, s=s: e.tensor_copy(
                w3b[s][:], ws[1][:].rearrange("p (c q h) -> p c h q", c=8, q=128, h=4)), ["ws1"], ["w3b%d" % s])
            P.add("dve", lambda e, s=s: e.tensor_copy(
                w2b[s][:], ws[2][:].rearrange("p (c n) -> p c n", c=4)), ["ws2"], ["w2b%d" % s])
            P.dma("sp", xb[:], xbuf_d[b * BLK:(b + 1) * BLK, :].rearrange("(s p) d -> p s d", p=128), [], ["xb"], "xb")
            P.add("act", lambda e: e.activation(xbb[:, 0:NS // 2, :], xb[:, 0:NS // 2, :], AF.Copy), ["xb"], ["xbb0"])
            P.add("pool", lambda e: e.tensor_copy(xbb[:, NS // 2:NS, :], xb[:, NS // 2:NS, :]), ["xb"], ["xbb1"])
            for sub in range(NS):
                for c in range(8):
                    P.tr(psT[c // 4][:, (c % 4) * 128:(c % 4 + 1) * 128],
                         xbb[:, sub, :].rearrange("p (q c) -> p c q", c=8)[:, c, :], identb[:],
                         ["xbb%d" % (sub // (NS // 2)), "identb3"], ["ps3T%d" % (c // 4)])
                for hh in range(2):
                    eng = "act" if hh == 0 else "dve"
                    if eng == "act":
                        P.add("act", lambda e, hh=hh, sub=sub: e.activation(
                            xbT[:, hh * 4:(hh + 1) * 4, sub * 128:(sub + 1) * 128],
                            psT[hh][:, 0:512].rearrange("p (c n) -> p c n", c=4), AF.Copy), ["ps3T%d" % hh], ["xbT"])
                    else:
                        P.add("dve", lambda e, hh=hh, sub=sub: e.tensor_copy(
                            xbT[:, hh * 4:(hh + 1) * 4, sub * 128:(sub + 1) * 128],
                            psT[hh][:, 0:512].rearrange("p (c n) -> p c n", c=4)), ["ps3T%d" % hh], ["xbT"])
            for hc in range(4):
                for c in range(8):
                    P.mm(psH1[:, 0:BLK], w1b[s][:, c, hc, :], xbT[:, c, :], c == 0, c == 7, ["w1b%d" % s, "xbT"], ["psH1"])
                for c in range(8):
                    P.mm(psH3[:, 0:BLK], w3b[s][:, c, hc, :], xbT[:, c, :], c == 0, c == 7, ["w3b%d" % s, "xbT"], ["psH3"])
                P.add("act", lambda e: e.activation(a1[:], psH1[:, 0:BLK], AF.Silu), ["psH1"], ["a1"])
                P.add("dve", lambda e, hc=hc: e.tensor_tensor(hact[:, hc, :], a1[:], psH3[:, 0:BLK], ALU.mult),
                      ["a1", "psH3"], ["hact"])
            for sub in range(NS):
                ys = sub % 2
                for hh in range(2):
                    for hc in range(4):
                        P.mm(psY[hh][:], hact[:, hc, sub * 128:(sub + 1) * 128], w2b[s][:, hc, hh * 512:(hh + 1) * 512],
                             hc == 0, hc == 3, ["hact", "w2b%d" % s], ["psY%d" % hh])
                P.add("act", lambda e, ys=ys: e.activation(ysb[ys][:, 0:512], psY[0][:], AF.Copy), ["psY0"], ["ysb%d" % ys])
                P.add("dve", lambda e, ys=ys: e.tensor_copy(ysb[ys][:, 512:1024], psY[1][:]), ["psY1"], ["ysb%d" % ys])
                r0 = b * BLK + sub * 128
                P.dma("sp", ybuf_d[r0:r0 + 128, :], ysb[ys][:], ["ysb%d" % ys], [], "stysb%d" % ys)
    P.barrier()


def phase_t4(P, nc, N, h1_d, ybuf_d, p_d, lng_d, lnb_d, wg_d, wp_d, ident_d, out_d, R):
    NT = N // 128
    dest_i, gates = R["dest_i"], R["gates"]
    with ExitStack() as es:
        wg_b = SB(es, nc, "wg_b", [128, 8, 1024], BF16); wp_b = SB(es, nc, "wp_b", [128, 2, 1024], BF16)
        wst = SB(es, nc, "wst4", [128, 8, 512], F32)
        g_t = SB(es, nc, "g_t4", [128, 1024]); b_t = SB(es, nc, "b_t4", [128, 1024])
        ident = SB(es, nc, "ident4", [128, 128])
        y1 = [SB(es, nc, "y1_%d" % i, [128, 1024]) for i in range(2)]
        y2 = [SB(es, nc, "y2_%d" % i, [128, 1024]) for i in range(2)]
        hh1 = [SB(es, nc, "hh1_%d" % i, [128, 1024]) for i in range(2)]
        pt = [SB(es, nc, "pt%d" % i, [128, 256]) for i in range(2)]
        u = SB(es, nc, "u4", [128, 1024]); h2 = SB(es, nc, "h2", [128, 1024])
        h2T = SB(es, nc, "h2T", [128, 8, 128], BF16); pT = SB(es, nc, "pT", [128, 2, 128], BF16)
        sg = SB(es, nc, "sg", [128, 1024]); ot = [SB(es, nc, "ot%d" % i, [128, 1024]) for i in range(2)]
        st = SB(es, nc, "st4", [128, 2, 6]); mv = SB(es, nc, "mv4", [128, 2])
        sd = SB(es, nc, "sd4", [128, 1]); rs = SB(es, nc, "rs4", [128, 1])
        psT = [PS(es, nc, "ps4T%d" % i, [128, 512]) for i in range(2)]
        psG = [PS(es, nc, "psG%d" % i, [128, 512]) for i in range(2)]
        psP = [PS(es, nc, "psP%d" % i, [128, 512]) for i in range(2)]
        psPT = PS(es, nc, "psPT", [128, 512])
        for hh in range(2):
            P.dma("sp", wst[:], wg_d[:, hh * 512:(hh + 1) * 512].rearrange("(c p) n -> p c n", p=128), [], ["wst4"], "wst4")
            P.add("act", lambda e, hh=hh: e.activation(wg_b[:, :, hh * 512:(hh + 1) * 512], wst[:], AF.Copy), ["wst4"], ["wg_b"])
        for hh in range(2):
            P.dma("sp", wst[:, 0:2, :], wp_d[:, hh * 512:(hh + 1) * 512].rearrange("(c p) n -> p c n", p=128), [], ["wst4"], "wst4")
            P.add("act", lambda e, hh=hh: e.activation(wp_b[:, :, hh * 512:(hh + 1) * 512], wst[:, 0:2, :], AF.Copy), ["wst4"], ["wp_b"])
        P.dma("sp", g_t[:], lng_d.partition_broadcast(128), [], ["L2g"], "c4")
        P.dma("sp", b_t[:], lnb_d.partition_broadcast(128), [], ["L2b"], "c4")
        P.dma("sp", ident[:], ident_d, [], ["ident4"], "c4")
        for i in range(NT):
            s = i % 2
            n0 = i * 128
            ky1, ky2, kh, kp, ko = "y1_%d" % s, "y2_%d" % s, "hh1_%d" % s, "pt%d" % s, "ot%d" % s
            P.add("pool", lambda e, i=i, s=s: e.indirect_dma_start(
                y1[s][:], None, ybuf_d, bass.IndirectOffsetOnAxis(dest_i[:, 2 * i:2 * i + 1], 0)),
                ["dest_i"], [ky1], dma=True, semkey=ky1)
            P.add("pool", lambda e, i=i, s=s: e.indirect_dma_start(
                y2[s][:], None, ybuf_d, bass.IndirectOffsetOnAxis(dest_i[:, 2 * i + 1:2 * i + 2], 0)),
                ["dest_i"], [ky2], dma=True, semkey=ky2)
            P.dma("sp", hh1[s][:], h1_d[n0:n0 + 128, :], [], [kh], kh)
            P.dma("sp", pt[s][:], p_d[n0:n0 + 128, :], [], [kp], kp)
            P.add("dve", lambda e, i=i, s=s: e.tensor_scalar(y1[s][:], y1[s][:], gates[:, i, 0:1], None, ALU.mult),
                  [ky1, "gates"], [ky1])
            P.add("dve", lambda e, i=i, s=s: e.scalar_tensor_tensor(y1[s][:], y2[s][:], gates[:, i, 1:2], y1[s][:], ALU.mult, ALU.add),
                  [ky1, ky2, "gates"], [ky1])
            P.add("dve", lambda e, s=s: e.scalar_tensor_tensor(u[:], hh1[s][:], ALPHA, y1[s][:], ALU.mult, ALU.add),
                  [kh, ky1], ["L2u"])
            P.add("dve", lambda e: e.bn_stats(st[:, 0, :], u[:, 0:512]), ["L2u"], ["L2st0"])
            P.add("dve", lambda e: e.bn_stats(st[:, 1, :], u[:, 512:1024]), ["L2u"], ["L2st1"])
            P.add("dve", lambda e: e.bn_aggr(mv[:], st[:].rearrange("p a b -> p (a b)")), ["L2st0", "L2st1"], ["L2mv"])
            P.add("act", lambda e: e.activation(sd[:], mv[:, 1:2], AF.Sqrt, bias=LN_EPS, scale=1.0), ["L2mv"], ["L2sd"])
            P.add("dve", lambda e: e.reciprocal(rs[:], sd[:]), ["L2sd"], ["L2rs"])
            P.add("dve", lambda e: e.tensor_scalar(h2[:], u[:], mv[:, 0:1], rs[:, 0:1], ALU.subtract, ALU.mult),
                  ["L2u", "L2mv", "L2rs"], ["h2"])
            P.add("pool", lambda e: e.tensor_tensor(h2[:], h2[:], g_t[:], ALU.mult), ["h2", "L2g"], ["h2"])
            P.add("pool", lambda e: e.tensor_tensor(h2[:], h2[:], b_t[:], ALU.add), ["h2", "L2b"], ["h2"])
            for c in range(8):
                P.tr(psT[c // 4][:, (c % 4) * 128:(c % 4 + 1) * 128], h2[:, c * 128:(c + 1) * 128], ident[:],
                     ["h2", "ident4"], ["ps4T%d" % (c // 4)])
            for hh in range(2):
                P.add("act", lambda e, hh=hh: e.activation(
                    h2T[:, hh * 4:(hh + 1) * 4, :], psT[hh][:].rearrange("p (c n) -> p c n", c=4), AF.Copy),
                    ["ps4T%d" % hh], ["h2T"])
            for c in range(2):
                P.tr(psPT[:, c * 128:(c + 1) * 128], pt[s][:, c * 128:(c + 1) * 128], ident[:], [kp, "ident4"], ["psPT"])
            P.add("dve", lambda e: e.tensor_copy(pT[:], psPT[:, 0:256].rearrange("p (c n) -> p c n", c=2)), ["psPT"], ["pT"])
            for hh in range(2):
                for c in range(8):
                    P.mm(psG[hh][:], h2T[:, c, :], wg_b[:, c, hh * 512:(hh + 1) * 512], c == 0, c == 7, ["h2T", "wg_b"], ["psG%d" % hh])
                for c in range(2):
                    P.mm(psP[hh][:], pT[:, c, :], wp_b[:, c, hh * 512:(hh + 1) * 512], c == 0, c == 1, ["pT", "wp_b"], ["psP%d" % hh])
                P.add("act", lambda e, hh=hh: e.activation(sg[:, hh * 512:(hh + 1) * 512], psG[hh][:], AF.Sigmoid), ["psG%d" % hh], ["sg"])
                P.add("dve", lambda e, hh=hh: e.tensor_tensor(sg[:, hh * 512:(hh + 1) * 512], sg[:, hh * 512:(hh + 1) * 512], psP[hh][:], ALU.mult),
                      ["sg", "psP%d" % hh], ["sg"])
            P.add("pool", lambda e, s=s: e.tensor_tensor(ot[s][:], sg[:], h2[:], ALU.add), ["sg", "h2"], [ko])
            P.dma("sp", out_d[n0:n0 + 128, :], ot[s][:], [ko], [], "st" + ko)
    P.barrier()


CH = 64
STAGE = int(os.environ.get('STAGE', '9'))


def gemm_fm(P, nc, N, x_d, w_d, C, out_d, ident_d, tag):
    NCC = (C + 127) // 128
    with ExitStack() as es:
        wb = SB(es, nc, tag + "wb", [128, 8, C], BF16)
        wst = [SB(es, nc, tag + "wst%d" % i, [128, 8, 512]) for i in range(2)]
        ident = SB(es, nc, tag + "id", [128, 128])
        xt = SB(es, nc, tag + "xt", [128, 4, 1024]); xtb = SB(es, nc, tag + "xtb", [128, 4, 1024], BF16)
        identb = SB(es, nc, tag + "idb", [128, 128], BF16)
        xT = SB(es, nc, tag + "xT", [128, 8, 512], BF16)
        ot = [SB(es, nc, tag + "ot%d" % i, [128, 512]) for i in range(3)]
        psT = [PS(es, nc, tag + "psT%d" % i, [128, 1024], BF16) for i in range(2)]
        psO = [PS(es, nc, tag + "psO%d" % i, [128, 512]) for i in range(2)]
        P.dma("sp", ident[:], ident_d, [], [tag + "id"], tag + "id")
        P.add("act", lambda e: e.activation(identb[:], ident[:], AF.Copy), [tag + "id"], [tag + "idb"])
        npc = (C + 511) // 512
        for pc in range(npc):
            s = pc % 2
            c0 = pc * 512
            cw = min(512, C - c0)
            P.dma("sp" if s == 0 else "pool", wst[s][:, :, 0:cw], w_d[:, c0:c0 + cw].rearrange("(c p) n -> p c n", p=128),
                  [], [tag + "wst%d" % s], tag + "wst%d" % s)
            if s == 0:
                P.add("act", lambda e, s=s, c0=c0, cw=cw: e.activation(wb[:, :, c0:c0 + cw], wst[s][:, :, 0:cw], AF.Copy),
                      [tag + "wst%d" % s], [tag + "wb"])
            else:
                P.add("dve", lambda e, s=s, c0=c0, cw=cw: e.tensor_copy(wb[:, :, c0:c0 + cw], wst[s][:, :, 0:cw]),
                      [tag + "wst%d" % s], [tag + "wb"])
        k = 0
        for tt in range(N // 512):
            n0 = tt * 512
            P.dma("sp", xt[:], x_d[n0:n0 + 512, :].rearrange("(s p) d -> p s d", p=128), [], [tag + "xt"], tag + "xt")
            P.add("act", lambda e: e.activation(xtb[:, 0:2, :], xt[:, 0:2, :], AF.Copy), [tag + "xt"], [tag + "xtb0"])
            P.add("pool", lambda e: e.tensor_copy(xtb[:, 2:4, :], xt[:, 2:4, :]), [tag + "xt"], [tag + "xtb1"])
            for c in range(8):
                b = c % 2
                for sub in range(4):
                    P.tr(psT[b][:, sub * 128:(sub + 1) * 128], xtb[:, sub, c * 128:(c + 1) * 128], identb[:],
                         [tag + "xtb%d" % (sub // 2), tag + "idb"], [tag + "psT%d" % b])
                if b == 0:
                    P.add("act", lambda e, c=c, b=b: e.activation(xT[:, c, :], psT[b][:, 0:512], AF.Copy), [tag + "psT%d" % b], [tag + "xT"])
                else:
                    P.add("dve", lambda e, c=c, b=b: e.tensor_copy(xT[:, c, :], psT[b][:, 0:512]), [tag + "psT%d" % b], [tag + "xT"])
            for cc in range(NCC):
                M = min(128, C - cc * 128)
                b = cc % 2
                o = k % 3
                k += 1
                for c in range(8):
                    P.mm(psO[b][0:M, :], wb[:, c, cc * 128:cc * 128 + M], xT[:, c, :], c == 0, c == 7,
                         [tag + "wb", tag + "xT"], [tag + "psO%d" % b])
                if b == 0:
                    P.add("act", lambda e, b=b, o=o, M=M: e.activation(ot[o][0:M, :], psO[b][0:M, :], AF.Copy),
                          [tag + "psO%d" % b], [tag + "ot%d" % o])
                else:
                    P.add("dve", lambda e, b=b, o=o, M=M: e.tensor_copy(ot[o][0:M, :], psO[b][0:M, :]),
                          [tag + "psO%d" % b], [tag + "ot%d" % o])
                P.dma("sp" if cc % 2 == 0 else "pool", out_d[cc * 128:cc * 128 + M, n0:n0 + 512], ot[o][0:M, :],
                      [tag + "ot%d" % o], [], tag + "sto%d" % o)
    P.barrier()


def rwkv_prep(P, nc, NB, T, zT_d, prm, S):
    TW = 512
    NW = T // TW
    NCW = TW // CH
    C1 = -math.exp(-0.5)
    with ExitStack() as es:
        col = SB(es, nc, "rp_col", [128, 4, 8])
        mul = SB(es, nc, "rp_mul", [128, 2])
        w2 = SB(es, nc, "rp_w2", [128, 512])
        g2 = SB(es, nc, "rp_g2", [128, 512])
        blk1 = SB(es, nc, "rp_blk1", [128, 128]); ident = SB(es, nc, "rp_id", [128, 128])
        rmask = SB(es, nc, "rp_rmask", [128, TW])
        lo = [SB(es, nc, "rp_lo%d" % i, [128, TW + 1]) for i in range(2)]
        lm = [SB(es, nc, "rp_lm%d" % i, [128, TW]) for i in range(2)]
        raw = [SB(es, nc, "rp_raw%d" % i, [128, TW + 1]) for i in range(3)]
        r = SB(es, nc, "rp_r", [128, TW]); k = SB(es, nc, "rp_k", [128, TW]); v = SB(es, nc, "rp_v", [128, TW])
        ld = SB(es, nc, "rp_ld", [128, TW]); a = SB(es, nc, "rp_a", [128, TW]); g = SB(es, nc, "rp_g", [128, TW])
        kk = SB(es, nc, "rp_kk", [128, TW]); t1 = SB(es, nc, "rp_t1", [128, TW]); t2 = SB(es, nc, "rp_t2", [128, TW])
        km = SB(es, nc, "rp_km", [128, TW]); be = SB(es, nc, "rp_be", [128, TW])
        c = SB(es, nc, "rp_c", [128, TW]); ep = SB(es, nc, "rp_ep", [128, TW]); en = SB(es, nc, "rp_en", [128, TW])
        ex = SB(es, nc, "rp_ex", [128, TW])
        o_rt = SB(es, nc, "rp_ort", [128, TW]); o_kt = SB(es, nc, "rp_okt", [128, TW]); o_kp = SB(es, nc, "rp_okp", [128, TW])
        o_bt = SB(es, nc, "rp_obt", [128, TW]); o_ktl = SB(es, nc, "rp_oktl", [128, TW]); o_btl = SB(es, nc, "rp_obtl", [128, TW])
        o_bon = SB(es, nc, "rp_obon", [128, TW]); o_pc = SB(es, nc, "rp_opc", [128, NCW])
        vtk = SB(es, nc, "rp_vtk", [128, 4, 128])
        ps1 = PS(es, nc, "rp_ps1", [128, 512]); ps2 = PS(es, nc, "rp_ps2", [128, 512]); ps3 = PS(es, nc, "rp_ps3", [128, 512])
        ps4 = PS(es, nc, "rp_ps4", [128, 512]); ps5 = PS(es, nc, "rp_ps5", [128, 512]); ps6 = PS(es, nc, "rp_ps6", [128, 512])

        def colload(j, src, fcmul=True):
            P.dma("pool", col[:, :, j:j + 1], src.rearrange("(f p o) -> p f o", p=128, o=1), [], ["rp_col"], "rp_c0", allow_slow_non_contiguous=True)
        colload(0, prm["mu"][0:512]); colload(1, prm["mu"][512:1024]); colload(2, prm["mu"][1024:1536])
        colload(3, prm["w0"]); colload(4, prm["a0"]); colload(5, prm["k_k"]); colload(6, prm["k_a"]); colload(7, prm["r_k"])
        P.dma("pool", mul[:], prm["mu"][1536:1792].rearrange("(f p) -> p f", p=128), [], ["rp_mul"], "rp_c0", allow_slow_non_contiguous=True)
        P.dma("pool", w2[0:64, :], prm["w2"], [], ["rp_w2"], "rp_c0")
        P.dma("pool", w2[64:128, :], prm["a2"], [], ["rp_w2"], "rp_c0")
        P.dma("pool", g2[:], prm["g2"], [], ["rp_g2"], "rp_c0")
        P.dma("pool", blk1[:], prm["blk1"], [], ["rp_blk1"], "rp_c0")
        P.dma("pool", ident[:], prm["ident"], [], ["rp_id"], "rp_c0")
        P.dma("pool", rmask[:], prm["rmask"].partition_broadcast(128), [], ["rp_rmask"], "rp_c0")

        def load_shift(dst, row0, n0, t0, key, q):
            if t0 == 0:
                P.add("pool", lambda e: e.memset(dst[:, 0:1], 0.0), [], [key])
                P.dma(q, dst[:, 1:TW + 1], zT_d[row0:row0 + 128, n0:n0 + TW], [], [key], key)
            else:
                P.dma(q, dst[:, :], zT_d[row0:row0 + 128, n0 - 1:n0 + TW], [], [key], key)

        def mix(dst, src, mu_ap, key_src, key_dst, eng="dve"):
            P.add("pool", lambda e: e.tensor_tensor(t2[:], src[:, 0:TW], src[:, 1:TW + 1], ALU.subtract), [key_src], ["rp_t2"])
            P.add("dve", lambda e: e.scalar_tensor_tensor(dst, t2[:], mu_ap, src[:, 1:TW + 1], ALU.mult, ALU.add),
                  ["rp_t2", key_src, "rp_col", "rp_mul"], [key_dst])

        for b in range(NB):
            for w in range(NW):
                t0 = w * TW
                n0 = b * T + t0
                for i in range(2):
                    load_shift(lo[i], 1536 + 128 * i, n0, t0, "rp_lo%d" % i, "sp")
                    mix(lm[i][:], lo[i], mul[:, i:i + 1], "rp_lo%d" % i, "rp_lm%d" % i)
                P.add("act", lambda e: e.activation(lm[0][0:64, :], lm[0][0:64, :], AF.Tanh), ["rp_lm0"], ["rp_lm0"])
                P.add("act", lambda e: e.activation(lm[1][:], lm[1][:], AF.Sigmoid), ["rp_lm1"], ["rp_lm1"])
                for fc in range(4):
                    for j, dst in enumerate((r, k, v)):
                        load_shift(raw[j], j * 512 + fc * 128, n0, t0, "rp_raw%d" % j, "sp")
                        mix(dst[:], raw[j], col[:, fc, j:j + 1], "rp_raw%d" % j, ("rp_r", "rp_k", "rp_v")[j])
                    fs = slice(fc * 128, (fc + 1) * 128)
                    P.mm(ps1[:, 0:TW], w2[0:64, fs], lm[0][0:64, :], True, True, ["rp_w2", "rp_lm0"], ["rp_ps1"])
                    P.add("act", lambda e, fc=fc: e.activation(ld[:], ps1[:, 0:TW], AF.Sigmoid, bias=col[:, fc, 3:4], scale=1.0),
                          ["rp_ps1", "rp_col"], ["rp_ld"])
                    P.add("pool", lambda e: e.tensor_scalar(ld[:], ld[:], C1, None, ALU.mult), ["rp_ld"], ["rp_ld"])
                    P.mm(ps2[:, 0:TW], w2[64:128, fs], lm[0][64:128, :], True, True, ["rp_w2", "rp_lm0"], ["rp_ps2"])
                    P.add("act", lambda e, fc=fc: e.activation(a[:], ps2[:, 0:TW], AF.Sigmoid, bias=col[:, fc, 4:5], scale=1.0),
                          ["rp_ps2", "rp_col"], ["rp_a"])
                    P.mm(ps3[:, 0:TW], g2[:, fs], lm[1][:], True, True, ["rp_g2", "rp_lm1"], ["rp_ps3"])
                    P.add("act", lambda e: e.activation(g[:], ps3[:, 0:TW], AF.Copy), ["rp_ps3"], ["rp_g"])
                    P.dma("pool", S["g"][fs, n0:n0 + TW], g[:], ["rp_g"], [], "rp_stg")
                    P.add("dve", lambda e, fc=fc: e.tensor_scalar(kk[:], k[:], col[:, fc, 5:6], None, ALU.mult), ["rp_k", "rp_col"], ["rp_kk"])
                    P.add("act", lambda e: e.activation(t1[:], kk[:], AF.Square), ["rp_kk"], ["rp_t1"])
                    P.mm(ps4[:, 0:TW], blk1[:], t1[:], True, True, ["rp_blk1", "rp_t1"], ["rp_ps4"])
                    P.add("act", lambda e: e.activation(t1[:], ps4[:, 0:TW], AF.Sqrt), ["rp_ps4"], ["rp_t1"])
                    P.add("dve", lambda e: e.tensor_scalar(t1[:], t1[:], 1e-12, None, ALU.max), ["rp_t1"], ["rp_t1"])
                    P.add("dve", lambda e: e.reciprocal(t1[:], t1[:]), ["rp_t1"], ["rp_t1"])
                    P.add("dve", lambda e: e.tensor_tensor(kk[:], kk[:], t1[:], ALU.mult), ["rp_kk", "rp_t1"], ["rp_kk"])
                    P.add("dve", lambda e, fc=fc: e.tensor_scalar(km[:], a[:], -1.0, col[:, fc, 6:7], ALU.add, ALU.mult), ["rp_a", "rp_col"], ["rp_km"])
                    P.add("dve", lambda e: e.scalar_tensor_tensor(km[:], km[:], 1.0, k[:], ALU.add, ALU.mult), ["rp_km", "rp_k"], ["rp_km"])
                    P.add("pool", lambda e: e.tensor_tensor(be[:], kk[:], a[:], ALU.mult), ["rp_kk", "rp_a"], ["rp_be"])
                    P.add("dve", lambda e: e.tensor_tensor_scan(c[:], rmask[:], ld[:], 0.0, ALU.mult, ALU.add), ["rp_rmask", "rp_ld"], ["rp_c"])
                    P.add("act", lambda e: e.activation(ep[:], c[:], AF.Exp), ["rp_c"], ["rp_ep"])
                    P.add("act", lambda e: e.activation(en[:], c[:], AF.Exp, scale=-1.0), ["rp_c"], ["rp_en"])
                    P.add("pool", lambda e: e.tensor_tensor(t2[:], c[:], ld[:], ALU.subtract), ["rp_c", "rp_ld"], ["rp_t2"])
                    P.add("act", lambda e: e.activation(ex[:], t2[:], AF.Exp), ["rp_t2"], ["rp_ex"])
                    P.add("dve", lambda e: e.tensor_tensor(o_rt[:], r[:], ep[:], ALU.mult), ["rp_r", "rp_ep"], ["rp_ort"])
                    P.add("pool", lambda e: e.tensor_tensor(o_kp[:], kk[:], ex[:], ALU.mult), ["rp_kk", "rp_ex"], ["rp_okp"])
                    P.add("dve", lambda e: e.tensor_tensor(o_kt[:], km[:], en[:], ALU.mult), ["rp_km", "rp_en"], ["rp_okt"])
                    P.add("pool", lambda e: e.tensor_tensor(o_bt[:], be[:], en[:], ALU.mult), ["rp_be", "rp_en"], ["rp_obt"])
                    P.add("dve", lambda e: e.tensor_copy(o_pc[:], ep[:].rearrange("p (n c) -> p n c", c=CH)[:, :, CH - 1]), ["rp_ep"], ["rp_opc"])
                    pcb = o_pc[:].unsqueeze(2).to_broadcast([128, NCW, CH])
                    P.add("dve", lambda e, pcb=pcb: e.tensor_tensor(o_ktl[:].rearrange("p (n c) -> p n c", c=CH),
                                                                     o_kt[:].rearrange("p (n c) -> p n c", c=CH), pcb, ALU.mult),
                          ["rp_okt", "rp_opc"], ["rp_oktl"])
                    P.add("dve", lambda e, pcb=pcb: e.scalar_tensor_tensor(o_btl[:].rearrange("p (n c) -> p n c", c=CH),
                                                                            o_bt[:].rearrange("p (n c) -> p n c", c=CH), -1.0, pcb,
                                                                            ALU.mult, ALU.mult),
                          ["rp_obt", "rp_opc"], ["rp_obtl"])
                    P.add("dve", lambda e, fc=fc: e.scalar_tensor_tensor(t1[:], r[:], col[:, fc, 7:8], km[:], ALU.mult, ALU.mult),
                          ["rp_r", "rp_km", "rp_col", "rp_t1"], ["rp_t1"])
                    P.mm(ps5[:, 0:TW], blk1[:], t1[:], True, True, ["rp_blk1", "rp_t1"], ["rp_ps5"])
                    P.add("dve", lambda e: e.tensor_tensor(o_bon[:], ps5[:, 0:TW], v[:], ALU.mult), ["rp_ps5", "rp_v"], ["rp_obon"])
                    for sub in range(TW // 128):
                        P.tr(ps6[:, sub * 128:(sub + 1) * 128], v[:, sub * 128:(sub + 1) * 128], ident[:], ["rp_v", "rp_id"], ["rp_ps6"])
                    P.add("act", lambda e: e.activation(vtk[:], ps6[:].rearrange("p (s f) -> p s f", f=128), AF.Copy), ["rp_ps6"], ["rp_vtk"])
                    P.dma("pool", S["vtok"][n0:n0 + TW, fs].rearrange("(s p) f -> p s f", p=128), vtk[:], ["rp_vtk"], [], "rp_stv")
                    for key, tl, dd in (("rp_ort", o_rt, "rt"), ("rp_okp", o_kp, "kp"), ("rp_okt", o_kt, "kt"), ("rp_obt", o_bt, "bt"),
                                        ("rp_oktl", o_ktl, "ktl"), ("rp_obtl", o_btl, "btl"), ("rp_obon", o_bon, "bon")):
                        P.dma("sp", S[dd][fs, n0:n0 + TW], tl[:], [key], [], "st" + key)
                    P.dma("sp", S["pc"][fs, n0 // CH:n0 // CH + NCW], o_pc[:], ["rp_opc"], [], "strp_opc")
    P.barrier()


def chunk_scan(P, nc, NB, T, D, delta, DK, DV, H, yT_d, yrow0, tag, gn_eps=0.0):
    TW = 256
    NCW = TW // CH
    NW = T // TW
    HV = H * DV
    names = ["rt", "kt", "ktl", "g"] + (["kp", "bt", "btl", "bon"] if delta else [])
    K = lambda s: tag + s
    with ExitStack() as es:
        A = {n: SB(es, nc, tag + "A" + n, [DK if n not in ("g", "bon") else DV, H, TW]) for n in names}
        vt = SB(es, nc, tag + "vt", [CH, NCW, HV])
        pcw = SB(es, nc, tag + "pcw", [DK, H, NCW])
        Hs = SB(es, nc, tag + "H", [DK, H, DV])
        masks = SB(es, nc, tag + "masks", [64, 320]); id64 = SB(es, nc, tag + "id64", [64, 64])
        ident = SB(es, nc, tag + "ident", [128, 128])
        am = SB(es, nc, tag + "am", [64, H, 320 if delta else 64])
        ktok = SB(es, nc, tag + "ktok", [64, H, 2 if delta else 1, DK])
        yw = SB(es, nc, tag + "yw", [DV, H, TW])
        on = SB(es, nc, tag + "on", [64, H, DV]); sq = SB(es, nc, tag + "sq", [64, H, DV]); oc = SB(es, nc, tag + "oc", [64, H, DV])
        s12 = SB(es, nc, tag + "s12", [64, 4, H])
        if delta:
            R = [SB(es, nc, tag + "R%d" % i, [64, H, 64]) for i in range(2)]
            pw = [SB(es, nc, tag + "pw%d" % i, [64, H, 2, 64]) for i in range(2)]
            rhs = SB(es, nc, tag + "rhs", [64, H, DV]); us = SB(es, nc, tag + "us", [64, H, DV])
            gng = SB(es, nc, tag + "gng", [DV, H]); gnb = SB(es, nc, tag + "gnb", [DV, H])
        else:
            normg = SB(es, nc, tag + "normg", [DV, 1])
        nbO = (H * DV * 4 + 2047) // 2048
        psA = [PS(es, nc, tag + "psA%d" % i, [128, 512]) for i in range(2)]
        psB = [PS(es, nc, tag + "psB%d" % i, [128, 512]) for i in range(2)]
        psC = PS(es, nc, tag + "psC", [128, 512])
        psD = PS(es, nc, tag + "psD", [128, 512])
        psO = PS(es, nc, tag + "psO", [128, 512])
        psT = PS(es, nc, tag + "psT", [128, 512])
        P.dma("pool", masks[:], D["masks"], [], [K("masks")], K("c0"))
        P.dma("pool", id64[:], D["id64"], [], [K("id64")], K("c0"))
        P.dma("pool", ident[:], D["ident"], [], [K("ident")], K("c0"))
        if delta:
            P.dma("pool", gng[:], D["gng"].rearrange("(h p) -> p h", p=DV), [], [K("gn")], K("c0"), allow_slow_non_contiguous=True)
            P.dma("pool", gnb[:], D["gnb"].rearrange("(h p) -> p h", p=DV), [], [K("gn")], K("c0"), allow_slow_non_contiguous=True)
        else:
            P.dma("pool", normg[:], D["normg"].rearrange("(p o) -> p o", o=1), [], [K("gn")], K("c0"))

        def psO_h(h):
            if DV == 64:
                return psO[0:64, h * 64:(h + 1) * 64]
            return (psB[h // 4])[0:64, (h % 4) * 128:(h % 4 + 1) * 128]

        def psH_h(h):
            if DV == 64:
                return psD[0:DK, h * 64:(h + 1) * 64]
            return (psC if h < 4 else psD)[0:DK, (h % 4) * 128:(h % 4 + 1) * 128]
        kO = [K("psO")] if DV == 64 else [K("psB0"), K("psB1")]
        kH = [K("psD")] if DV == 64 else [K("psC"), K("psD")]

        for b in range(NB):
            P.add("dve", lambda e: e.memset(Hs[:], 0.0), [], [K("H")])
            for w in range(NW):
                n0 = b * T + w * TW
                for i, n in enumerate(names):
                    p_ = DK if n not in ("g", "bon") else DV
                    P.dma("sp" if i % 2 == 0 else "act", A[n][:], D[n][:, n0:n0 + TW].rearrange("(h p) n -> p h n", p=p_),
                          [], [K("A" + n)], K("A" + n))
                P.dma("sp", vt[:], D["vtok"][n0:n0 + TW, :].rearrange("(c s) f -> s c f", s=CH), [], [K("vt")], K("vt"))
                P.dma("sp", pcw[:], D["pc"][:, n0 // CH:n0 // CH + NCW].rearrange("(h p) c -> p h c", p=DK), [], [K("pcw")], K("pcw"))
                for n in range(NCW):
                    cs = slice(n * CH, (n + 1) * CH)
                    for h in range(H):
                        pa = psA[h % 2]
                        kpa = K("psA%d" % (h % 2))
                        if delta:
                            pairs = (("bt", "kp"), ("kp", "bt"), ("kt", "kp"), ("kt", "rt"), ("bt", "rt"))
                        else:
                            pairs = (("kt", "rt"),)
                        for q, (l, r_) in enumerate(pairs):
                            P.mm(pa[0:64, q * 64:(q + 1) * 64], A[l][:, h, cs], A[r_][:, h, cs], True, True,
                                 [K("A" + l), K("A" + r_)], [kpa])
                        if delta:
                            P.add("dve", lambda e, h=h, pa=pa: e.tensor_tensor(am[:, h, :], pa[0:64, 0:320], masks[:], ALU.mult),
                                  [kpa, K("masks")], [K("am")])
                        else:
                            P.add("dve", lambda e, h=h, pa=pa: e.tensor_tensor(am[:, h, :], pa[0:64, 0:64], masks[:, 192:256], ALU.mult),
                                  [kpa, K("masks")], [K("am")])
                    if STAGE < 2: continue
                    nk = 2 if delta else 1
                    for h in range(H):
                        for q, nm in enumerate(("ktl", "btl")[:nk]):
                            if DK == 64:
                                dst = psA[(h * nk + q) // 8][0:64, ((h * nk + q) % 8) * 64:((h * nk + q) % 8 + 1) * 64]
                                kd = K("psA%d" % ((h * nk + q) // 8))
                            else:
                                dst = psA[h // 4][0:64, (h % 4) * 128:(h % 4 + 1) * 128]
                                kd = K("psA%d" % (h // 4))
                            P.tr(dst, A[nm][:, h, cs], ident[0:DK, 0:DK], [K("A" + nm), K("ident"), K("am")], [kd])
                    for half in range(2):
                        hs = slice(half * (H // 2), (half + 1) * (H // 2))
                        src = psA[half][0:64, 0:(H // 2) * nk * DK].rearrange("p (h q d) -> p h q d", q=nk, d=DK)
                        P.add("act", lambda e, hs=hs, src=src: e.activation(ktok[:, hs, :, :], src, AF.Copy),
                              [K("psA%d" % half)], [K("ktok")])
                    if STAGE < 3: continue
                    if delta:
                        P.add("dve", lambda e: e.tensor_tensor(R[0][:], am[:, :, 0:64], id64[:].unsqueeze(1).to_broadcast([64, H, 64]), ALU.add),
                              [K("am"), K("id64")], [K("R0")])
                        cur = 0
                        for j in range(1, 6):
                            src_p = (lambda h: am[:, h, 0:64]) if j == 1 else (lambda h, pwp=pw[(j - 1) % 2]: pwp[:, h, 0, :])
                            src_pt = (lambda h: am[:, h, 64:128]) if j == 1 else (lambda h, pwp=pw[(j - 1) % 2]: pwp[:, h, 1, :])
                            ksrc = K("am") if j == 1 else K("pw%d" % ((j - 1) % 2))
                            for h in range(H):
                                pb = psB[h // 4]
                                o0 = (h % 4) * 128
                                if j < 5:
                                    P.mm(pb[0:64, o0:o0 + 64], src_pt(h), src_p(h), True, True, [ksrc], [K("psB%d" % (h // 4))])
                                P.mm(pb[0:64, o0 + 64:o0 + 128], src_p(h), src_pt(h), True, True, [ksrc], [K("psB%d" % (h // 4))])
                            for half in range(2):
                                hs = slice(half * 4, half * 4 + 4)
                                eng = "act" if half == 0 else "dve"
                                srcv = psB[half][0:64, :].rearrange("p (h q d) -> p h q d", q=2, d=64)
                                if j < 5:
                                    if eng == "act":
                                        P.add("act", lambda e, hs=hs, srcv=srcv, j=j: e.activation(pw[j % 2][:, hs, :, :], srcv, AF.Copy),
                                              [K("psB%d" % half)], [K("pw%d" % (j % 2))])
                                    else:
                                        P.add("dve", lambda e, hs=hs, srcv=srcv, j=j: e.tensor_copy(pw[j % 2][:, hs, :, :], srcv),
                                              [K("psB%d" % half)], [K("pw%d" % (j % 2))])
                                else:
                                    if eng == "act":
                                        P.add("act", lambda e, hs=hs, srcv=srcv, j=j: e.activation(pw[j % 2][:, hs, 1, :], srcv[:, :, 1, :], AF.Copy),
                                              [K("psB%d" % half)], [K("pw%d" % (j % 2))])
                                    else:
                                        P.add("dve", lambda e, hs=hs, srcv=srcv, j=j: e.tensor_copy(pw[j % 2][:, hs, 1, :], srcv[:, :, 1, :]),
                                              [K("psB%d" % half)], [K("pw%d" % (j % 2))])
                            for h in range(H):
                                P.mm(psC[0:64, h * 64:(h + 1) * 64], pw[j % 2][:, h, 1, :], R[cur][:, h, :], True, True,
                                     [K("pw%d" % (j % 2)), K("R%d" % cur)], [K("psC")])
                            P.add("dve", lambda e, cur=cur: e.tensor_tensor(R[1 - cur][:], R[cur][:], psC[0:64, :].rearrange("p (h d) -> p h d", d=64), ALU.add),
                                  [K("R%d" % cur), K("psC")], [K("R%d" % (1 - cur))])
                            cur = 1 - cur
                        if STAGE < 4: continue
                        for h in range(H):
                            P.mm(psD[0:64, h * 64:(h + 1) * 64], A["kp"][:, h, cs], Hs[:, h, :], True, False, [K("Akp"), K("H")], [K("psD")])
                            P.mm(psD[0:64, h * 64:(h + 1) * 64], am[:, h, 128:192], vt[:, n, h * DV:(h + 1) * DV], False, True,
                                 [K("am"), K("vt")], [K("psD")])
                        P.add("act", lambda e: e.activation(rhs[:], psD[0:64, :].rearrange("p (h d) -> p h d", d=64), AF.Copy), [K("psD")], [K("rhs")])
                        for h in range(H):
                            P.mm(psC[0:64, h * 64:(h + 1) * 64], R[cur][:, h, :], rhs[:, h, :], True, True, [K("R%d" % cur), K("rhs")], [K("psC")])
                        P.add("dve", lambda e: e.tensor_copy(us[:], psC[0:64, :].rearrange("p (h d) -> p h d", d=64)), [K("psC")], [K("us")])
                    if STAGE < 5: continue
                    for h in range(H):
                        a3 = am[:, h, 192:256] if delta else am[:, h, :]
                        P.mm(psO_h(h), A["rt"][:, h, cs], Hs[:, h, :], True, False, [K("Art"), K("H")], kO)
                        P.mm(psO_h(h), a3, vt[:, n, h * DV:(h + 1) * DV], False, not delta, [K("am"), K("vt")], kO)
                        if delta:
                            P.mm(psO_h(h), am[:, h, 256:320], us[:, h, :], False, True, [K("am"), K("us")], kO)
                    for h in range(H):
                        P.mm(psH_h(h), ktok[:, h, 0, :], vt[:, n, h * DV:(h + 1) * DV], True, not delta, [K("ktok"), K("vt")], kH)
                        if delta:
                            P.mm(psH_h(h), ktok[:, h, 1, :], us[:, h, :], False, True, [K("ktok"), K("us")], kH)
                    if STAGE < 6: continue
                    if DV == 64:
                        o3p = psO[0:64, :].rearrange("p (h d) -> p h d", d=64)
                        P.add("act", lambda e, o3p=o3p: e.activation(oc[:], o3p, AF.Copy), kO, [K("oc")])
                        o3 = oc[:]
                        kO_ = [K("oc")]
                        P.add("dve", lambda e, o3=o3: e.reduce_sum(s12[:, 0, :], o3, AX.X), kO_, [K("s12")])
                        P.add("act", lambda e, o3=o3: e.activation(sq[:], o3, AF.Square), kO_, [K("sq")])
                        P.add("dve", lambda e: e.reduce_sum(s12[:, 1, :], sq[:], AX.X), [K("sq")], [K("s12")])
                        P.add("dve", lambda e: e.tensor_scalar(s12[:, 0, :], s12[:, 0, :], 1.0 / DV, None, ALU.mult), [K("s12")], [K("s12")])
                        P.add("dve", lambda e: e.tensor_tensor(s12[:, 2, :], s12[:, 0, :], s12[:, 0, :], ALU.mult), [K("s12")], [K("s12")])
                        P.add("dve", lambda e: e.scalar_tensor_tensor(s12[:, 1, :], s12[:, 1, :], 1.0 / DV, s12[:, 2, :], ALU.mult, ALU.subtract),
                              [K("s12")], [K("s12")])
                        P.add("act", lambda e: e.activation(s12[:, 1, :], s12[:, 1, :], AF.Sqrt, bias=gn_eps, scale=1.0), [K("s12")], [K("s12")])
                        P.add("dve", lambda e: e.reciprocal(s12[:, 1, :], s12[:, 1, :]), [K("s12")], [K("s12")])
                        P.add("dve", lambda e, o3=o3: e.tensor_tensor(on[:], o3, s12[:, 0, :].unsqueeze(2).to_broadcast([64, H, DV]), ALU.subtract),
                              kO_ + [K("s12")], [K("on")])
                        P.add("dve", lambda e: e.tensor_tensor(on[:], on[:], s12[:, 1, :].unsqueeze(2).to_broadcast([64, H, DV]), ALU.mult),
                              [K("on"), K("s12")], [K("on")])
                    else:
                        for half in range(2):
                            o3 = psB[half][0:64, :].rearrange("p (h d) -> p h d", d=128)
                            hs = slice(half * 4, half * 4 + 4)
                            P.add("act", lambda e, o3=o3, hs=hs: e.activation(sq[:, hs, :], o3, AF.Square), [K("psB%d" % half)], [K("sq")])
                        P.add("dve", lambda e: e.reduce_sum(s12[:, 1, :], sq[:], AX.X), [K("sq")], [K("s12")])
                        P.add("act", lambda e: e.activation(s12[:, 1, :], s12[:, 1, :], AF.Sqrt, bias=gn_eps, scale=1.0 / DV), [K("s12")], [K("s12")])
                        P.add("dve", lambda e: e.reciprocal(s12[:, 1, :], s12[:, 1, :]), [K("s12")], [K("s12")])
                        for half in range(2):
                            o3 = psB[half][0:64, :].rearrange("p (h d) -> p h d", d=128)
                            hs = slice(half * 4, half * 4 + 4)
                            P.add("dve", lambda e, o3=o3, hs=hs: e.tensor_tensor(on[:, hs, :], o3, s12[:, 1, hs].unsqueeze(2).to_broadcast([64, 4, DV]), ALU.mult),
                                  [K("psB%d" % half), K("s12")], [K("on")])
                    if DV == 64:
                        h3 = psD[0:DK, :].rearrange("p (h d) -> p h d", d=64)
                        P.add("dve", lambda e, n=n: e.tensor_tensor(Hs[:], Hs[:], pcw[:, :, n:n + 1].to_broadcast([DK, H, DV]), ALU.mult),
                              [K("H"), K("pcw")], [K("H")])
                        P.add("dve", lambda e, h3=h3: e.tensor_tensor(Hs[:], Hs[:], h3, ALU.add), [K("H")] + kH, [K("H")])
                    else:
                        P.add("dve", lambda e, n=n: e.tensor_tensor(Hs[:], Hs[:], pcw[:, :, n:n + 1].to_broadcast([DK, H, DV]), ALU.mult),
                              [K("H"), K("pcw")], [K("H")])
                        for half, pp in enumerate((psC, psD)):
                            hs = slice(half * 4, half * 4 + 4)
                            h3 = pp[0:DK, :].rearrange("p (h d) -> p h d", d=128)
                            P.add("dve", lambda e, h3=h3, hs=hs: e.tensor_tensor(Hs[:, hs, :], Hs[:, hs, :], h3, ALU.add),
                                  [K("H"), kH[half]], [K("H")])
                    if STAGE < 7: continue
                    for h in range(H):
                        P.tr(psT[0:DV, h * 64:(h + 1) * 64], on[:, h, :], ident[0:64, 0:64], [K("on"), K("ident")], [K("psT")])
                    t3 = psT[0:DV, :].rearrange("p (h t) -> p h t", t=64)
                    ywc = yw[:, :, cs]
                    if delta:
                        P.add("dve", lambda e, t3=t3, ywc=ywc: e.tensor_tensor(ywc, t3, gng[:].unsqueeze(2).to_broadcast([DV, H, 64]), ALU.mult),
                              [K("psT"), K("gn")], [K("yw")])
                        P.add("pool", lambda e, ywc=ywc: e.tensor_tensor(ywc, ywc, gnb[:].unsqueeze(2).to_broadcast([DV, H, 64]), ALU.add),
                              [K("yw"), K("gn")], [K("yw")])
                        P.add("pool", lambda e, ywc=ywc, cs=cs: e.tensor_tensor(ywc, ywc, A["bon"][:, :, cs], ALU.add), [K("yw"), K("Abon")], [K("yw")])
                        P.add("pool", lambda e, ywc=ywc, cs=cs: e.tensor_tensor(ywc, ywc, A["g"][:, :, cs], ALU.mult), [K("yw"), K("Ag")], [K("yw")])
                    else:
                        P.add("dve", lambda e, t3=t3, ywc=ywc: e.tensor_scalar(ywc, t3, normg[:, 0:1], None, ALU.mult), [K("psT"), K("gn")], [K("yw")])
                        P.add("pool", lambda e, ywc=ywc, cs=cs: e.tensor_tensor(ywc, ywc, A["g"][:, :, cs], ALU.mult), [K("yw"), K("Ag")], [K("yw")])
                P.dma("pool", yT_d[yrow0:yrow0 + HV, n0:n0 + TW].rearrange("(h p) n -> p h n", p=DV), yw[:], [K("yw")], [], K("styw"))
    P.barrier()


def hgrn_prep(P, nc, NB, T, zT_d, prm, S):
    TW = 512
    NW = T // TW
    NCW = TW // CH
    with ExitStack() as es:
        lbc = SB(es, nc, "hp_lbc", [128, 4, 8])
        nl = SB(es, nc, "hp_nl", [128, 8])
        ident = SB(es, nc, "hp_id", [128, 128]); rmask = SB(es, nc, "hp_rmask", [128, TW])
        q = SB(es, nc, "hp_q", [128, TW]); f = SB(es, nc, "hp_f", [128, TW]); iv = SB(es, nc, "hp_i", [128, TW]); g = SB(es, nc, "hp_g", [128, TW])
        k = SB(es, nc, "hp_k", [128, TW]); c = SB(es, nc, "hp_c", [128, TW]); ep = SB(es, nc, "hp_ep", [128, TW]); en = SB(es, nc, "hp_en", [128, TW])
        o_rt = SB(es, nc, "hp_ort", [128, TW]); o_kt = SB(es, nc, "hp_okt", [128, TW]); o_ktl = SB(es, nc, "hp_oktl", [128, TW])
        o_pc = SB(es, nc, "hp_opc", [128, NCW]); vtk = SB(es, nc, "hp_vtk", [128, 4, 128])
        ps6 = PS(es, nc, "hp_ps6", [128, 512])
        P.dma("pool", lbc[:, 0, :], prm["lb2"][0].rearrange("(h p) -> p h", p=128), [], ["hp_lbc"], "hp_c0", allow_slow_non_contiguous=True)
        P.dma("pool", lbc[:, 1, :], prm["lb2"][1].rearrange("(h p) -> p h", p=128), [], ["hp_lbc"], "hp_c0", allow_slow_non_contiguous=True)
        P.dma("pool", ident[:], prm["ident"], [], ["hp_id"], "hp_c0")
        P.dma("pool", rmask[:], prm["rmask"].partition_broadcast(128), [], ["hp_rmask"], "hp_c0")
        P.add("dve", lambda e: e.tensor_tensor(lbc[:, 2, :], lbc[:, 1, :], lbc[:, 0, :], ALU.subtract), ["hp_lbc"], ["hp_lbc"])
        P.add("act", lambda e: e.activation(lbc[:, 2, :], lbc[:, 2, :], AF.Sigmoid), ["hp_lbc"], ["hp_lbc"])
        P.add("dve", lambda e: e.tensor_scalar(lbc[:, 3, :], lbc[:, 2, :], -1.0, 1.0, ALU.mult, ALU.add), ["hp_lbc"], ["hp_lbc"])
        P.add("dve", lambda e: e.tensor_scalar(nl[:], lbc[:, 3, :], -1.0, None, ALU.mult), ["hp_lbc"], ["hp_nl"])
        for b in range(NB):
            for w in range(NW):
                n0 = b * T + w * TW
                for h in range(8):
                    fs = slice(h * 128, (h + 1) * 128)
                    for j, (dst, key) in enumerate(((q, "hp_q"), (f, "hp_f"), (iv, "hp_i"), (g, "hp_g"))):
                        P.dma("sp" if j % 2 == 0 else "act", dst[:], zT_d[j * 1024 + h * 128:j * 1024 + (h + 1) * 128, n0:n0 + TW], [], [key], key)
                    P.add("act", lambda e: e.activation(f[:], f[:], AF.Sigmoid), ["hp_f"], ["hp_f"])
                    P.add("dve", lambda e, h=h: e.tensor_scalar(k[:], f[:], nl[:, h:h + 1], lbc[:, 3, h:h + 1], ALU.mult, ALU.add),
                          ["hp_f", "hp_nl", "hp_lbc"], ["hp_k"])
                    P.add("dve", lambda e, h=h: e.tensor_scalar(f[:], f[:], lbc[:, 3, h:h + 1], lbc[:, 2, h:h + 1], ALU.mult, ALU.add),
                          ["hp_f", "hp_lbc", "hp_k"], ["hp_f"])
                    P.add("act", lambda e: e.activation(f[:], f[:], AF.Ln), ["hp_f"], ["hp_f"])
                    P.add("act", lambda e: e.activation(q[:], q[:], AF.Silu), ["hp_q"], ["hp_q"])
                    P.add("act", lambda e: e.activation(g[:], g[:], AF.Silu), ["hp_g"], ["hp_g"])
                    P.dma("pool", S["g"][fs, n0:n0 + TW], g[:], ["hp_g"], [], "hp_stg")
                    P.add("dve", lambda e: e.tensor_tensor_scan(c[:], rmask[:], f[:], 0.0, ALU.mult, ALU.add), ["hp_rmask", "hp_f"], ["hp_c"])
                    P.add("act", lambda e: e.activation(ep[:], c[:], AF.Exp), ["hp_c"], ["hp_ep"])
                    P.add("act", lambda e: e.activation(en[:], c[:], AF.Exp, scale=-1.0), ["hp_c"], ["hp_en"])
                    P.add("dve", lambda e: e.tensor_tensor(o_rt[:], q[:], ep[:], ALU.mult), ["hp_q", "hp_ep"], ["hp_ort"])
                    P.add("pool", lambda e: e.tensor_tensor(o_kt[:], k[:], en[:], ALU.mult), ["hp_k", "hp_en"], ["hp_okt"])
                    P.add("dve", lambda e: e.tensor_copy(o_pc[:], ep[:].rearrange("p (n c) -> p n c", c=CH)[:, :, CH - 1]), ["hp_ep"], ["hp_opc"])
                    pcb = o_pc[:].unsqueeze(2).to_broadcast([128, NCW, CH])
                    P.add("dve", lambda e, pcb=pcb: e.tensor_tensor(o_ktl[:].rearrange("p (n c) -> p n c", c=CH),
                                                                     o_kt[:].rearrange("p (n c) -> p n c", c=CH), pcb, ALU.mult),
                          ["hp_okt", "hp_opc"], ["hp_oktl"])
                    for sub in range(TW // 128):
                        P.tr(ps6[:, sub * 128:(sub + 1) * 128], iv[:, sub * 128:(sub + 1) * 128], ident[:], ["hp_i", "hp_id"], ["hp_ps6"])
                    P.add("act", lambda e: e.activation(vtk[:], ps6[:].rearrange("p (s f) -> p s f", f=128), AF.Copy), ["hp_ps6"], ["hp_vtk"])
                    P.dma("pool", S["vtok"][n0:n0 + TW, fs].rearrange("(s p) f -> p s f", p=128), vtk[:], ["hp_vtk"], [], "hp_stv")
                    for key, tl, dd in (("hp_ort", o_rt, "rt"), ("hp_okt", o_kt, "kt"), ("hp_oktl", o_ktl, "ktl")):
                        P.dma("sp", S[dd][fs, n0:n0 + TW], tl[:], [key], [], "st" + key)
                    P.dma("sp", S["pc"][fs, n0 // CH:n0 // CH + NCW], o_pc[:], ["hp_opc"], [], "sthp_opc")
    P.barrier()


R0 = 1792
NEG = -30000.0
PI = math.pi


def nsa_prep(P, nc, NB, T, zT_d, pos_d, cst, S):
    TW = 512
    NW = T // TW
    with ExitStack() as es:
        invf = SB(es, nc, "np_invf", [128, 1]); rot = SB(es, nc, "np_rot", [128, 128]); ident = SB(es, nc, "np_id", [128, 128])
        posi = SB(es, nc, "np_posi", [128, TW], I32); ang = SB(es, nc, "np_ang", [128, TW])
        tt = SB(es, nc, "np_tt", [128, TW]); ki = SB(es, nc, "np_ki", [128, TW], I32); kf = SB(es, nc, "np_kf", [128, TW])
        sinF = SB(es, nc, "np_sin", [128, TW]); cosF = SB(es, nc, "np_cos", [128, TW])
        x = SB(es, nc, "np_x", [128, TW]); xr = SB(es, nc, "np_xr", [128, TW]); xq = SB(es, nc, "np_xq", [128, TW]); t2 = SB(es, nc, "np_t2", [128, TW])
        vtk = SB(es, nc, "np_vtk", [128, 4, 128]); gl = SB(es, nc, "np_gl", [24, TW]); gtk = SB(es, nc, "np_gtk", [128, 4, 24])
        ps1 = PS(es, nc, "np_ps1", [128, 512]); ps2 = PS(es, nc, "np_ps2", [128, 512]); ps3 = PS(es, nc, "np_ps3", [128, 512])
        P.dma("pool", invf[:], cst["invf"], [], ["np_c"], "np_c0")
        P.dma("pool", rot[:], cst["rot"], [], ["np_c"], "np_c0")
        P.dma("pool", ident[:], cst["ident"], [], ["np_c"], "np_c0")

        def reduce_sin(dst, shift, kd):
            P.add("dve", lambda e: e.tensor_scalar(tt[:], ang[:], shift, 1.0 / (2 * PI), ALU.add, ALU.mult), ["np_ang"], ["np_tt"])
            P.add("dve", lambda e: e.tensor_copy(ki[:], tt[:]), ["np_tt"], ["np_ki"])
            P.add("dve", lambda e: e.tensor_copy(kf[:], ki[:]), ["np_ki"], ["np_kf"])
            P.add("dve", lambda e: e.tensor_scalar(tt[:], ang[:], shift, None, ALU.add), ["np_ang", "np_ki"], ["np_tt"])
            P.add("dve", lambda e: e.scalar_tensor_tensor(tt[:], kf[:], -2 * PI, tt[:], ALU.mult, ALU.add), ["np_kf", "np_tt"], ["np_tt"])
            P.add("dve", lambda e: e.tensor_scalar(kf[:], tt[:], PI, -2 * PI, ALU.is_gt, ALU.mult), ["np_tt"], ["np_kf"])
            P.add("dve", lambda e: e.tensor_tensor(tt[:], tt[:], kf[:], ALU.add), ["np_tt", "np_kf"], ["np_tt"])
            P.add("dve", lambda e: e.tensor_scalar(kf[:], tt[:], -PI, 2 * PI, ALU.is_lt, ALU.mult), ["np_tt"], ["np_kf"])
            P.add("dve", lambda e: e.tensor_tensor(tt[:], tt[:], kf[:], ALU.add), ["np_tt", "np_kf"], ["np_tt"])
            P.add("dve", lambda e: e.tensor_scalar(tt[:], tt[:], PI, -PI, ALU.min, ALU.max), ["np_tt"], ["np_tt"])
            P.add("act", lambda e: e.activation(dst[:], tt[:], AF.Sin), ["np_tt"], [kd])

        for b in range(NB):
            for w in range(NW):
                t0 = w * TW
                n0 = b * T + t0
                P.dma("sp", posi[:], pos_d[b, t0:t0 + TW].partition_broadcast(128), [], ["np_posi"], "np_posi")
                P.add("dve", lambda e: e.tensor_copy(ang[:], posi[:]), ["np_posi"], ["np_ang"])
                P.add("dve", lambda e: e.tensor_scalar(ang[:], ang[:], invf[:, 0:1], None, ALU.mult), ["np_ang", "np_c"], ["np_ang"])
                reduce_sin(sinF, 0.0, "np_sin")
                reduce_sin(cosF, PI / 2, "np_cos")
                for ti, (row, dname, drow, sc, raw) in enumerate(
                        [(128 * i, "qrot", 128 * i, 0.125, True) for i in range(4)] + [(768, "ksr", 0, 1.0, False), (1024, "kwr", 0, 1.0, False)]):
                    P.dma("sp", x[:], zT_d[R0 + row:R0 + row + 128, n0:n0 + TW], [], ["np_x"], "np_x")
                    P.mm(ps1[:, 0:TW], rot[:], x[:], True, True, ["np_c", "np_x"], ["np_ps1"])
                    P.add("dve", lambda e: e.tensor_tensor(t2[:], ps1[:, 0:TW], sinF[:], ALU.mult), ["np_ps1", "np_sin"], ["np_t2"])
                    P.add("pool", lambda e: e.tensor_tensor(xr[:], x[:], cosF[:], ALU.mult), ["np_x", "np_cos"], ["np_xr"])
                    P.add("dve", lambda e, sc=sc: e.scalar_tensor_tensor(xr[:], xr[:], 1.0, t2[:], ALU.mult, ALU.add), ["np_xr", "np_t2"], ["np_xr"])
                    if sc != 1.0:
                        P.add("pool", lambda e, sc=sc: e.tensor_scalar(xr[:], xr[:], sc, None, ALU.mult), ["np_xr"], ["np_xr"])
                    P.dma("pool", S[dname][drow:drow + 128, n0:n0 + TW], xr[:], ["np_xr"], [], "np_stxr")
                    if raw:
                        P.add("act", lambda e, sc=sc: e.activation(xq[:], x[:], AF.Copy, scale=sc), ["np_x"], ["np_xq"])
                        P.dma("pool", S["qraw"][drow:drow + 128, n0:n0 + TW], xq[:], ["np_xq"], [], "np_stxq")
                for row, dname in ((896, "vs_tok"), (1152, "vw_tok")):
                    P.dma("sp", x[:], zT_d[R0 + row:R0 + row + 128, n0:n0 + TW], [], ["np_x"], "np_x")
                    for sub in range(4):
                        P.tr(ps2[:, sub * 128:(sub + 1) * 128], x[:, sub * 128:(sub + 1) * 128], ident[:], ["np_x", "np_c"], ["np_ps2"])
                    P.add("act", lambda e: e.activation(vtk[:], ps2[:].rearrange("p (s f) -> p s f", f=128), AF.Copy), ["np_ps2"], ["np_vtk"])
                    P.dma("pool", S[dname][n0:n0 + TW, :].rearrange("(s p) f -> p s f", p=128), vtk[:], ["np_vtk"], [], "np_stv")
                P.dma("sp", gl[:], zT_d[R0 + 1280:R0 + 1304, n0:n0 + TW], [], ["np_gl"], "np_gl")
                P.add("act", lambda e: e.activation(gl[:], gl[:], AF.Sigmoid), ["np_gl"], ["np_gl"])
                for sub in range(4):
                    P.tr(ps3[:, sub * 24:(sub + 1) * 24], gl[:, sub * 128:(sub + 1) * 128], ident[0:24, 0:24], ["np_gl", "np_c"], ["np_ps3"])
                P.add("act", lambda e: e.activation(gtk[:], ps3[:, 0:96].rearrange("p (s f) -> p s f", f=24), AF.Copy), ["np_ps3"], ["np_gtk"])
                P.dma("pool", S["gat_tok"][n0:n0 + TW, :].rearrange("(s p) f -> p s f", p=128), gtk[:], ["np_gtk"], [], "np_stg")
    P.barrier()


def nsa_cmp(P, nc, NB, T, zT_d, w1_d, w2_d, cpos_d, S):
    ncmp = (T - 32) // 16 + 1
    nch = (ncmp + 127) // 128
    C0 = math.sqrt(2.0 / math.pi)
    with ExitStack() as es:
        w1 = SB(es, nc, "nc_w1", [64, 2, 32, 128]); w2 = SB(es, nc, "nc_w2", [128, 2, 64]); cpos = SB(es, nc, "nc_cpos", [64, 2, 32])
        bias = SB(es, nc, "nc_bias", [128, 2]); kc = SB(es, nc, "nc_kc", [64, T])
        hx = SB(es, nc, "nc_hx", [128, 256]); h2 = SB(es, nc, "nc_h2", [128, 256]); h3 = SB(es, nc, "nc_h3", [128, 256])
        ko = SB(es, nc, "nc_ko", [64, 256]); vo = SB(es, nc, "nc_vo", [128, 2, 64])
        psb = PS(es, nc, "nc_psb", [128, 512]); psh = PS(es, nc, "nc_psh", [128, 512]); pso = PS(es, nc, "nc_pso", [128, 512])
        for z in range(2):
            P.dma("sp", w1[:, z, :, :], w1_d[z].rearrange("(l d) o -> d l o", d=64), [], ["nc_w1"], "nc_c0")
            P.dma("sp", w2[:, z, :], w2_d[z], [], ["nc_w2"], "nc_c0")
            P.dma("sp", cpos[:, z, :], cpos_d[z].rearrange("l d -> d l"), [], ["nc_cpos"], "nc_c0", allow_slow_non_contiguous=True)
        for z in range(2):
            for l in range(32):
                P.mm(psb[:, z:z + 1], w1[:, z, l, :], cpos[:, z, l:l + 1], l == 0, l == 31, ["nc_w1", "nc_cpos"], ["nc_psb"])
        P.add("dve", lambda e: e.tensor_copy(bias[:], psb[:, 0:2]), ["nc_psb"], ["nc_bias"])
        for b in range(NB):
            for z in range(2):
                for hk in range(2):
                    row = R0 + 512 + z * 128 + hk * 64
                    P.dma("sp", kc[:], zT_d[row:row + 64, b * T:(b + 1) * T], [], ["nc_kc"], "nc_kc")
                    for l in range(32):
                        P.mm(psh[:, 0:ncmp], w1[:, z, l, :], kc[:, l:l + 16 * (ncmp - 1) + 1:16], l == 0, l == 31, ["nc_w1", "nc_kc"], ["nc_psh"])
                    P.add("act", lambda e, z=z: e.activation(hx[:, 0:ncmp], psh[:, 0:ncmp], AF.Identity, bias=bias[:, z:z + 1], scale=1.0),
                          ["nc_psh", "nc_bias"], ["nc_hx"])
                    P.add("act", lambda e: e.activation(h2[:, 0:ncmp], hx[:, 0:ncmp], AF.Square), ["nc_hx"], ["nc_h2"])
                    P.add("dve", lambda e: e.tensor_scalar(h2[:, 0:ncmp], h2[:, 0:ncmp], 0.044715, 1.0, ALU.mult, ALU.add), ["nc_h2"], ["nc_h2"])
                    P.add("dve", lambda e: e.tensor_tensor(h2[:, 0:ncmp], h2[:, 0:ncmp], hx[:, 0:ncmp], ALU.mult), ["nc_h2", "nc_hx"], ["nc_h2"])
                    P.add("act", lambda e: e.activation(h3[:, 0:ncmp], h2[:, 0:ncmp], AF.Tanh, scale=C0), ["nc_h2"], ["nc_h3"])
                    P.add("dve", lambda e: e.tensor_scalar(h3[:, 0:ncmp], h3[:, 0:ncmp], 0.5, 0.5, ALU.mult, ALU.add), ["nc_h3"], ["nc_h3"])
                    P.add("dve", lambda e: e.tensor_tensor(h3[:, 0:ncmp], h3[:, 0:ncmp], hx[:, 0:ncmp], ALU.mult), ["nc_h3", "nc_hx"], ["nc_h3"])
                    r = (b * 2 + hk)
                    if z == 0:
                        P.mm(pso[0:64, 0:ncmp], w2[:, 0, :], h3[:, 0:ncmp], True, True, ["nc_w2", "nc_h3"], ["nc_pso"])
                        P.add("act", lambda e: e.activation(ko[:, 0:ncmp], pso[0:64, 0:ncmp], AF.Copy), ["nc_pso"], ["nc_ko"])
                        P.dma("pool", S["kcmpT"][r * 64:(r + 1) * 64, 0:ncmp], ko[:, 0:ncmp], ["nc_ko"], [], "nc_stko")
                    else:
                        for c in range(nch):
                            nn = min(128, ncmp - c * 128)
                            P.mm(pso[0:nn, c * 64:(c + 1) * 64], h3[:, c * 128:c * 128 + nn], w2[:, 1, :], True, True, ["nc_w2", "nc_h3"], ["nc_pso"])
                            P.add("act", lambda e, c=c, nn=nn: e.activation(vo[0:nn, c, :], pso[0:nn, c * 64:(c + 1) * 64], AF.Copy), ["nc_pso"], ["nc_vo"])
                            P.dma("pool", S["vcmp"][r, c * 128:c * 128 + nn, :], vo[0:nn, c, :], ["nc_vo"], [], "nc_stvo")
    P.barrier()


def nsa_attn_a(P, nc, NB, T, cst, S):
    ncmp = (T - 32) // 16 + 1
    nch = (ncmp + 127) // 128
    NT = T // 128
    NS = T // 64
    with ExitStack() as es:
        tab = SB(es, nc, "na_tab", [128, 2048 + T]); ident = SB(es, nc, "na_id", [128, 128])
        keepw = SB(es, nc, "na_keepw", [128, 2 * NS]); addw = SB(es, nc, "na_addw", [128, 2 * NS])
        kcm = SB(es, nc, "na_kcm", [64, nch * 128]); vx = SB(es, nc, "na_vx", [128, nch, 129])
        qr = SB(es, nc, "na_qr", [64, 4, T])
        ee = [SB(es, nc, "na_e%d" % i, [128, 512]) for i in range(2)]
        o1 = SB(es, nc, "na_o1", [128, 4, 65]); o2 = SB(es, nc, "na_o2", [128, 4, NS]); rden = SB(es, nc, "na_rden", [128, 4])
        oc = [SB(es, nc, "na_oc%d" % i, [128, 4, 64]) for i in range(2)]
        imp = SB(es, nc, "na_imp", [128, NS]); imp2 = SB(es, nc, "na_imp2", [128, NS]); m8 = SB(es, nc, "na_m8", [128, 16])
        selb = SB(es, nc, "na_selb", [128, NS]); sbT = SB(es, nc, "na_sbT", [64, T])
        psS = [PS(es, nc, "na_psS%d" % i, [128, 512]) for i in range(2)]
        psO1 = PS(es, nc, "na_psO1", [128, 512]); psO2 = PS(es, nc, "na_psO2", [128, 512]); psT = PS(es, nc, "na_psT", [128, 512])
        P.dma("sp", tab[:], cst["tab"], [], ["na_c"], "na_c0")
        P.dma("sp", ident[:], cst["ident"], [], ["na_c"], "na_c0")
        P.dma("sp", keepw[:], cst["keepw"], [], ["na_c"], "na_c0")
        P.dma("sp", addw[:], cst["addw"], [], ["na_c"], "na_c0")
        for c in range(nch):
            nn = min(128, ncmp - c * 128)
            P.dma("sp", vx[0:nn, c, 64:65 + NS], cst["ovx"][c * 128:c * 128 + nn, :], [], ["na_vxc"], "na_c0")
        for b in range(NB):
            for hk in range(2):
                r = b * 2 + hk
                P.dma("sp", kcm[:, 0:ncmp], S["kcmpT"][r * 64:(r + 1) * 64, 0:ncmp], [], ["na_kcm"], "na_kcm")
                for c in range(nch):
                    nn = min(128, ncmp - c * 128)
                    P.dma("sp", vx[0:nn, c, 0:64], S["vcmp"][r, c * 128:c * 128 + nn, :], [], ["na_vx"], "na_vx")
                P.dma("act", qr[:], S["qraw"][hk * 256:(hk + 1) * 256, b * T:(b + 1) * T].rearrange("(g d) n -> d g n", d=64), [], ["na_qr"], "na_qr")
                for j in range(NT):
                    chunks = [c for c in range(nch) if 16 * 128 * c + 31 <= 128 * j + 127]
                    s_ = j % 2
                    koc = "na_oc%d" % s_
                    if not chunks:
                        P.add("dve", lambda e, s_=s_: e.memset(oc[s_][:], 0.0), [], [koc])
                        P.add("dve", lambda e: e.memset(imp[:], 0.0), [], ["na_imp"])
                    else:
                        for ci, c in enumerate(chunks):
                            nn = min(128, ncmp - c * 128)
                            off = 2048 + 128 * j - 2048 * c
                            ps = psS[c % 2]
                            P.mm(ps[0:nn, :], kcm[:, c * 128:c * 128 + nn], qr[:, :, j * 128:(j + 1) * 128], True, False,
                                 ["na_kcm", "na_qr"], ["na_psS%d" % (c % 2)])
                            P.mm(ps[0:nn, :], ident[0:nn, 0:nn], tab[0:nn, off:off + 128].unsqueeze(1).to_broadcast([nn, 4, 128]), False, True,
                                 ["na_c"], ["na_psS%d" % (c % 2)])
                            P.add("act", lambda e, c=c, nn=nn, ps=ps: e.activation(ee[c % 2][0:nn, :], ps[0:nn, :], AF.Exp),
                                  ["na_psS%d" % (c % 2)], ["na_e%d" % (c % 2)])
                        for g in range(4):
                            for ci, c in enumerate(chunks):
                                nn = min(128, ncmp - c * 128)
                                P.mm(psO1[:, g * 65:(g + 1) * 65], ee[c % 2][0:nn, g * 128:(g + 1) * 128], vx[0:nn, c, 0:65],
                                     ci == 0, ci == len(chunks) - 1, ["na_e%d" % (c % 2), "na_vx", "na_vxc"], ["na_psO1"])
                        for g in range(4):
                            for ci, c in enumerate(chunks):
                                nn = min(128, ncmp - c * 128)
                                P.mm(psO2[:, g * NS:(g + 1) * NS], ee[c % 2][0:nn, g * 128:(g + 1) * 128], vx[0:nn, c, 65:65 + NS],
                                     ci == 0, ci == len(chunks) - 1, ["na_e%d" % (c % 2), "na_vx", "na_vxc"], ["na_psO2"])
                        P.add("act", lambda e: e.activation(o1[:], psO1[:, 0:260].rearrange("p (g d) -> p g d", d=65), AF.Copy), ["na_psO1"], ["na_o1"])
                        P.add("dve", lambda e: e.tensor_copy(o2[:], psO2[:, 0:4 * NS].rearrange("p (g d) -> p g d", d=NS)), ["na_psO2"], ["na_o2"])
                        P.add("dve", lambda e: e.tensor_scalar(rden[:], o1[:, :, 64], 1e-30, None, ALU.max), ["na_o1"], ["na_rden"])
                        P.add("dve", lambda e: e.reciprocal(rden[:], rden[:]), ["na_rden"], ["na_rden"])
                        P.add("dve", lambda e, s_=s_: e.tensor_tensor(oc[s_][:], o1[:, :, 0:64], rden[:].unsqueeze(2).to_broadcast([128, 4, 64]), ALU.mult),
                              ["na_o1", "na_rden"], [koc])
                        P.add("dve", lambda e: e.tensor_tensor(o2[:], o2[:], rden[:].unsqueeze(2).to_broadcast([128, 4, NS]), ALU.mult),
                              ["na_o2", "na_rden"], ["na_o2"])
                        P.add("dve", lambda e: e.reduce_sum(imp[:], o2[:].rearrange("p g s -> p s g"), AX.X), ["na_o2"], ["na_imp"])
                    P.dma("pool", S["oc_tok"][b * T + j * 128:b * T + (j + 1) * 128, hk * 256:(hk + 1) * 256],
                          oc[s_][:].rearrange("p g d -> p (g d)"), [koc], [], "st" + koc)
                    o2_ = NS - 2 * j
                    P.add("dve", lambda e, o2_=o2_: e.tensor_tensor(imp[:], imp[:], keepw[:, o2_:o2_ + NS], ALU.mult), ["na_imp", "na_c"], ["na_imp"])
                    P.add("dve", lambda e, o2_=o2_: e.tensor_tensor(imp[:], imp[:], addw[:, o2_:o2_ + NS], ALU.add), ["na_imp", "na_c"], ["na_imp"])
                    P.add("dve", lambda e: e.memset(imp[:, 0:1], 1e30), ["na_imp"], ["na_imp"])
                    P.add("dve", lambda e: e.max(m8[:, 0:8], imp[:]), ["na_imp"], ["na_m8"])
                    P.add("dve", lambda e: e.match_replace(imp2[:], m8[:, 0:8], imp[:], -3e38), ["na_imp", "na_m8"], ["na_imp2"])
                    P.add("dve", lambda e: e.max(m8[:, 8:16], imp2[:]), ["na_imp2"], ["na_m8"])
                    P.add("dve", lambda e: e.tensor_scalar(selb[:], imp[:], m8[:, 15:16], None, ALU.is_ge), ["na_imp", "na_m8"], ["na_selb"])
                    P.add("dve", lambda e: e.tensor_scalar(selb[:], selb[:], -1.0, -NEG, ALU.add, ALU.mult), ["na_selb"], ["na_selb"])
                    P.tr(psT[0:NS, 0:128], selb[:], ident[:], ["na_selb", "na_c"], ["na_psT"])
                    P.add("act", lambda e, j=j: e.activation(sbT[0:NS, j * 128:(j + 1) * 128], psT[0:NS, 0:128], AF.Copy), ["na_psT"], ["na_sbT"])
                P.dma("sp", S["selbT"][r * 64:r * 64 + NS, :], sbT[0:NS, :], ["na_sbT"], [], "na_stsb")
    P.barrier()


def nsa_attn_b(P, nc, NB, T, cst, S, yT_d, yrow0):
    NT = T // 128
    NS = T // 64
    WT = 4
    with ExitStack() as es:
        stg = SB(es, nc, "nb_stg", [64, 4, 512]); stg2 = SB(es, nc, "nb_stg2", [128, 512])
        qb = SB(es, nc, "nb_qb", [64, 4, T], BF16); ks = SB(es, nc, "nb_ks", [64, T], BF16); kw = SB(es, nc, "nb_kw", [64, T], BF16)
        sb = SB(es, nc, "nb_sb", [64, T], BF16); eall = SB(es, nc, "nb_eall", [64, T], BF16)
        caus = SB(es, nc, "nb_caus", [128, 128], BF16); anti = SB(es, nc, "nb_anti", [128, 128], BF16); idb = SB(es, nc, "nb_idb", [128, 128], BF16)
        ident = SB(es, nc, "nb_id", [128, 128])
        vsx = SB(es, nc, "nb_vsx", [128, NT, 65], BF16); vwx = SB(es, nc, "nb_vwx", [128, NT, 65], BF16)
        vst = SB(es, nc, "nb_vst", [128, NT, 64])
        gat = SB(es, nc, "nb_gat", [128, NT, 24])
        eall_s = SB(es, nc, "nb_eas", [128, NT, 512], BF16); eall_w = SB(es, nc, "nb_eaw", [128, WT + 1, 512], BF16)
        oct_ = SB(es, nc, "nb_oct", [128, 4, 64]); os_ = SB(es, nc, "nb_os", [128, 4, 65]); ow_ = SB(es, nc, "nb_ow", [128, 4, 65])
        wsw = SB(es, nc, "nb_wsw", [128, 2, 4]); acc = SB(es, nc, "nb_acc", [128, 4, 64]); tmp = SB(es, nc, "nb_tmp", [128, 4, 64])
        yw = SB(es, nc, "nb_yw", [128, 2, T])
        psS = [PS(es, nc, "nb_psS%d" % i, [128, 512]) for i in range(2)]
        psOs = PS(es, nc, "nb_psOs", [128, 512]); psOw = PS(es, nc, "nb_psOw", [128, 512]); psT = PS(es, nc, "nb_psT", [128, 512])

        def load_cast(dst, src, shape_p, keyd, wide):
            P.dma("sp", stg2[0:shape_p, 0:wide], src, [], ["nb_stg2"], "nb_stg2")
            P.add("act", lambda e: e.activation(dst, stg2[0:shape_p, 0:wide], AF.Copy), ["nb_stg2"], [keyd])
        for c0 in range(0, T, 512):
            load_cast(eall[:, c0:c0 + 512], cst["eall"][:, c0:c0 + 512], 64, "nb_eall", 512)
        load_cast(caus[:], cst["caus"], 128, "nb_cm", 128)
        load_cast(anti[:], cst["anti"], 128, "nb_cm", 128)
        load_cast(idb[:], cst["ident"], 128, "nb_cm", 128)
        P.dma("sp", ident[:], cst["ident"], [], ["nb_id"], "nb_c0")
        for b in range(NB):
            P.dma("sp", gat[:], S["gat_tok"][b * T:(b + 1) * T, :].rearrange("(j p) f -> p j f", p=128), [], ["nb_gat"], "nb_gat")
            for hk in range(2):
                r = b * 2 + hk
                for c0 in range(0, T, 512):
                    P.dma("sp", stg[:], S["qrot"][hk * 256:(hk + 1) * 256, b * T + c0:b * T + c0 + 512].rearrange("(g d) n -> d g n", d=64),
                          [], ["nb_stg"], "nb_stg")
                    P.add("act", lambda e, c0=c0: e.activation(qb[:, :, c0:c0 + 512], stg[:], AF.Copy), ["nb_stg"], ["nb_qb"])
                    load_cast(ks[:, c0:c0 + 512], S["ksr"][hk * 64:(hk + 1) * 64, b * T + c0:b * T + c0 + 512], 64, "nb_ks", 512)
                    load_cast(kw[:, c0:c0 + 512], S["kwr"][hk * 64:(hk + 1) * 64, b * T + c0:b * T + c0 + 512], 64, "nb_kw", 512)
                    load_cast(sb[0:NS, c0:c0 + 512], S["selbT"][r * 64:r * 64 + NS, c0:c0 + 512], NS, "nb_sb", 512)
                for vx_, nm, kk_ in ((vsx, "vs_tok", "nb_vsx"), (vwx, "vw_tok", "nb_vwx")):
                    P.dma("sp", vst[:], S[nm][b * T:(b + 1) * T, hk * 64:(hk + 1) * 64].rearrange("(j p) d -> p j d", p=128), [], ["nb_vst"], "nb_vst")
                    P.add("dve", lambda e, vx_=vx_: e.tensor_copy(vx_[:, :, 0:64], vst[:]), ["nb_vst"], [kk_])
                    P.add("dve", lambda e, vx_=vx_: e.memset(vx_[:, :, 64:65], 1.0), [], [kk_])
                k = 0
                for j in range(NT):
                    qs = qb[:, :, j * 128:(j + 1) * 128]
                    for br, (kk_b, vx_, pso, kps) in enumerate(((ks, vsx, psOs, "nb_psOs"), (kw, vwx, psOw, "nb_psOw"))):
                        kts = list(range(0, j + 1)) if br == 0 else list(range(max(0, j - WT), j + 1))
                        ea = eall_s if br == 0 else eall_w
                        kea = "nb_eas" if br == 0 else "nb_eaw"
                        for ki_, kt in enumerate(kts):
                            ps = psS[k % 2]; kp = "nb_psS%d" % (k % 2)
                            k += 1
                            extra = []
                            if br == 0 and NS > 16 and (2 * j + 1) >= 16:
                                extra.append((eall[0:NS, kt * 128:(kt + 1) * 128], sb[0:NS, j * 128:(j + 1) * 128], NS, ["nb_eall", "nb_sb"]))
                            if kt == j:
                                extra.append((idb[:], caus[:], 128, ["nb_cm"]))
                            if br == 1 and kt == j - WT:
                                extra.append((idb[:], anti[:], 128, ["nb_cm"]))
                            P.mm(ps[:], kk_b[:, kt * 128:(kt + 1) * 128], qs, True, not extra, ["nb_ks", "nb_kw", "nb_qb"], [kp])
                            for xi, (l_, r_, pp, rk) in enumerate(extra):
                                P.mm(ps[:], l_, r_.unsqueeze(1).to_broadcast([pp, 4, 128]), False, xi == len(extra) - 1, rk, [kp])
                            P.add("act", lambda e, ea=ea, ki_=ki_, ps=ps: e.activation(ea[:, ki_, :], ps[:], AF.Exp), [kp], [kea])
                        for g in range(4):
                            for ki_, kt in enumerate(kts):
                                P.mm(pso[:, g * 65:(g + 1) * 65], ea[:, ki_, g * 128:(g + 1) * 128], vx_[:, kt, :], ki_ == 0, ki_ == len(kts) - 1,
                                     [kea, "nb_vsx", "nb_vwx"], [kps])
                    P.dma("sp", oct_[:].rearrange("p g d -> p (g d)"), S["oc_tok"][b * T + j * 128:b * T + (j + 1) * 128, hk * 256:(hk + 1) * 256],
                          [], ["nb_oct"], "nb_oct")
                    P.add("act", lambda e: e.activation(os_[:], psOs[:, 0:260].rearrange("p (g d) -> p g d", d=65), AF.Copy), ["nb_psOs"], ["nb_os"])
                    P.add("dve", lambda e: e.tensor_copy(ow_[:], psOw[:, 0:260].rearrange("p (g d) -> p g d", d=65)), ["nb_psOw"], ["nb_ow"])
                    gv = gat[:, j, hk * 12:(hk + 1) * 12].rearrange("p (g t) -> p g t", t=3)
                    P.add("dve", lambda e: e.tensor_scalar(wsw[:, 0, :], os_[:, :, 64], 1e-30, None, ALU.max), ["nb_os"], ["nb_wsw"])
                    P.add("dve", lambda e: e.tensor_scalar(wsw[:, 1, :], ow_[:, :, 64], 1e-30, None, ALU.max), ["nb_ow"], ["nb_wsw"])
                    P.add("dve", lambda e: e.reciprocal(wsw[:], wsw[:]), ["nb_wsw"], ["nb_wsw"])
                    P.add("dve", lambda e, gv=gv: e.tensor_tensor(wsw[:, 0, :], wsw[:, 0, :], gv[:, :, 1], ALU.mult), ["nb_wsw", "nb_gat"], ["nb_wsw"])
                    P.add("dve", lambda e, gv=gv: e.tensor_tensor(wsw[:, 1, :], wsw[:, 1, :], gv[:, :, 2], ALU.mult), ["nb_wsw", "nb_gat"], ["nb_wsw"])
                    P.add("dve", lambda e, gv=gv: e.tensor_tensor(acc[:], oct_[:], gv[:, :, 0:1].to_broadcast([128, 4, 64]), ALU.mult),
                          ["nb_oct", "nb_gat"], ["nb_acc"])
                    P.add("pool", lambda e: e.tensor_tensor(tmp[:], os_[:, :, 0:64], wsw[:, 0, :].unsqueeze(2).to_broadcast([128, 4, 64]), ALU.mult),
                          ["nb_os", "nb_wsw"], ["nb_tmp"])
                    P.add("dve", lambda e: e.tensor_tensor(acc[:], acc[:], tmp[:], ALU.add), ["nb_acc", "nb_tmp"], ["nb_acc"])
                    P.add("pool", lambda e: e.tensor_tensor(tmp[:], ow_[:, :, 0:64], wsw[:, 1, :].unsqueeze(2).to_broadcast([128, 4, 64]), ALU.mult),
                          ["nb_ow", "nb_wsw", "nb_acc"], ["nb_tmp"])
                    P.add("dve", lambda e: e.tensor_tensor(acc[:], acc[:], tmp[:], ALU.add), ["nb_acc", "nb_tmp"], ["nb_acc"])
                    for c in range(2):
                        P.tr(psT[:, c * 128:(c + 1) * 128], acc[:, 2 * c:2 * c + 2, :].rearrange("p g d -> p (g d)"), ident[:], ["nb_acc", "nb_id"], ["nb_psT"])
                    P.add("act", lambda e, j=j: e.activation(yw[:, :, j * 128:(j + 1) * 128], psT[:, 0:256].rearrange("p (c n) -> p c n", c=2), AF.Copy),
                          ["nb_psT"], ["nb_yw"])
                P.dma("pool", yT_d[yrow0 + hk * 256:yrow0 + (hk + 1) * 256, b * T:(b + 1) * T].rearrange("(c p) n -> p c n", p=128), yw[:], ["nb_yw"], [], "nb_styw")
    P.barrier()

def make_consts(T):
    NS = T // 64
    ncmp = (T - 32) // 16 + 1
    NCP = (ncmp + 127) // 128 * 128
    c = {}
    c["ident"] = np.eye(128, dtype=np.float32)
    c["id64"] = np.eye(64, dtype=np.float32)
    b = np.zeros((128, 128), np.float32); b[:64, :64] = 1; b[64:, 64:] = 1
    c["blk1"] = b
    rm = np.ones(512, np.float32); rm[::64] = 0
    c["rmask"] = rm
    ms = np.triu(np.ones((64, 64), np.float32), 1); ml = np.tril(np.ones((64, 64), np.float32), -1); mi = np.triu(np.ones((64, 64), np.float32), 0)
    c["masks"] = np.concatenate([-ms, -ml, ms, mi, -mi], 1)
    c["tri"] = np.concatenate([np.triu(np.ones((128, 128), np.float32), 1), np.ones((128, 128), np.float32)], 1)
    c["iop"] = np.arange(128, dtype=np.float32).reshape(128, 1)
    inv = (1.0 / (10000.0 ** (np.arange(0, 64, 2, dtype=np.float32) / 64))).astype(np.float32)
    c["invf"] = inv[(np.arange(128) % 32)].reshape(128, 1).astype(np.float32)
    R = np.zeros((64, 64), np.float32)
    for m in range(32):
        R[m, m + 32] = -1.0; R[m + 32, m] = 1.0
    rot = np.zeros((128, 128), np.float32); rot[:64, :64] = R.T; rot[64:, 64:] = R.T
    c["rot"] = rot
    n = np.arange(128)[:, None]; xp = np.arange(2048 + T)[None, :]
    c["tab"] = np.where(16 * n + 31 <= xp - 2048, 0.0, -30000.0).astype(np.float32)
    cs = np.arange(NCP)[:, None] * 16; ss = np.arange(NS)[None, :] * 64
    ov = np.clip(np.minimum(cs + 32, ss + 64) - np.maximum(cs, ss), 0, None) / 32.0
    ov[ncmp:] = 0
    c["ovx"] = np.concatenate([np.ones((NCP, 1)), ov], 1).astype(np.float32)
    tl = np.arange(128)[:, None]; x = np.arange(2 * NS)[None, :]
    rel = x - NS; cur = tl // 64
    c["keepw"] = (rel < cur - 1).astype(np.float32)
    c["addw"] = np.where((rel == cur) | (rel == cur - 1), 1e30, np.where(rel > cur, -1e30, 0.0)).astype(np.float32)
    ea = np.zeros((64, T), np.float32); ea[np.arange(T) // 64, np.arange(T)] = 1.0
    c["eall"] = ea
    k = np.arange(128)[:, None]; t = np.arange(128)[None, :]
    c["caus"] = np.where(k <= t, 0.0, -30000.0).astype(np.float32)
    c["anti"] = np.where(k > t, 0.0, -30000.0).astype(np.float32)
    return c


def build(NB, T, debug=False, same_engine_sync=True):
    N = NB * T
    NS = T // 64
    ncmp = (T - 32) // 16 + 1
    NCP = (ncmp + 127) // 128 * 128
    NBLK = (2 * N) // BLK + NEXP
    NT = N // 128
    Cc = make_consts(T)
    nc = bass.Bass("TRN2", target_bir_lowering=False)
    din = lambda name, shape, dt=F32: nc.dram_tensor(name, list(shape), dt, kind="ExternalInput").ap()
    scr = lambda name, shape: nc.dram_tensor(name, list(shape), F32, kind=("ExternalOutput" if debug else "Internal")).ap()
    x = din("x", [N, 1024]); p = din("p", [2, N, 256]); pos = din("pos", [NB, T], I32)
    ev_w_in = din("ev_w_in", [1024, 3096]); ev_w_out = din("ev_w_out", [1024, 1024])
    od_w_in = din("od_w_in", [1024, 4096]); od_w_out = din("od_w_out", [1024, 1024])
    rw = dict(mu=din("rw_mu", [1792]), w0=din("rw_w0", [512]), w2=din("rw_w2", [64, 512]), a0=din("rw_a0", [512]),
              a2=din("rw_a2", [64, 512]), g2=din("rw_g2", [128, 512]), k_k=din("rw_k_k", [512]), k_a=din("rw_k_a", [512]),
              r_k=din("rw_r_k", [512]))
    gng = din("rw_gn_g", [512]); gnb = din("rw_gn_b", [512])
    cpos = din("nsa_cmp_pos", [2, 32, 64]); cw1 = din("nsa_cmp_w1", [2, 2048, 128]); cw2 = din("nsa_cmp_w2", [2, 128, 64])
    hg_lb = din("hg_lb", [2, 1024]); hg_ng = din("hg_norm_g", [128])
    wrg = din("moe_w_rg", [2, 1024, 4]); brg = din("moe_b_rg", [2, 4]); wre = din("moe_w_re", [2, 1024, 32]); bre = din("moe_b_re", [2, 32])
    mw1 = [din("moe_w1_%d" % i, [4096, 4096]) for i in range(2)]; mw3 = [din("moe_w3_%d" % i, [4096, 4096]) for i in range(2)]
    mw2 = [din("moe_w2_%d" % i, [4096, 4096]) for i in range(2)]
    ln_g = din("ln_g", [2, 2, 1024]); ln_b = din("ln_b", [2, 2, 1024])
    ple_w = din("ple_w", [2, 256, 1024]); ple_gw = din("ple_gate_w", [2, 1024, 1024])
    cst = {k: din("c_" + k, Cc[k].shape) for k in Cc}
    thr = din("c_thr", [NBLK])
    out = nc.dram_tensor("out", [N, 1024], F32, kind="ExternalOutput").ap()
    zT = scr("zT", [4096, N]); yT = scr("yT0", [1024, N]); yT1 = scr("yT1", [1024, N]); h1 = scr("h1", [N, 1024]); x1 = scr("x1", [N, 1024])
    xbuf = nc.dram_tensor("xbuf", [NBLK * BLK, 1024], F32).ap(); ybuf = nc.dram_tensor("ybuf", [NBLK * BLK, 1024], F32).ap()
    SR = {k: scr("SR_" + k, [512, N]) for k in ("rt", "kt", "kp", "bt", "ktl", "btl", "g", "bon")}
    SR["pc"] = scr("SR_pc", [512, N // 64]); SR["vtok"] = scr("SR_vtok", [N, 512])
    SH = {k: scr("SH_" + k, [1024, N]) for k in ("rt", "kt", "ktl", "g")}
    SH["pc"] = scr("SH_pc", [1024, N // 64]); SH["vtok"] = scr("SH_vtok", [N, 1024])
    SN = dict(qraw=scr("SN_qraw", [512, N]), qrot=scr("SN_qrot", [512, N]), ksr=scr("SN_ksr", [128, N]), kwr=scr("SN_kwr", [128, N]),
              vs_tok=scr("SN_vs", [N, 128]), vw_tok=scr("SN_vw", [N, 128]), gat_tok=scr("SN_gat", [N, 24]),
              kcmpT=scr("SN_kcmpT", [NB * 2 * 64, NCP]), vcmp=scr("SN_vcmp", [NB * 2, NCP, 64]),
              oc_tok=scr("SN_oc", [N, 512]), selbT=scr("SN_selbT", [NB * 2 * 64, T]))
    R = {}
    R["OH"] = nc.alloc_sbuf_tensor("r_OH", [128, NT, 2, 32], F32)
    R["rank"] = nc.alloc_sbuf_tensor("r_rank", [128, NT, 2], F32)
    R["gates"] = nc.alloc_sbuf_tensor("r_gates", [128, NT, 2], F32)
    R["base"] = nc.alloc_sbuf_tensor("r_base", [128, 32], F32)
    R["dest_i"] = nc.alloc_sbuf_tensor("r_dest_i", [128, NT * 2], I32)
    R["widx"] = nc.alloc_sbuf_tensor("r_widx", [128, NBLK], I32)
    P = Prog(nc, same_engine_sync=same_engine_sync)
    ident = cst["ident"]
    gemm_fm(P, nc, N, x, ev_w_in, 3096, zT, ident, "g0")
    prm = dict(rw); prm.update(blk1=cst["blk1"], ident=ident, rmask=cst["rmask"])
    rwkv_prep(P, nc, NB, T, zT, prm, SR)
    D = dict(SR); D.update(masks=cst["masks"], id64=cst["id64"], ident=ident, gng=gng, gnb=gnb)
    chunk_scan(P, nc, NB, T, D, True, 64, 64, 8, yT, 0, "rw", gn_eps=64e-5)
    nsa_prep(P, nc, NB, T, zT, pos, cst, SN)
    nsa_cmp(P, nc, NB, T, zT, cw1, cw2, cpos, SN)
    nsa_attn_a(P, nc, NB, T, cst, SN)
    nsa_attn_b(P, nc, NB, T, cst, SN, yT, 512)
    phase_t1(P, nc, N, yT, x, ev_w_out, ln_g[0, 0], ln_b[0, 0], wrg[0], wre[0], brg[0], bre[0], ident, cst["tri"], h1, R)
    phase_t2(P, nc, N, h1, xbuf, thr, cst["iop"], R)
    phase_t3(P, nc, N, xbuf, ybuf, mw1[0], mw3[0], mw2[0], ident, R)
    phase_t4(P, nc, N, h1, ybuf, p[0], ln_g[0, 1], ln_b[0, 1], ple_gw[0], ple_w[0], ident, x1, R)
    gemm_fm(P, nc, N, x1, od_w_in, 4096, zT, ident, "g1")
    hgrn_prep(P, nc, NB, T, zT, dict(lb2=hg_lb, ident=ident, rmask=cst["rmask"]), SH)
    D = dict(SH); D.update(masks=cst["masks"], id64=cst["id64"], ident=ident, normg=hg_ng)
    chunk_scan(P, nc, NB, T, D, False, 128, 128, 8, yT1, 0, "hg", gn_eps=1e-5)
    phase_t1(P, nc, N, yT1, x1, od_w_out, ln_g[1, 0], ln_b[1, 0], wrg[1], wre[1], brg[1], bre[1], ident, cst["tri"], h1, R)
    phase_t2(P, nc, N, h1, xbuf, thr, cst["iop"], R)
    phase_t3(P, nc, N, xbuf, ybuf, mw1[1], mw3[1], mw2[1], ident, R)
    phase_t4(P, nc, N, h1, ybuf, p[1], ln_g[1, 1], ln_b[1, 1], ple_gw[1], ple_w[1], ident, out, R)
    P.finalize(); P.emit()
    return nc, Cc, len(P.ops)


def make_in_map(inp, b0, NB, T, Cc):
    N = NB * T
    NBLK = (2 * N) // BLK + NEXP
    f = lambda a: np.ascontiguousarray(np.asarray(a))
    m = {}
    m["x"] = f(inp["x"][b0:b0 + NB, :T]).reshape(N, 1024)
    m["p"] = f(inp["p"][:, b0:b0 + NB, :T]).reshape(2, N, 256)
    m["pos"] = f(inp["positions"][b0:b0 + NB, :T]).astype(np.int32)
    m["ev_w_in"] = f(inp["ev_w_in"][0]); m["ev_w_out"] = f(inp["ev_w_out"][0])
    m["od_w_in"] = f(inp["od_w_in"][0]); m["od_w_out"] = f(inp["od_w_out"][0])
    for k in ("mu", "w0", "w2", "a0", "a2", "g2", "k_k", "k_a", "gn_g", "gn_b"):
        m["rw_" + k] = f(inp["rw_" + k][0])
    m["rw_r_k"] = f(inp["rw_r_k"][0]).reshape(512)
    m["nsa_cmp_pos"] = f(inp["nsa_cmp_pos"][0]); m["nsa_cmp_w1"] = f(inp["nsa_cmp_w1"][0]); m["nsa_cmp_w2"] = f(inp["nsa_cmp_w2"][0])
    m["hg_lb"] = f(inp["hg_lb"]); m["hg_norm_g"] = f(inp["hg_norm_g"][0])
    for k in ("moe_w_rg", "moe_b_rg", "moe_w_re", "moe_b_re", "ln_g", "ln_b", "ple_w", "ple_gate_w"):
        m[k] = f(inp[k])
    for k in ("moe_w1", "moe_w3", "moe_w2"):
        for li in range(2):
            m[k + "_%d" % li] = f(inp[k][li]).reshape(4096, 4096)
    for k in Cc:
        m["c_" + k] = Cc[k]
    m["c_thr"] = (np.arange(NBLK) * BLK).astype(np.float32)
    return m


def kernel(**inputs):
    NB, T = 2, 4096
    nc, Cc, nops = build(NB, T)
    in_maps = [make_in_map(inputs, c * NB, NB, T, Cc) for c in range(8)]
    res = run_bass_kernel_spmd(nc, in_maps, core_ids=list(range(8)))
    out = np.concatenate([np.asarray(r["out"]).reshape(NB, T, 1024) for r in res.results], 0)
    return out.astype(np.float32)
```

```python
from contextlib import ExitStack
import math, os
import numpy as np
import concourse.bass as bass
import concourse.mybir as mybir
from concourse.bass_utils import run_bass_kernel_spmd

F32 = mybir.dt.float32
BF16 = mybir.dt.bfloat16
I32 = mybir.dt.int32
U32 = mybir.dt.uint32
AF = mybir.ActivationFunctionType
ALU = mybir.AluOpType
AX = mybir.AxisListType

EPOCH = 12000
DMA_CAP = 1800


class _Op:
    __slots__ = ("eng", "fn", "reads", "writes", "dma", "semkey", "deps", "waits",
                 "inc", "sem", "val", "barrier", "seq")


class Prog:
    ENGS = ("pe", "act", "dve", "pool", "sp")

    def __init__(self, nc, same_engine_sync=True):
        self.nc = nc
        self.ops = []
        self.same_engine_sync = same_engine_sync
        self._sems = []
        self._keep = []

    def add(self, eng, fn, reads=(), writes=(), dma=False, semkey=None):
        o = _Op()
        o.eng = eng; o.fn = fn
        o.reads = tuple(reads); o.writes = tuple(writes)
        o.dma = dma; o.semkey = semkey
        o.deps = []; o.waits = []; o.inc = False; o.sem = None; o.val = 0
        o.barrier = False; o.seq = len(self.ops)
        if dma and semkey is None:
            raise ValueError("dma op needs semkey")
        self.ops.append(o)
        return o

    def barrier(self):
        for e in self.ENGS:
            o = self.add(e, None)
            o.barrier = True

    def dma(self, q, out, in_, reads, writes, semkey, **kw):
        return self.add(q, lambda e: e.dma_start(out=out, in_=in_, **kw), reads, writes,
                        dma=True, semkey=semkey)

    def mm(self, out, lhsT, rhs, start, stop, reads, writes):
        return self.add("pe", lambda e: e.matmul(out, lhsT, rhs, start=start, stop=stop),
                        reads, writes)

    def tr(self, out, in_, ident, reads, writes):
        return self.add("pe", lambda e: e.transpose(out, in_, ident), reads, writes)

    def _new_sem(self, name):
        h = self.nc.alloc_semaphore(name=name)
        self._sems.append(h)
        return h

    def finalize(self):
        ops = self.ops
        last_w = {}
        readers = {}
        last_of_eng = {}
        for o in ops:
            if o.barrier:
                continue
            deps = set()
            for k in o.reads:
                w = last_w.get(k)
                if w is not None:
                    deps.add(w)
            for k in o.writes:
                w = last_w.get(k)
                if w is not None:
                    deps.add(w)
                for r in readers.get(k, ()):
                    deps.add(r)
            deps.discard(o.seq)
            for d in deps:
                od = ops[d]
                if od.dma:
                    o.deps.append(d)
                elif od.eng != o.eng or o.dma:
                    o.deps.append(d)
                else:
                    if self.same_engine_sync and o.eng != "pe":
                        o.deps.append(d)
            for k in o.reads:
                readers.setdefault(k, []).append(o.seq)
            for k in o.writes:
                last_w[k] = o.seq
                readers[k] = []
        for o in ops:
            for d in o.deps:
                ops[d].inc = True
        eng_sem = {}
        dma_sem = {}
        last_inc_op = {}
        cur_last = {}
        for o in ops:
            if o.barrier:
                if o.eng == self.ENGS[0]:
                    for e, lo in cur_last.items():
                        lo.inc = True
                continue
            if not o.dma:
                cur_last[o.eng] = o
        waited = {e: {} for e in self.ENGS}
        released = set()
        all_dma_state = {}
        eng_state = {}
        nsem = 0
        for o in ops:
            if o.barrier:
                if o.eng == self.ENGS[-1]:
                    pass
                for e, (s, c) in eng_state.items():
                    if e != o.eng and c > 0:
                        o.waits.append((s, c))
                for k, (s, c) in all_dma_state.items():
                    if c > 0:
                        o.waits.append((s, c * 16))
                if o.eng == self.ENGS[-1]:
                    for v in dma_sem.values():
                        released.add(id(v))
                    dma_sem = {}
                continue
            for d in o.deps:
                od = ops[d]
                o.waits.append((od.sem, od.val))
            if o.dma:
                st = dma_sem.get(o.semkey)
                if st is None or st[1] >= DMA_CAP:
                    used = set(id(v) for v in dma_sem.values())
                    cands = [v for v in self._keep if id(v) not in used and id(v) in released and v[1] < DMA_CAP - 300]
                    if cands:
                        st = min(cands, key=lambda v: v[1])
                        released.discard(id(st))
                    else:
                        st = [self._new_sem("d%d" % nsem), 0]; nsem += 1
                        self._keep.append(st)
                    dma_sem[o.semkey] = st
                st[1] += 1
                o.sem = st[0]; o.val = st[1] * 16; o.inc = True
                all_dma_state[id(st)] = (st[0], st[1])
            elif o.inc:
                st = eng_sem.get(o.eng)
                if st is None or st[1] >= EPOCH:
                    st = [self._new_sem("e%d" % nsem), 0]; nsem += 1
                    eng_sem[o.eng] = st
                st[1] += 1
                o.sem = st[0]; o.val = st[1]
                eng_state[o.eng] = (st[0], st[1])
        for o in ops:
            w = waited[o.eng]
            best = {}
            for (s, v) in o.waits:
                key = id(s)
                if w.get(key, 0) >= v:
                    continue
                if key not in best or best[key][1] < v:
                    best[key] = (s, v)
            o.waits = list(best.values())
            for key, (s, v) in best.items():
                w[key] = v
        self.nsem = nsem
        return self

    def emit(self):
        nc = self.nc
        per = {e: [o for o in self.ops if o.eng == e] for e in self.ENGS}

        def run(engine, lst):
            for o in lst:
                for (s, v) in o.waits:
                    engine.wait_ge(s, v)
                if o.fn is None:
                    continue
                ins = o.fn(engine)
                if o.inc:
                    ins.then_inc(o.sem, 16 if o.dma else 1)

        with nc.Block() as block:
            @block.tensor
            def _(e):
                run(e, per["pe"])

            @block.scalar
            def _(e):
                run(e, per["act"])

            @block.vector
            def _(e):
                run(e, per["dve"])

            @block.gpsimd
            def _(e):
                run(e, per["pool"])

            @block.sync
            def _(e):
                run(e, per["sp"])


ALPHA = float((2 * 2) ** 0.25)
LN_EPS = 1e-5
BLK = 512
NEXP = 32


_UNIQ = [0]


def SB(es, nc, name, shape, dt=F32):
    _UNIQ[0] += 1
    return es.enter_context(nc.sbuf_tensor("s%d_%s" % (_UNIQ[0], name), list(shape), dt))


def PS(es, nc, name, shape, dt=F32):
    _UNIQ[0] += 1
    return es.enter_context(nc.psum_tensor("p%d_%s" % (_UNIQ[0], name), list(shape), dt))


def layer_norm_tile(P, u, out, g_t, b_t, st, mv, sd, rs, tag, eng2="pool"):
    ku, ko = tag + "u", tag + "o"
    P.add("dve", lambda e: e.bn_stats(st[:, 0, :], u[:, 0:512]), [ku], [tag + "st0"])
    P.add("dve", lambda e: e.bn_stats(st[:, 1, :], u[:, 512:1024]), [ku], [tag + "st1"])
    P.add("dve", lambda e: e.bn_aggr(mv[:], st[:].rearrange("p a b -> p (a b)")),
          [tag + "st0", tag + "st1"], [tag + "mv"])
    P.add("act", lambda e: e.activation(sd[:], mv[:, 1:2], AF.Sqrt, bias=LN_EPS, scale=1.0),
          [tag + "mv"], [tag + "sd"])
    P.add("dve", lambda e: e.reciprocal(rs[:], sd[:]), [tag + "sd"], [tag + "rs"])
    P.add("dve", lambda e: e.tensor_scalar(out, u, mv[:, 0:1], rs[:, 0:1], ALU.subtract, ALU.mult),
          [ku, tag + "mv", tag + "rs"], [ko])
    P.add(eng2, lambda e: e.tensor_tensor(out, out, g_t, ALU.mult), [ko, tag + "g"], [ko])
    P.add(eng2, lambda e: e.tensor_tensor(out, out, b_t, ALU.add), [ko, tag + "b"], [ko])


def phase_t1(P, nc, N, yT_d, x_d, wout_d, lng_d, lnb_d, wrg_d, wre_d, brg_d, bre_d,
             ident_d, tri_d, h1_d, R):
    NT = N // 128
    with ExitStack() as es:
        wout_b = SB(es, nc, "wout_b", [128, 8, 1024], BF16)
        wst = SB(es, nc, "wst", [128, 8, 512], F32)
        g_t = SB(es, nc, "g_t", [128, 1024]); b_t = SB(es, nc, "b_t", [128, 1024])
        wr = SB(es, nc, "wr", [128, 8, 36]); br = SB(es, nc, "br", [128, 36])
        ident = SB(es, nc, "ident", [128, 128]); tri = SB(es, nc, "tri", [128, 256])
        yt = [SB(es, nc, "yt%d" % i, [128, 8, 128]) for i in range(2)]
        ytb = [SB(es, nc, "ytb%d" % i, [128, 8, 128], BF16) for i in range(2)]
        xt = [SB(es, nc, "xt%d" % i, [128, 1024]) for i in range(2)]
        u = SB(es, nc, "u", [128, 1024]); h1 = [SB(es, nc, "h1_%d" % i, [128, 1024]) for i in range(2)]
        h1T = SB(es, nc, "h1T", [128, 8, 128])
        st = SB(es, nc, "st", [128, 2, 6]); mv = SB(es, nc, "mv", [128, 2])
        sd = SB(es, nc, "sd", [128, 1]); rs = SB(es, nc, "rs", [128, 1])
        lg = SB(es, nc, "lg", [128, 36]); sm = SB(es, nc, "sm", [128, 16])
        ohg = SB(es, nc, "ohg", [128, 4]); eg = SB(es, nc, "eg", [128, 4])
        le = SB(es, nc, "le", [128, 8]); top8 = SB(es, nc, "top8", [128, 8])
        m12 = SB(es, nc, "m12", [128, 2, 8]); t12 = SB(es, nc, "t12", [128, 32]); junk = SB(es, nc, "junk", [128, 32])
        psA = [PS(es, nc, "psA%d" % i, [128, 512]) for i in range(2)]
        psT = [PS(es, nc, "psT%d" % i, [128, 512]) for i in range(2)]
        psL = PS(es, nc, "psL", [128, 512]); psR = PS(es, nc, "psR", [128, 512])
        OH, rank, gates, base = R["OH"], R["rank"], R["gates"], R["base"]

        for hh in range(2):
            P.dma("sp", wst[:], wout_d[:, hh * 512:(hh + 1) * 512].rearrange("(c p) n -> p c n", p=128),
                  [], ["wst"], "wst")
            P.add("act", lambda e, hh=hh: e.activation(wout_b[:, :, hh * 512:(hh + 1) * 512], wst[:], AF.Copy),
                  ["wst"], ["wout_b"])
        P.dma("sp", g_t[:], lng_d.partition_broadcast(128), [], ["L1g"], "cst1")
        P.dma("sp", b_t[:], lnb_d.partition_broadcast(128), [], ["L1b"], "cst1")
        P.dma("pool", wr[:, :, 0:4], wrg_d.rearrange("(c p) e -> p c e", p=128), [], ["wr"], "cst2")
        P.dma("pool", wr[:, :, 4:36], wre_d.rearrange("(c p) e -> p c e", p=128), [], ["wr"], "cst2")
        P.dma("pool", br[:, 0:4], brg_d.partition_broadcast(128), [], ["br"], "cst2")
        P.dma("pool", br[:, 4:36], bre_d.partition_broadcast(128), [], ["br"], "cst2")
        P.dma("pool", ident[:], ident_d, [], ["ident"], "cst2")
        P.dma("pool", tri[:], tri_d, [], ["tri"], "cst2")
        P.add("dve", lambda e: e.memset(base[:], 0.0), [], ["base"])

        for i in range(NT):
            s = i % 2
            n0 = i * 128
            kyt, kytb, kxt, kh1 = "yt%d" % s, "ytb%d" % s, "xt%d" % s, "h1_%d" % s
            P.dma("sp", yt[s][:], yT_d[:, n0:n0 + 128].rearrange("(c p) n -> p c n", p=128), [], [kyt], kyt)
            P.dma("sp", xt[s][:], x_d[n0:n0 + 128, :], [], [kxt], kxt)
            P.add("act", lambda e, s=s: e.activation(ytb[s][:], yt[s][:], AF.Copy), [kyt], [kytb])
            for hh in range(2):
                for c in range(8):
                    P.mm(psA[hh][:], ytb[s][:, c, :], wout_b[:, c, hh * 512:(hh + 1) * 512], c == 0, c == 7,
                         [kytb, "wout_b"], ["psA%d" % hh])
                P.add("dve", lambda e, s=s, hh=hh: e.scalar_tensor_tensor(
                    u[:, hh * 512:(hh + 1) * 512], xt[s][:, hh * 512:(hh + 1) * 512], ALPHA, psA[hh][:],
                    ALU.mult, ALU.add), [kxt, "psA%d" % hh], ["L1u"])
            P.add("dve", lambda e: e.bn_stats(st[:, 0, :], u[:, 0:512]), ["L1u"], ["L1st0"])
            P.add("dve", lambda e: e.bn_stats(st[:, 1, :], u[:, 512:1024]), ["L1u"], ["L1st1"])
            P.add("dve", lambda e: e.bn_aggr(mv[:], st[:].rearrange("p a b -> p (a b)")), ["L1st0", "L1st1"], ["L1mv"])
            P.add("act", lambda e: e.activation(sd[:], mv[:, 1:2], AF.Sqrt, bias=LN_EPS, scale=1.0), ["L1mv"], ["L1sd"])
            P.add("dve", lambda e: e.reciprocal(rs[:], sd[:]), ["L1sd"], ["L1rs"])
            P.add("dve", lambda e, s=s: e.tensor_scalar(h1[s][:], u[:], mv[:, 0:1], rs[:, 0:1], ALU.subtract, ALU.mult),
                  ["L1u", "L1mv", "L1rs"], [kh1])
            P.add("pool", lambda e, s=s: e.tensor_tensor(h1[s][:], h1[s][:], g_t[:], ALU.mult), [kh1, "L1g"], [kh1])
            P.add("pool", lambda e, s=s: e.tensor_tensor(h1[s][:], h1[s][:], b_t[:], ALU.add), [kh1, "L1b"], [kh1])
            P.dma("sp", h1_d[n0:n0 + 128, :], h1[s][:], [kh1], [], "st" + kh1)
            for c in range(8):
                P.tr(psT[c // 4][:, (c % 4) * 128:(c % 4 + 1) * 128], h1[s][:, c * 128:(c + 1) * 128], ident[:],
                     [kh1, "ident"], ["psT%d" % (c // 4)])
            for hh in range(2):
                P.add("act", lambda e, hh=hh: e.activation(
                    h1T[:, hh * 4:(hh + 1) * 4, :], psT[hh][:].rearrange("p (c n) -> p c n", c=4), AF.Copy),
                    ["psT%d" % hh], ["h1T"])
            for c in range(8):
                P.mm(psL[:, 0:36], h1T[:, c, :], wr[:, c, :], c == 0, c == 7, ["h1T", "wr"], ["psL"])
            P.add("dve", lambda e: e.tensor_tensor(lg[:], psL[:, 0:36], br[:], ALU.add), ["psL", "br"], ["lg"])
            P.add("dve", lambda e: e.reduce_max(sm[:, 0:1], lg[:, 0:4], AX.X), ["lg"], ["sm0"])
            P.add("dve", lambda e: e.tensor_scalar(ohg[:], lg[:, 0:4], sm[:, 0:1], None, ALU.is_equal), ["lg", "sm0"], ["ohg"])
            P.add("dve", lambda e: e.tensor_scalar(sm[:, 1:2], sm[:, 0:1], -1.0, None, ALU.mult), ["sm0"], ["sm1"])
            P.add("act", lambda e: e.activation(eg[:], lg[:, 0:4], AF.Exp, bias=sm[:, 1:2], scale=1.0, accum_out=sm[:, 2:3]),
                  ["lg", "sm1"], ["sm2", "eg"])
            P.add("dve", lambda e: e.reciprocal(sm[:, 3:4], sm[:, 2:3]), ["sm2"], ["sm3"])
            P.add("dve", lambda e: e.tensor_scalar(le[:], lg[:, 4:12], ohg[:, 0:1], None, ALU.mult), ["lg", "ohg"], ["le"])
            for g in range(1, 4):
                P.add("dve", lambda e, g=g: e.scalar_tensor_tensor(le[:], lg[:, 4 + 8 * g:12 + 8 * g], ohg[:, g:g + 1], le[:],
                                                                   ALU.mult, ALU.add), ["lg", "ohg", "le"], ["le"])
            P.add("dve", lambda e: e.max(top8[:], le[:]), ["le"], ["top8"])
            P.add("dve", lambda e: e.tensor_scalar(m12[:, 0, :], le[:], top8[:, 0:1], None, ALU.is_equal), ["le", "top8"], ["m1"])
            P.add("dve", lambda e: e.tensor_scalar(m12[:, 1, :], le[:], top8[:, 1:2], None, ALU.is_equal), ["le", "top8"], ["m2"])
            P.add("dve", lambda e: e.tensor_tensor(sm[:, 4:5], top8[:, 1:2], top8[:, 0:1], ALU.subtract), ["top8"], ["sm4"])
            P.add("act", lambda e: e.activation(sm[:, 5:6], sm[:, 4:5], AF.Exp), ["sm4"], ["sm5"])
            P.add("dve", lambda e: e.tensor_scalar(sm[:, 6:7], sm[:, 5:6], 1.0, None, ALU.add), ["sm5"], ["sm6"])
            P.add("dve", lambda e: e.reciprocal(sm[:, 7:8], sm[:, 6:7]), ["sm6"], ["sm7"])
            P.add("dve", lambda e, i=i: e.tensor_tensor(gates[:, i, 0:1], sm[:, 7:8], sm[:, 3:4], ALU.mult), ["sm7", "sm3"], ["gates"])
            P.add("dve", lambda e, i=i: e.tensor_tensor(gates[:, i, 1:2], gates[:, i, 0:1], sm[:, 5:6], ALU.mult), ["gates", "sm5"], ["gates"])
            for j in range(2):
                for g in range(4):
                    P.add("dve", lambda e, i=i, j=j, g=g: e.tensor_scalar(
                        OH[:, i, j, 8 * g:8 * g + 8], m12[:, j, :], ohg[:, g:g + 1], None, ALU.mult),
                        ["m1", "m2", "ohg"], ["OH"])
            P.mm(psR[:, 0:32], tri[:, 0:128], OH[:, i, 0, :], True, True, ["tri", "OH"], ["psR"])
            P.mm(psR[:, 32:64], tri[:, 128:256], OH[:, i, 0, :], True, True, ["tri", "OH"], ["psR"])
            P.mm(psR[:, 64:96], tri[:, 0:128], OH[:, i, 1, :], True, True, ["tri", "OH"], ["psR"])
            P.mm(psR[:, 96:128], tri[:, 128:256], OH[:, i, 1, :], True, True, ["tri", "OH"], ["psR"])
            for j in range(2):
                P.add("dve", lambda e, j=j: e.tensor_tensor(t12[:], base[:], psR[:, 64 * j:64 * j + 32], ALU.add),
                      ["base", "psR"], ["t12"])
                P.add("dve", lambda e, i=i, j=j: e.tensor_tensor(junk[:], OH[:, i, j, :], t12[:], ALU.mult),
                      ["OH", "t12"], ["junk"])
                P.add("dve", lambda e, i=i, j=j: e.reduce_sum(rank[:, i, j:j + 1], junk[:], AX.X), ["junk"], ["rank"])
                P.add("dve", lambda e, j=j: e.tensor_tensor(base[:], base[:], psR[:, 64 * j + 32:64 * j + 64], ALU.add),
                      ["base", "psR"], ["base"])
    P.barrier()


def phase_t2(P, nc, N, h1_d, xbuf_d, thr_d, iop_d, R):
    NT = N // 128
    NBLK = (2 * N) // BLK + NEXP
    OH, rank, base = R["OH"], R["rank"], R["base"]
    dest_i, widx = R["dest_i"], R["widx"]
    sh = BLK.bit_length() - 1
    with ExitStack() as es:
        ci = SB(es, nc, "ci", [128, 32], I32); padded = SB(es, nc, "padded", [128, 32])
        ones = SB(es, nc, "ones32", [128, 32]); ends = SB(es, nc, "ends", [128, 32]); start = SB(es, nc, "start", [128, 32])
        tmp = SB(es, nc, "tmpd", [128, NT * 2, 32]); dsum = SB(es, nc, "dsum", [128, NT * 2])
        thr = SB(es, nc, "thr", [128, NBLK]); iop = SB(es, nc, "iop", [128, 1])
        cmp = SB(es, nc, "cmp", [128, NBLK, 32]); be = SB(es, nc, "be", [128, NBLK])
        hx = [SB(es, nc, "hx%d" % i, [128, 1024]) for i in range(2)]
        P.dma("sp", thr[:], thr_d.partition_broadcast(128), [], ["thr"], "c2a")
        P.dma("sp", iop[:], iop_d, [], ["iop"], "c2a")
        P.add("dve", lambda e: e.tensor_copy(ci[:], base[:]), ["base"], ["ci"])
        P.add("dve", lambda e: e.tensor_scalar(ci[:], ci[:], BLK - 1, None, ALU.add), ["ci"], ["ci"])
        P.add("dve", lambda e: e.tensor_scalar(ci[:], ci[:], sh, None, ALU.arith_shift_right), ["ci"], ["ci"])
        P.add("dve", lambda e: e.tensor_scalar(ci[:], ci[:], sh, None, ALU.logical_shift_left), ["ci"], ["ci"])
        P.add("dve", lambda e: e.tensor_copy(padded[:], ci[:]), ["ci"], ["padded"])
        P.add("dve", lambda e: e.memset(ones[:], 1.0), [], ["ones32"])
        P.add("dve", lambda e: e.tensor_tensor_scan(ends[:], ones[:], padded[:], 0.0, ALU.mult, ALU.add),
              ["ones32", "padded"], ["ends"])
        P.add("dve", lambda e: e.tensor_tensor(start[:], ends[:], padded[:], ALU.subtract), ["ends", "padded"], ["start"])
        P.add("dve", lambda e: e.tensor_tensor(
            tmp[:], OH[:].rearrange("p i j e -> p (i j) e"), start[:].unsqueeze(1).to_broadcast([128, NT * 2, 32]), ALU.mult),
            ["OH", "start"], ["tmpd"])
        P.add("dve", lambda e: e.reduce_sum(dsum[:], tmp[:], AX.X), ["tmpd"], ["dsum"])
        P.add("dve", lambda e: e.tensor_tensor(dsum[:], dsum[:], rank[:].rearrange("p i j -> p (i j)"), ALU.add),
              ["dsum", "rank"], ["dsum"])
        P.add("dve", lambda e: e.tensor_copy(dest_i[:], dsum[:]), ["dsum"], ["dest_i"])
        P.add("dve", lambda e: e.tensor_tensor(
            cmp[:], ends[:].unsqueeze(1).to_broadcast([128, NBLK, 32]), thr[:].unsqueeze(2).to_broadcast([128, NBLK, 32]),
            ALU.is_le), ["ends", "thr"], ["cmp"])
        P.add("dve", lambda e: e.reduce_sum(be[:], cmp[:], AX.X), ["cmp"], ["be"])
        P.add("dve", lambda e: e.tensor_scalar(be[:], be[:], float(NEXP - 1), 128.0, ALU.min, ALU.mult), ["be"], ["be"])
        P.add("dve", lambda e: e.tensor_scalar(be[:], be[:], iop[:, 0:1], None, ALU.add), ["be", "iop"], ["be"])
        P.add("dve", lambda e: e.tensor_copy(widx[:], be[:]), ["be"], ["widx"])
        for i in range(NT):
            s = i % 2
            k = "hx%d" % s
            P.dma("sp", hx[s][:], h1_d[i * 128:(i + 1) * 128, :], [], [k], k)
            for j in range(2):
                P.add("pool", lambda e, i=i, j=j, s=s: e.indirect_dma_start(
                    xbuf_d, bass.IndirectOffsetOnAxis(dest_i[:, 2 * i + j:2 * i + j + 1], 0), hx[s][:], None),
                    [k, "dest_i"], [], dma=True, semkey="sc%d_%d" % (s, j))
    P.barrier()


def phase_t3(P, nc, N, xbuf_d, ybuf_d, w1_d, w3_d, w2_d, ident_d, R):
    NBLK = (2 * N) // BLK + NEXP
    widx = R["widx"]
    NS = BLK // 128
    with ExitStack() as es:
        ident = SB(es, nc, "ident3", [128, 128])
        ws = [SB(es, nc, "ws%d" % i, [128, 4096]) for i in range(3)]
        w1b = [SB(es, nc, "w1b%d" % i, [128, 8, 4, 128], BF16) for i in range(2)]
        w3b = [SB(es, nc, "w3b%d" % i, [128, 8, 4, 128], BF16) for i in range(2)]
        w2b = [SB(es, nc, "w2b%d" % i, [128, 4, 1024], BF16) for i in range(2)]
        xb = SB(es, nc, "xb", [128, NS, 1024])
        xbT = SB(es, nc, "xbT", [128, 8, BLK], BF16)
        a1 = SB(es, nc, "a1", [128, BLK])
        hact = SB(es, nc, "hact", [128, 4, BLK], BF16)
        ysb = [SB(es, nc, "ysb%d" % i, [128, 1024]) for i in range(2)]
        psT = [PS(es, nc, "ps3T%d" % i, [128, 512]) for i in range(2)]
        psH1 = PS(es, nc, "psH1", [128, 512]); psH3 = PS(es, nc, "psH3", [128, 512])
        psY = [PS(es, nc, "psY%d" % i, [128, 512]) for i in range(2)]
        P.dma("sp", ident[:], ident_d, [], ["ident3"], "c3")
        for b in range(NBLK):
            s = b % 2
            for wi, wd in enumerate((w1_d, w3_d, w2_d)):
                P.add("pool", lambda e, wi=wi, wd=wd, b=b: e.indirect_dma_start(
                    ws[wi][:], None, wd, bass.IndirectOffsetOnAxis(widx[:, b:b + 1], 0)),
                    ["widx"], ["ws%d" % wi], dma=True, semkey="ws%d" % wi)
            P.add("act", lambda e, s=s: e.activation(
                w1b[s][:], ws[0][:].rearrange("p (c q h) -> p c h q", c=8, q=128, h=4), AF.Copy), ["ws0"], ["w1b%d" % s])
            P.add("pool", lambda e, s=s: e.tensor_copy(
                w3b[s][:], ws[1][:].rearrange("p (c q h) -> p c h q", c=8, q=128, h=4)), ["ws1"], ["w3b%d" % s])
            P.add("dve", lambda e, s=s: e.tensor_copy(
                w2b[s][:], ws[2][:].rearrange("p (c n) -> p c n", c=4)), ["ws2"], ["w2b%d" % s])
            P.dma("sp", xb[:], xbuf_d[b * BLK:(b + 1) * BLK, :].rearrange("(s p) d -> p s d", p=128), [], ["xb"], "xb")
            for sub in range(NS):
                for c in range(8):
                    P.tr(psT[c // 4][:, (c % 4) * 128:(c % 4 + 1) * 128],
                         xb[:, sub, :].rearrange("p (q c) -> p c q", c=8)[:, c, :], ident[:],
                         ["xb", "ident3"], ["ps3T%d" % (c // 4)])
                for hh in range(2):
                    eng = "act" if hh == 0 else "dve"
                    if eng == "act":
                        P.add("act", lambda e, hh=hh, sub=sub: e.activation(
                            xbT[:, hh * 4:(hh + 1) * 4, sub * 128:(sub + 1) * 128],
                            psT[hh][:].rearrange("p (c n) -> p c n", c=4), AF.Copy), ["ps3T%d" % hh], ["xbT"])
                    else:
                        P.add("dve", lambda e, hh=hh, sub=sub: e.tensor_copy(
                            xbT[:, hh * 4:(hh + 1) * 4, sub * 128:(sub + 1) * 128],
                            psT[hh][:].rearrange("p (c n) -> p c n", c=4)), ["ps3T%d" % hh], ["xbT"])
            for hc in range(4):
                for c in range(8):
                    P.mm(psH1[:, 0:BLK], w1b[s][:, c, hc, :], xbT[:, c, :], c == 0, c == 7, ["w1b%d" % s, "xbT"], ["psH1"])
                for c in range(8):
                    P.mm(psH3[:, 0:BLK], w3b[s][:, c, hc, :], xbT[:, c, :], c == 0, c == 7, ["w3b%d" % s, "xbT"], ["psH3"])
                P.add("act", lambda e: e.activation(a1[:], psH1[:, 0:BLK], AF.Silu), ["psH1"], ["a1"])
                P.add("dve", lambda e, hc=hc: e.tensor_tensor(hact[:, hc, :], a1[:], psH3[:, 0:BLK], ALU.mult),
                      ["a1", "psH3"], ["hact"])
            for sub in range(NS):
                ys = sub % 2
                for hh in range(2):
                    for hc in range(4):
                        P.mm(psY[hh][:], hact[:, hc, sub * 128:(sub + 1) * 128], w2b[s][:, hc, hh * 512:(hh + 1) * 512],
                             hc == 0, hc == 3, ["hact", "w2b%d" % s], ["psY%d" % hh])
                P.add("act", lambda e, ys=ys: e.activation(ysb[ys][:, 0:512], psY[0][:], AF.Copy), ["psY0"], ["ysb%d" % ys])
                P.add("dve", lambda e, ys=ys: e.tensor_copy(ysb[ys][:, 512:1024], psY[1][:]), ["psY1"], ["ysb%d" % ys])
                r0 = b * BLK + sub * 128
                P.dma("sp", ybuf_d[r0:r0 + 128, :], ysb[ys][:], ["ysb%d" % ys], [], "stysb%d" % ys)
    P.barrier()


def phase_t4(P, nc, N, h1_d, ybuf_d, p_d, lng_d, lnb_d, wg_d, wp_d, ident_d, out_d, R):
    NT = N // 128
    dest_i, gates = R["dest_i"], R["gates"]
    with ExitStack() as es:
        wg_b = SB(es, nc, "wg_b", [128, 8, 1024], BF16); wp_b = SB(es, nc, "wp_b", [128, 2, 1024], BF16)
        wst = SB(es, nc, "wst4", [128, 8, 512], F32)
        g_t = SB(es, nc, "g_t4", [128, 1024]); b_t = SB(es, nc, "b_t4", [128, 1024])
        ident = SB(es, nc, "ident4", [128, 128])
        y1 = [SB(es, nc, "y1_%d" % i, [128, 1024]) for i in range(2)]
        y2 = [SB(es, nc, "y2_%d" % i, [128, 1024]) for i in range(2)]
        hh1 = [SB(es, nc, "hh1_%d" % i, [128, 1024]) for i in range(2)]
        pt = [SB(es, nc, "pt%d" % i, [128, 256]) for i in range(2)]
        u = SB(es, nc, "u4", [128, 1024]); h2 = SB(es, nc, "h2", [128, 1024])
        h2T = SB(es, nc, "h2T", [128, 8, 128], BF16); pT = SB(es, nc, "pT", [128, 2, 128], BF16)
        sg = SB(es, nc, "sg", [128, 1024]); ot = [SB(es, nc, "ot%d" % i, [128, 1024]) for i in range(2)]
        st = SB(es, nc, "st4", [128, 2, 6]); mv = SB(es, nc, "mv4", [128, 2])
        sd = SB(es, nc, "sd4", [128, 1]); rs = SB(es, nc, "rs4", [128, 1])
        psT = [PS(es, nc, "ps4T%d" % i, [128, 512]) for i in range(2)]
        psG = [PS(es, nc, "psG%d" % i, [128, 512]) for i in range(2)]
        psP = [PS(es, nc, "psP%d" % i, [128, 512]) for i in range(2)]
        psPT = PS(es, nc, "psPT", [128, 512])
        for hh in range(2):
            P.dma("sp", wst[:], wg_d[:, hh * 512:(hh + 1) * 512].rearrange("(c p) n -> p c n", p=128), [], ["wst4"], "wst4")
            P.add("act", lambda e, hh=hh: e.activation(wg_b[:, :, hh * 512:(hh + 1) * 512], wst[:], AF.Copy), ["wst4"], ["wg_b"])
        for hh in range(2):
            P.dma("sp", wst[:, 0:2, :], wp_d[:, hh * 512:(hh + 1) * 512].rearrange("(c p) n -> p c n", p=128), [], ["wst4"], "wst4")
            P.add("act", lambda e, hh=hh: e.activation(wp_b[:, :, hh * 512:(hh + 1) * 512], wst[:, 0:2, :], AF.Copy), ["wst4"], ["wp_b"])
        P.dma("sp", g_t[:], lng_d.partition_broadcast(128), [], ["L2g"], "c4")
        P.dma("sp", b_t[:], lnb_d.partition_broadcast(128), [], ["L2b"], "c4")
        P.dma("sp", ident[:], ident_d, [], ["ident4"], "c4")
        for i in range(NT):
            s = i % 2
            n0 = i * 128
            ky1, ky2, kh, kp, ko = "y1_%d" % s, "y2_%d" % s, "hh1_%d" % s, "pt%d" % s, "ot%d" % s
            P.add("pool", lambda e, i=i, s=s: e.indirect_dma_start(
                y1[s][:], None, ybuf_d, bass.IndirectOffsetOnAxis(dest_i[:, 2 * i:2 * i + 1], 0)),
                ["dest_i"], [ky1], dma=True, semkey=ky1)
            P.add("pool", lambda e, i=i, s=s: e.indirect_dma_start(
                y2[s][:], None, ybuf_d, bass.IndirectOffsetOnAxis(dest_i[:, 2 * i + 1:2 * i + 2], 0)),
                ["dest_i"], [ky2], dma=True, semkey=ky2)
            P.dma("sp", hh1[s][:], h1_d[n0:n0 + 128, :], [], [kh], kh)
            P.dma("sp", pt[s][:], p_d[n0:n0 + 128, :], [], [kp], kp)
            P.add("dve", lambda e, i=i, s=s: e.tensor_scalar(y1[s][:], y1[s][:], gates[:, i, 0:1], None, ALU.mult),
                  [ky1, "gates"], [ky1])
            P.add("dve", lambda e, i=i, s=s: e.scalar_tensor_tensor(y1[s][:], y2[s][:], gates[:, i, 1:2], y1[s][:], ALU.mult, ALU.add),
                  [ky1, ky2, "gates"], [ky1])
            P.add("dve", lambda e, s=s: e.scalar_tensor_tensor(u[:], hh1[s][:], ALPHA, y1[s][:], ALU.mult, ALU.add),
                  [kh, ky1], ["L2u"])
            P.add("dve", lambda e: e.bn_stats(st[:, 0, :], u[:, 0:512]), ["L2u"], ["L2st0"])
            P.add("dve", lambda e: e.bn_stats(st[:, 1, :], u[:, 512:1024]), ["L2u"], ["L2st1"])
            P.add("dve", lambda e: e.bn_aggr(mv[:], st[:].rearrange("p a b -> p (a b)")), ["L2st0", "L2st1"], ["L2mv"])
            P.add("act", lambda e: e.activation(sd[:], mv[:, 1:2], AF.Sqrt, bias=LN_EPS, scale=1.0), ["L2mv"], ["L2sd"])
            P.add("dve", lambda e: e.reciprocal(rs[:], sd[:]), ["L2sd"], ["L2rs"])
            P.add("dve", lambda e: e.tensor_scalar(h2[:], u[:], mv[:, 0:1], rs[:, 0:1], ALU.subtract, ALU.mult),
                  ["L2u", "L2mv", "L2rs"], ["h2"])
            P.add("pool", lambda e: e.tensor_tensor(h2[:], h2[:], g_t[:], ALU.mult), ["h2", "L2g"], ["h2"])
            P.add("pool", lambda e: e.tensor_tensor(h2[:], h2[:], b_t[:], ALU.add), ["h2", "L2b"], ["h2"])
            for c in range(8):
                P.tr(psT[c // 4][:, (c % 4) * 128:(c % 4 + 1) * 128], h2[:, c * 128:(c + 1) * 128], ident[:],
                     ["h2", "ident4"], ["ps4T%d" % (c // 4)])
            for hh in range(2):
                P.add("act", lambda e, hh=hh: e.activation(
                    h2T[:, hh * 4:(hh + 1) * 4, :], psT[hh][:].rearrange("p (c n) -> p c n", c=4), AF.Copy),
                    ["ps4T%d" % hh], ["h2T"])
            for c in range(2):
                P.tr(psPT[:, c * 128:(c + 1) * 128], pt[s][:, c * 128:(c + 1) * 128], ident[:], [kp, "ident4"], ["psPT"])
            P.add("dve", lambda e: e.tensor_copy(pT[:], psPT[:, 0:256].rearrange("p (c n) -> p c n", c=2)), ["psPT"], ["pT"])
            for hh in range(2):
                for c in range(8):
                    P.mm(psG[hh][:], h2T[:, c, :], wg_b[:, c, hh * 512:(hh + 1) * 512], c == 0, c == 7, ["h2T", "wg_b"], ["psG%d" % hh])
                for c in range(2):
                    P.mm(psP[hh][:], pT[:, c, :], wp_b[:, c, hh * 512:(hh + 1) * 512], c == 0, c == 1, ["pT", "wp_b"], ["psP%d" % hh])
                P.add("act", lambda e, hh=hh: e.activation(sg[:, hh * 512:(hh + 1) * 512], psG[hh][:], AF.Sigmoid), ["psG%d" % hh], ["sg"])
                P.add("dve", lambda e, hh=hh: e.tensor_tensor(sg[:, hh * 512:(hh + 1) * 512], sg[:, hh * 512:(hh + 1) * 512], psP[hh][:], ALU.mult),
                      ["sg", "psP%d" % hh], ["sg"])
            P.add("pool", lambda e, s=s: e.tensor_tensor(ot[s][:], sg[:], h2[:], ALU.add), ["sg", "h2"], [ko])
            P.dma("sp", out_d[n0:n0 + 128, :], ot[s][:], [ko], [], "st" + ko)
    P.barrier()


CH = 64
STAGE = int(os.environ.get('STAGE', '9'))


def gemm_fm(P, nc, N, x_d, w_d, C, out_d, ident_d, tag):
    NCC = (C + 127) // 128
    with ExitStack() as es:
        wb = SB(es, nc, tag + "wb", [128, 8, C], BF16)
        wst = [SB(es, nc, tag + "wst%d" % i, [128, 8, 512]) for i in range(2)]
        ident = SB(es, nc, tag + "id", [128, 128])
        xt = SB(es, nc, tag + "xt", [128, 4, 1024])
        xT = SB(es, nc, tag + "xT", [128, 8, 512], BF16)
        ot = [SB(es, nc, tag + "ot%d" % i, [128, 512]) for i in range(3)]
        psT = [PS(es, nc, tag + "psT%d" % i, [128, 512]) for i in range(2)]
        psO = [PS(es, nc, tag + "psO%d" % i, [128, 512]) for i in range(2)]
        P.dma("sp", ident[:], ident_d, [], [tag + "id"], tag + "id")
        npc = (C + 511) // 512
        for pc in range(npc):
            s = pc % 2
            c0 = pc * 512
            cw = min(512, C - c0)
            P.dma("sp" if s == 0 else "pool", wst[s][:, :, 0:cw], w_d[:, c0:c0 + cw].rearrange("(c p) n -> p c n", p=128),
                  [], [tag + "wst%d" % s], tag + "wst%d" % s)
            if s == 0:
                P.add("act", lambda e, s=s, c0=c0, cw=cw: e.activation(wb[:, :, c0:c0 + cw], wst[s][:, :, 0:cw], AF.Copy),
                      [tag + "wst%d" % s], [tag + "wb"])
            else:
                P.add("dve", lambda e, s=s, c0=c0, cw=cw: e.tensor_copy(wb[:, :, c0:c0 + cw], wst[s][:, :, 0:cw]),
                      [tag + "wst%d" % s], [tag + "wb"])
        k = 0
        for tt in range(N // 512):
            n0 = tt * 512
            P.dma("sp", xt[:], x_d[n0:n0 + 512, :].rearrange("(s p) d -> p s d", p=128), [], [tag + "xt"], tag + "xt")
            for c in range(8):
                b = c % 2
                for sub in range(4):
                    P.tr(psT[b][:, sub * 128:(sub + 1) * 128], xt[:, sub, c * 128:(c + 1) * 128], ident[:],
                         [tag + "xt", tag + "id"], [tag + "psT%d" % b])
                if b == 0:
                    P.add("act", lambda e, c=c, b=b: e.activation(xT[:, c, :], psT[b][:], AF.Copy), [tag + "psT%d" % b], [tag + "xT"])
                else:
                    P.add("dve", lambda e, c=c, b=b: e.tensor_copy(xT[:, c, :], psT[b][:]), [tag + "psT%d" % b], [tag + "xT"])
            for cc in range(NCC):
                M = min(128, C - cc * 128)
                b = cc % 2
                o = k % 3
                k += 1
                for c in range(8):
                    P.mm(psO[b][0:M, :], wb[:, c, cc * 128:cc * 128 + M], xT[:, c, :], c == 0, c == 7,
                         [tag + "wb", tag + "xT"], [tag + "psO%d" % b])
                if b == 0:
                    P.add("act", lambda e, b=b, o=o, M=M: e.activation(ot[o][0:M, :], psO[b][0:M, :], AF.Copy),
                          [tag + "psO%d" % b], [tag + "ot%d" % o])
                else:
                    P.add("dve", lambda e, b=b, o=o, M=M: e.tensor_copy(ot[o][0:M, :], psO[b][0:M, :]),
                          [tag + "psO%d" % b], [tag + "ot%d" % o])
                P.dma("sp" if cc % 2 == 0 else "pool", out_d[cc * 128:cc * 128 + M, n0:n0 + 512], ot[o][0:M, :],
                      [tag + "ot%d" % o], [], tag + "sto%d" % o)
    P.barrier()


def rwkv_prep(P, nc, NB, T, zT_d, prm, S):
    TW = 512
    NW = T // TW
    NCW = TW // CH
    C1 = -math.exp(-0.5)
    with ExitStack() as es:
        col = SB(es, nc, "rp_col", [128, 4, 8])
        mul = SB(es, nc, "rp_mul", [128, 2])
        w2 = SB(es, nc, "rp_w2", [128, 512])
        g2 = SB(es, nc, "rp_g2", [128, 512])
        blk1 = SB(es, nc, "rp_blk1", [128, 128]); ident = SB(es, nc, "rp_id", [128, 128])
        rmask = SB(es, nc, "rp_rmask", [128, TW])
        lo = [SB(es, nc, "rp_lo%d" % i, [128, TW + 1]) for i in range(2)]
        lm = [SB(es, nc, "rp_lm%d" % i, [128, TW]) for i in range(2)]
        raw = [SB(es, nc, "rp_raw%d" % i, [128, TW + 1]) for i in range(3)]
        r = SB(es, nc, "rp_r", [128, TW]); k = SB(es, nc, "rp_k", [128, TW]); v = SB(es, nc, "rp_v", [128, TW])
        ld = SB(es, nc, "rp_ld", [128, TW]); a = SB(es, nc, "rp_a", [128, TW]); g = SB(es, nc, "rp_g", [128, TW])
        kk = SB(es, nc, "rp_kk", [128, TW]); t1 = SB(es, nc, "rp_t1", [128, TW]); t2 = SB(es, nc, "rp_t2", [128, TW])
        km = SB(es, nc, "rp_km", [128, TW]); be = SB(es, nc, "rp_be", [128, TW])
        c = SB(es, nc, "rp_c", [128, TW]); ep = SB(es, nc, "rp_ep", [128, TW]); en = SB(es, nc, "rp_en", [128, TW])
        ex = SB(es, nc, "rp_ex", [128, TW])
        o_rt = SB(es, nc, "rp_ort", [128, TW]); o_kt = SB(es, nc, "rp_okt", [128, TW]); o_kp = SB(es, nc, "rp_okp", [128, TW])
        o_bt = SB(es, nc, "rp_obt", [128, TW]); o_ktl = SB(es, nc, "rp_oktl", [128, TW]); o_btl = SB(es, nc, "rp_obtl", [128, TW])
        o_bon = SB(es, nc, "rp_obon", [128, TW]); o_pc = SB(es, nc, "rp_opc", [128, NCW])
        vtk = SB(es, nc, "rp_vtk", [128, 4, 128])
        ps1 = PS(es, nc, "rp_ps1", [128, 512]); ps2 = PS(es, nc, "rp_ps2", [128, 512]); ps3 = PS(es, nc, "rp_ps3", [128, 512])
        ps4 = PS(es, nc, "rp_ps4", [128, 512]); ps5 = PS(es, nc, "rp_ps5", [128, 512]); ps6 = PS(es, nc, "rp_ps6", [128, 512])

        def colload(j, src, fcmul=True):
            P.dma("pool", col[:, :, j:j + 1], src.rearrange("(f p o) -> p f o", p=128, o=1), [], ["rp_col"], "rp_c0", allow_slow_non_contiguous=True)
        colload(0, prm["mu"][0:512]); colload(1, prm["mu"][512:1024]); colload(2, prm["mu"][1024:1536])
        colload(3, prm["w0"]); colload(4, prm["a0"]); colload(5, prm["k_k"]); colload(6, prm["k_a"]); colload(7, prm["r_k"])
        P.dma("pool", mul[:], prm["mu"][1536:1792].rearrange("(f p) -> p f", p=128), [], ["rp_mul"], "rp_c0", allow_slow_non_contiguous=True)
        P.dma("pool", w2[0:64, :], prm["w2"], [], ["rp_w2"], "rp_c0")
        P.dma("pool", w2[64:128, :], prm["a2"], [], ["rp_w2"], "rp_c0")
        P.dma("pool", g2[:], prm["g2"], [], ["rp_g2"], "rp_c0")
        P.dma("pool", blk1[:], prm["blk1"], [], ["rp_blk1"], "rp_c0")
        P.dma("pool", ident[:], prm["ident"], [], ["rp_id"], "rp_c0")
        P.dma("pool", rmask[:], prm["rmask"].partition_broadcast(128), [], ["rp_rmask"], "rp_c0")

        def load_shift(dst, row0, n0, t0, key, q):
            if t0 == 0:
                P.add("pool", lambda e: e.memset(dst[:, 0:1], 0.0), [], [key])
                P.dma(q, dst[:, 1:TW + 1], zT_d[row0:row0 + 128, n0:n0 + TW], [], [key], key)
            else:
                P.dma(q, dst[:, :], zT_d[row0:row0 + 128, n0 - 1:n0 + TW], [], [key], key)

        def mix(dst, src, mu_ap, key_src, key_dst, eng="dve"):
            P.add("pool", lambda e: e.tensor_tensor(t2[:], src[:, 0:TW], src[:, 1:TW + 1], ALU.subtract), [key_src], ["rp_t2"])
            P.add("dve", lambda e: e.scalar_tensor_tensor(dst, t2[:], mu_ap, src[:, 1:TW + 1], ALU.mult, ALU.add),
                  ["rp_t2", key_src, "rp_col", "rp_mul"], [key_dst])

        for b in range(NB):
            for w in range(NW):
                t0 = w * TW
                n0 = b * T + t0
                for i in range(2):
                    load_shift(lo[i], 1536 + 128 * i, n0, t0, "rp_lo%d" % i, "sp")
                    mix(lm[i][:], lo[i], mul[:, i:i + 1], "rp_lo%d" % i, "rp_lm%d" % i)
                P.add("act", lambda e: e.activation(lm[0][0:64, :], lm[0][0:64, :], AF.Tanh), ["rp_lm0"], ["rp_lm0"])
                P.add("act", lambda e: e.activation(lm[1][:], lm[1][:], AF.Sigmoid), ["rp_lm1"], ["rp_lm1"])
                for fc in range(4):
                    for j, dst in enumerate((r, k, v)):
                        load_shift(raw[j], j * 512 + fc * 128, n0, t0, "rp_raw%d" % j, "sp")
                        mix(dst[:], raw[j], col[:, fc, j:j + 1], "rp_raw%d" % j, ("rp_r", "rp_k", "rp_v")[j])
                    fs = slice(fc * 128, (fc + 1) * 128)
                    P.mm(ps1[:, 0:TW], w2[0:64, fs], lm[0][0:64, :], True, True, ["rp_w2", "rp_lm0"], ["rp_ps1"])
                    P.add("act", lambda e, fc=fc: e.activation(ld[:], ps1[:, 0:TW], AF.Sigmoid, bias=col[:, fc, 3:4], scale=1.0),
                          ["rp_ps1", "rp_col"], ["rp_ld"])
                    P.add("pool", lambda e: e.tensor_scalar(ld[:], ld[:], C1, None, ALU.mult), ["rp_ld"], ["rp_ld"])
                    P.mm(ps2[:, 0:TW], w2[64:128, fs], lm[0][64:128, :], True, True, ["rp_w2", "rp_lm0"], ["rp_ps2"])
                    P.add("act", lambda e, fc=fc: e.activation(a[:], ps2[:, 0:TW], AF.Sigmoid, bias=col[:, fc, 4:5], scale=1.0),
                          ["rp_ps2", "rp_col"], ["rp_a"])
                    P.mm(ps3[:, 0:TW], g2[:, fs], lm[1][:], True, True, ["rp_g2", "rp_lm1"], ["rp_ps3"])
                    P.add("act", lambda e: e.activation(g[:], ps3[:, 0:TW], AF.Copy), ["rp_ps3"], ["rp_g"])
                    P.dma("pool", S["g"][fs, n0:n0 + TW], g[:], ["rp_g"], [], "rp_stg")
                    P.add("dve", lambda e, fc=fc: e.tensor_scalar(kk[:], k[:], col[:, fc, 5:6], None, ALU.mult), ["rp_k", "rp_col"], ["rp_kk"])
                    P.add("act", lambda e: e.activation(t1[:], kk[:], AF.Square), ["rp_kk"], ["rp_t1"])
                    P.mm(ps4[:, 0:TW], blk1[:], t1[:], True, True, ["rp_blk1", "rp_t1"], ["rp_ps4"])
                    P.add("act", lambda e: e.activation(t1[:], ps4[:, 0:TW], AF.Sqrt), ["rp_ps4"], ["rp_t1"])
                    P.add("dve", lambda e: e.tensor_scalar(t1[:], t1[:], 1e-12, None, ALU.max), ["rp_t1"], ["rp_t1"])
                    P.add("dve", lambda e: e.reciprocal(t1[:], t1[:]), ["rp_t1"], ["rp_t1"])
                    P.add("dve", lambda e: e.tensor_tensor(kk[:], kk[:], t1[:], ALU.mult), ["rp_kk", "rp_t1"], ["rp_kk"])
                    P.add("dve", lambda e, fc=fc: e.tensor_scalar(km[:], a[:], -1.0, col[:, fc, 6:7], ALU.add, ALU.mult), ["rp_a", "rp_col"], ["rp_km"])
                    P.add("dve", lambda e: e.scalar_tensor_tensor(km[:], km[:], 1.0, k[:], ALU.add, ALU.mult), ["rp_km", "rp_k"], ["rp_km"])
                    P.add("pool", lambda e: e.tensor_tensor(be[:], kk[:], a[:], ALU.mult), ["rp_kk", "rp_a"], ["rp_be"])
                    P.add("dve", lambda e: e.tensor_tensor_scan(c[:], rmask[:], ld[:], 0.0, ALU.mult, ALU.add), ["rp_rmask", "rp_ld"], ["rp_c"])
                    P.add("act", lambda e: e.activation(ep[:], c[:], AF.Exp), ["rp_c"], ["rp_ep"])
                    P.add("act", lambda e: e.activation(en[:], c[:], AF.Exp, scale=-1.0), ["rp_c"], ["rp_en"])
                    P.add("pool", lambda e: e.tensor_tensor(t2[:], c[:], ld[:], ALU.subtract), ["rp_c", "rp_ld"], ["rp_t2"])
                    P.add("act", lambda e: e.activation(ex[:], t2[:], AF.Exp), ["rp_t2"], ["rp_ex"])
                    P.add("dve", lambda e: e.tensor_tensor(o_rt[:], r[:], ep[:], ALU.mult), ["rp_r", "rp_ep"], ["rp_ort"])
                    P.add("pool", lambda e: e.tensor_tensor(o_kp[:], kk[:], ex[:], ALU.mult), ["rp_kk", "rp_ex"], ["rp_okp"])
                    P.add("dve", lambda e: e.tensor_tensor(o_kt[:], km[:], en[:], ALU.mult), ["rp_km", "rp_en"], ["rp_okt"])
                    P.add("pool", lambda e: e.tensor_tensor(o_bt[:], be[:], en[:], ALU.mult), ["rp_be", "rp_en"], ["rp_obt"])
                    P.add("dve", lambda e: e.tensor_copy(o_pc[:], ep[:].rearrange("p (n c) -> p n c", c=CH)[:, :, CH - 1]), ["rp_ep"], ["rp_opc"])
                    pcb = o_pc[:].unsqueeze(2).to_broadcast([128, NCW, CH])
                    P.add("dve", lambda e, pcb=pcb: e.tensor_tensor(o_ktl[:].rearrange("p (n c) -> p n c", c=CH),
                                                                     o_kt[:].rearrange("p (n c) -> p n c", c=CH), pcb, ALU.mult),
                          ["rp_okt", "rp_opc"], ["rp_oktl"])
                    P.add("dve", lambda e, pcb=pcb: e.scalar_tensor_tensor(o_btl[:].rearrange("p (n c) -> p n c", c=CH),
                                                                            o_bt[:].rearrange("p (n c) -> p n c", c=CH), -1.0, pcb,
                                                                            ALU.mult, ALU.mult),
                          ["rp_obt", "rp_opc"], ["rp_obtl"])
                    P.add("dve", lambda e, fc=fc: e.scalar_tensor_tensor(t1[:], r[:], col[:, fc, 7:8], km[:], ALU.mult, ALU.mult),
                          ["rp_r", "rp_km", "rp_col", "rp_t1"], ["rp_t1"])
                    P.mm(ps5[:, 0:TW], blk1[:], t1[:], True, True, ["rp_blk1", "rp_t1"], ["rp_ps5"])
                    P.add("dve", lambda e: e.tensor_tensor(o_bon[:], ps5[:, 0:TW], v[:], ALU.mult), ["rp_ps5", "rp_v"], ["rp_obon"])
                    for sub in range(TW // 128):
                        P.tr(ps6[:, sub * 128:(sub + 1) * 128], v[:, sub * 128:(sub + 1) * 128], ident[:], ["rp_v", "rp_id"], ["rp_ps6"])
                    P.add("act", lambda e: e.activation(vtk[:], ps6[:].rearrange("p (s f) -> p s f", f=128), AF.Copy), ["rp_ps6"], ["rp_vtk"])
                    P.dma("pool", S["vtok"][n0:n0 + TW, fs].rearrange("(s p) f -> p s f", p=128), vtk[:], ["rp_vtk"], [], "rp_stv")
                    for key, tl, dd in (("rp_ort", o_rt, "rt"), ("rp_okp", o_kp, "kp"), ("rp_okt", o_kt, "kt"), ("rp_obt", o_bt, "bt"),
                                        ("rp_oktl", o_ktl, "ktl"), ("rp_obtl", o_btl, "btl"), ("rp_obon", o_bon, "bon")):
                        P.dma("sp", S[dd][fs, n0:n0 + TW], tl[:], [key], [], "st" + key)
                    P.dma("sp", S["pc"][fs, n0 // CH:n0 // CH + NCW], o_pc[:], ["rp_opc"], [], "strp_opc")
    P.barrier()


def chunk_scan(P, nc, NB, T, D, delta, DK, DV, H, yT_d, yrow0, tag, gn_eps=0.0):
    TW = 256
    NCW = TW // CH
    NW = T // TW
    HV = H * DV
    names = ["rt", "kt", "ktl", "g"] + (["kp", "bt", "btl", "bon"] if delta else [])
    K = lambda s: tag + s
    with ExitStack() as es:
        A = {n: SB(es, nc, tag + "A" + n, [DK if n not in ("g", "bon") else DV, H, TW]) for n in names}
        vt = SB(es, nc, tag + "vt", [CH, NCW, HV])
        pcw = SB(es, nc, tag + "pcw", [DK, H, NCW])
        Hs = SB(es, nc, tag + "H", [DK, H, DV])
        masks = SB(es, nc, tag + "masks", [64, 320]); id64 = SB(es, nc, tag + "id64", [64, 64])
        ident = SB(es, nc, tag + "ident", [128, 128])
        am = SB(es, nc, tag + "am", [64, H, 320 if delta else 64])
        ktok = SB(es, nc, tag + "ktok", [64, H, 2 if delta else 1, DK])
        yw = SB(es, nc, tag + "yw", [DV, H, TW])
        on = SB(es, nc, tag + "on", [64, H, DV]); sq = SB(es, nc, tag + "sq", [64, H, DV]); oc = SB(es, nc, tag + "oc", [64, H, DV])
        s12 = SB(es, nc, tag + "s12", [64, 4, H])
        if delta:
            R = [SB(es, nc, tag + "R%d" % i, [64, H, 64]) for i in range(2)]
            pw = [SB(es, nc, tag + "pw%d" % i, [64, H, 2, 64]) for i in range(2)]
            rhs = SB(es, nc, tag + "rhs", [64, H, DV]); us = SB(es, nc, tag + "us", [64, H, DV])
            gng = SB(es, nc, tag + "gng", [DV, H]); gnb = SB(es, nc, tag + "gnb", [DV, H])
        else:
            normg = SB(es, nc, tag + "normg", [DV, 1])
        nbO = (H * DV * 4 + 2047) // 2048
        psA = [PS(es, nc, tag + "psA%d" % i, [128, 512]) for i in range(2)]
        psB = [PS(es, nc, tag + "psB%d" % i, [128, 512]) for i in range(2)]
        psC = PS(es, nc, tag + "psC", [128, 512])
        psD = PS(es, nc, tag + "psD", [128, 512])
        psO = PS(es, nc, tag + "psO", [128, 512])
        psT = PS(es, nc, tag + "psT", [128, 512])
        P.dma("pool", masks[:], D["masks"], [], [K("masks")], K("c0"))
        P.dma("pool", id64[:], D["id64"], [], [K("id64")], K("c0"))
        P.dma("pool", ident[:], D["ident"], [], [K("ident")], K("c0"))
        if delta:
            P.dma("pool", gng[:], D["gng"].rearrange("(h p) -> p h", p=DV), [], [K("gn")], K("c0"), allow_slow_non_contiguous=True)
            P.dma("pool", gnb[:], D["gnb"].rearrange("(h p) -> p h", p=DV), [], [K("gn")], K("c0"), allow_slow_non_contiguous=True)
        else:
            P.dma("pool", normg[:], D["normg"].rearrange("(p o) -> p o", o=1), [], [K("gn")], K("c0"))

        def psO_h(h):
            if DV == 64:
                return psO[0:64, h * 64:(h + 1) * 64]
            return (psB[h // 4])[0:64, (h % 4) * 128:(h % 4 + 1) * 128]

        def psH_h(h):
            if DV == 64:
                return psD[0:DK, h * 64:(h + 1) * 64]
            return (psC if h < 4 else psD)[0:DK, (h % 4) * 128:(h % 4 + 1) * 128]
        kO = [K("psO")] if DV == 64 else [K("psB0"), K("psB1")]
        kH = [K("psD")] if DV == 64 else [K("psC"), K("psD")]

        for b in range(NB):
            P.add("dve", lambda e: e.memset(Hs[:], 0.0), [], [K("H")])
            for w in range(NW):
                n0 = b * T + w * TW
                for i, n in enumerate(names):
                    p_ = DK if n not in ("g", "bon") else DV
                    P.dma("sp" if i % 2 == 0 else "act", A[n][:], D[n][:, n0:n0 + TW].rearrange("(h p) n -> p h n", p=p_),
                          [], [K("A" + n)], K("A" + n))
                P.dma("sp", vt[:], D["vtok"][n0:n0 + TW, :].rearrange("(c s) f -> s c f", s=CH), [], [K("vt")], K("vt"))
                P.dma("sp", pcw[:], D["pc"][:, n0 // CH:n0 // CH + NCW].rearrange("(h p) c -> p h c", p=DK), [], [K("pcw")], K("pcw"))
                for n in range(NCW):
                    cs = slice(n * CH, (n + 1) * CH)
                    for h in range(H):
                        pa = psA[h % 2]
                        kpa = K("psA%d" % (h % 2))
                        if delta:
                            pairs = (("bt", "kp"), ("kp", "bt"), ("kt", "kp"), ("kt", "rt"), ("bt", "rt"))
                        else:
                            pairs = (("kt", "rt"),)
                        for q, (l, r_) in enumerate(pairs):
                            P.mm(pa[0:64, q * 64:(q + 1) * 64], A[l][:, h, cs], A[r_][:, h, cs], True, True,
                                 [K("A" + l), K("A" + r_)], [kpa])
                        if delta:
                            P.add("dve", lambda e, h=h, pa=pa: e.tensor_tensor(am[:, h, :], pa[0:64, 0:320], masks[:], ALU.mult),
                                  [kpa, K("masks")], [K("am")])
                        else:
                            P.add("dve", lambda e, h=h, pa=pa: e.tensor_tensor(am[:, h, :], pa[0:64, 0:64], masks[:, 192:256], ALU.mult),
                                  [kpa, K("masks")], [K("am")])
                    if STAGE < 2: continue
                    nk = 2 if delta else 1
                    for h in range(H):
                        for q, nm in enumerate(("ktl", "btl")[:nk]):
                            if DK == 64:
                                dst = psA[(h * nk + q) // 8][0:64, ((h * nk + q) % 8) * 64:((h * nk + q) % 8 + 1) * 64]
                                kd = K("psA%d" % ((h * nk + q) // 8))
                            else:
                                dst = psA[h // 4][0:64, (h % 4) * 128:(h % 4 + 1) * 128]
                                kd = K("psA%d" % (h // 4))
                            P.tr(dst, A[nm][:, h, cs], ident[0:DK, 0:DK], [K("A" + nm), K("ident"), K("am")], [kd])
                    for half in range(2):
                        hs = slice(half * (H // 2), (half + 1) * (H // 2))
                        src = psA[half][0:64, 0:(H // 2) * nk * DK].rearrange("p (h q d) -> p h q d", q=nk, d=DK)
                        P.add("act", lambda e, hs=hs, src=src: e.activation(ktok[:, hs, :, :], src, AF.Copy),
                              [K("psA%d" % half)], [K("ktok")])
                    if STAGE < 3: continue
                    if delta:
                        P.add("dve", lambda e: e.tensor_tensor(R[0][:], am[:, :, 0:64], id64[:].unsqueeze(1).to_broadcast([64, H, 64]), ALU.add),
                              [K("am"), K("id64")], [K("R0")])
                        cur = 0
                        for j in range(1, 6):
                            src_p = (lambda h: am[:, h, 0:64]) if j == 1 else (lambda h, pwp=pw[(j - 1) % 2]: pwp[:, h, 0, :])
                            src_pt = (lambda h: am[:, h, 64:128]) if j == 1 else (lambda h, pwp=pw[(j - 1) % 2]: pwp[:, h, 1, :])
                            ksrc = K("am") if j == 1 else K("pw%d" % ((j - 1) % 2))
                            for h in range(H):
                                pb = psB[h // 4]
                                o0 = (h % 4) * 128
                                if j < 5:
                                    P.mm(pb[0:64, o0:o0 + 64], src_pt(h), src_p(h), True, True, [ksrc], [K("psB%d" % (h // 4))])
                                P.mm(pb[0:64, o0 + 64:o0 + 128], src_p(h), src_pt(h), True, True, [ksrc], [K("psB%d" % (h // 4))])
                            for half in range(2):
                                hs = slice(half * 4, half * 4 + 4)
                                eng = "act" if half == 0 else "dve"
                                srcv = psB[half][0:64, :].rearrange("p (h q d) -> p h q d", q=2, d=64)
                                if j < 5:
                                    if eng == "act":
                                        P.add("act", lambda e, hs=hs, srcv=srcv, j=j: e.activation(pw[j % 2][:, hs, :, :], srcv, AF.Copy),
                                              [K("psB%d" % half)], [K("pw%d" % (j % 2))])
                                    else:
                                        P.add("dve", lambda e, hs=hs, srcv=srcv, j=j: e.tensor_copy(pw[j % 2][:, hs, :, :], srcv),
                                              [K("psB%d" % half)], [K("pw%d" % (j % 2))])
                                else:
                                    if eng == "act":
                                        P.add("act", lambda e, hs=hs, srcv=srcv, j=j: e.activation(pw[j % 2][:, hs, 1, :], srcv[:, :, 1, :], AF.Copy),
                                              [K("psB%d" % half)], [K("pw%d" % (j % 2))])
                                    else:
                                        P.add("dve", lambda e, hs=hs, srcv=srcv, j=j: e.tensor_copy(pw[j % 2][:, hs, 1, :], srcv[:, :, 1, :]),
                                              [K("psB%d" % half)], [K("pw%d" % (j % 2))])
                            for h in range(H):
                                P.mm(psC[0:64, h * 64:(h + 1) * 64], pw[j % 2][:, h, 1, :], R[cur][:, h, :], True, True,
                                     [K("pw%d" % (j % 2)), K("R%d" % cur)], [K("psC")])
                            P.add("dve", lambda e, cur=cur: e.tensor_tensor(R[1 - cur][:], R[cur][:], psC[0:64, :].rearrange("p (h d) -> p h d", d=64), ALU.add),
                                  [K("R%d" % cur), K("psC")], [K("R%d" % (1 - cur))])
                            cur = 1 - cur
                        if STAGE < 4: continue
                        for h in range(H):
                            P.mm(psD[0:64, h * 64:(h + 1) * 64], A["kp"][:, h, cs], Hs[:, h, :], True, False, [K("Akp"), K("H")], [K("psD")])
                            P.mm(psD[0:64, h * 64:(h + 1) * 64], am[:, h, 128:192], vt[:, n, h * DV:(h + 1) * DV], False, True,
                                 [K("am"), K("vt")], [K("psD")])
                        P.add("act", lambda e: e.activation(rhs[:], psD[0:64, :].rearrange("p (h d) -> p h d", d=64), AF.Copy), [K("psD")], [K("rhs")])
                        for h in range(H):
                            P.mm(psC[0:64, h * 64:(h + 1) * 64], R[cur][:, h, :], rhs[:, h, :], True, True, [K("R%d" % cur), K("rhs")], [K("psC")])
                        P.add("dve", lambda e: e.tensor_copy(us[:], psC[0:64, :].rearrange("p (h d) -> p h d", d=64)), [K("psC")], [K("us")])
                    if STAGE < 5: continue
                    for h in range(H):
                        a3 = am[:, h, 192:256] if delta else am[:, h, :]
                        P.mm(psO_h(h), A["rt"][:, h, cs], Hs[:, h, :], True, False, [K("Art"), K("H")], kO)
                        P.mm(psO_h(h), a3, vt[:, n, h * DV:(h + 1) * DV], False, not delta, [K("am"), K("vt")], kO)
                        if delta:
                            P.mm(psO_h(h), am[:, h, 256:320], us[:, h, :], False, True, [K("am"), K("us")], kO)
                    for h in range(H):
                        P.mm(psH_h(h), ktok[:, h, 0, :], vt[:, n, h * DV:(h + 1) * DV], True, not delta, [K("ktok"), K("vt")], kH)
                        if delta:
                            P.mm(psH_h(h), ktok[:, h, 1, :], us[:, h, :], False, True, [K("ktok"), K("us")], kH)
                    if STAGE < 6: continue
                    if DV == 64:
                        o3p = psO[0:64, :].rearrange("p (h d) -> p h d", d=64)
                        P.add("act", lambda e, o3p=o3p: e.activation(oc[:], o3p, AF.Copy), kO, [K("oc")])
                        o3 = oc[:]
                        kO_ = [K("oc")]
                        P.add("dve", lambda e, o3=o3: e.reduce_sum(s12[:, 0, :], o3, AX.X), kO_, [K("s12")])
                        P.add("act", lambda e, o3=o3: e.activation(sq[:], o3, AF.Square), kO_, [K("sq")])
                        P.add("dve", lambda e: e.reduce_sum(s12[:, 1, :], sq[:], AX.X), [K("sq")], [K("s12")])
                        P.add("dve", lambda e: e.tensor_scalar(s12[:, 0, :], s12[:, 0, :], 1.0 / DV, None, ALU.mult), [K("s12")], [K("s12")])
                        P.add("dve", lambda e: e.tensor_tensor(s12[:, 2, :], s12[:, 0, :], s12[:, 0, :], ALU.mult), [K("s12")], [K("s12")])
                        P.add("dve", lambda e: e.scalar_tensor_tensor(s12[:, 1, :], s12[:, 1, :], 1.0 / DV, s12[:, 2, :], ALU.mult, ALU.subtract),
                              [K("s12")], [K("s12")])
                        P.add("act", lambda e: e.activation(s12[:, 1, :], s12[:, 1, :], AF.Sqrt, bias=gn_eps, scale=1.0), [K("s12")], [K("s12")])
                        P.add("dve", lambda e: e.reciprocal(s12[:, 1, :], s12[:, 1, :]), [K("s12")], [K("s12")])
                        P.add("dve", lambda e, o3=o3: e.tensor_tensor(on[:], o3, s12[:, 0, :].unsqueeze(2).to_broadcast([64, H, DV]), ALU.subtract),
                              kO_ + [K("s12")], [K("on")])
                        P.add("dve", lambda e: e.tensor_tensor(on[:], on[:], s12[:, 1, :].unsqueeze(2).to_broadcast([64, H, DV]), ALU.mult),
                              [K("on"), K("s12")], [K("on")])
                    else:
                        for half in range(2):
                            o3 = psB[half][0:64, :].rearrange("p (h d) -> p h d", d=128)
                            hs = slice(half * 4, half * 4 + 4)
                            P.add("act", lambda e, o3=o3, hs=hs: e.activation(sq[:, hs, :], o3, AF.Square), [K("psB%d" % half)], [K("sq")])
                        P.add("dve", lambda e: e.reduce_sum(s12[:, 1, :], sq[:], AX.X), [K("sq")], [K("s12")])
                        P.add("act", lambda e: e.activation(s12[:, 1, :], s12[:, 1, :], AF.Sqrt, bias=gn_eps, scale=1.0 / DV), [K("s12")], [K("s12")])
                        P.add("dve", lambda e: e.reciprocal(s12[:, 1, :], s12[:, 1, :]), [K("s12")], [K("s12")])
                        for half in range(2):
                            o3 = psB[half][0:64, :].rearrange("p (h d) -> p h d", d=128)
                            hs = slice(half * 4, half * 4 + 4)
                            P.add("dve", lambda e, o3=o3, hs=hs: e.tensor_tensor(on[:, hs, :], o3, s12[:, 1, hs].unsqueeze(2).to_broadcast([64, 4, DV]), ALU.mult),
                                  [K("psB%d" % half), K("s12")], [K("on")])
                    if DV == 64:
                        h3 = psD[0:DK, :].rearrange("p (h d) -> p h d", d=64)
                        P.add("dve", lambda e, n=n: e.tensor_tensor(Hs[:], Hs[:], pcw[:, :, n:n + 1].to_broadcast([DK, H, DV]), ALU.mult),
                              [K("H"), K("pcw")], [K("H")])
                        P.add("dve", lambda e, h3=h3: e.tensor_tensor(Hs[:], Hs[:], h3, ALU.add), [K("H")] + kH, [K("H")])
                    else:
                        P.add("dve", lambda e, n=n: e.tensor_tensor(Hs[:], Hs[:], pcw[:, :, n:n + 1].to_broadcast([DK, H, DV]), ALU.mult),
                              [K("H"), K("pcw")], [K("H")])
                        for half, pp in enumerate((psC, psD)):
                            hs = slice(half * 4, half * 4 + 4)
                            h3 = pp[0:DK, :].rearrange("p (h d) -> p h d", d=128)
                            P.add("dve", lambda e, h3=h3, hs=hs: e.tensor_tensor(Hs[:, hs, :], Hs[:, hs, :], h3, ALU.add),
                                  [K("H"), kH[half]], [K("H")])
                    if STAGE < 7: continue
                    for h in range(H):
                        P.tr(psT[0:DV, h * 64:(h + 1) * 64], on[:, h, :], ident[0:64, 0:64], [K("on"), K("ident")], [K("psT")])
                    t3 = psT[0:DV, :].rearrange("p (h t) -> p h t", t=64)
                    ywc = yw[:, :, cs]
                    if delta:
                        P.add("dve", lambda e, t3=t3, ywc=ywc: e.tensor_tensor(ywc, t3, gng[:].unsqueeze(2).to_broadcast([DV, H, 64]), ALU.mult),
                              [K("psT"), K("gn")], [K("yw")])
                        P.add("pool", lambda e, ywc=ywc: e.tensor_tensor(ywc, ywc, gnb[:].unsqueeze(2).to_broadcast([DV, H, 64]), ALU.add),
                              [K("yw"), K("gn")], [K("yw")])
                        P.add("pool", lambda e, ywc=ywc, cs=cs: e.tensor_tensor(ywc, ywc, A["bon"][:, :, cs], ALU.add), [K("yw"), K("Abon")], [K("yw")])
                        P.add("pool", lambda e, ywc=ywc, cs=cs: e.tensor_tensor(ywc, ywc, A["g"][:, :, cs], ALU.mult), [K("yw"), K("Ag")], [K("yw")])
                    else:
                        P.add("dve", lambda e, t3=t3, ywc=ywc: e.tensor_scalar(ywc, t3, normg[:, 0:1], None, ALU.mult), [K("psT"), K("gn")], [K("yw")])
                        P.add("pool", lambda e, ywc=ywc, cs=cs: e.tensor_tensor(ywc, ywc, A["g"][:, :, cs], ALU.mult), [K("yw"), K("Ag")], [K("yw")])
                P.dma("pool", yT_d[yrow0:yrow0 + HV, n0:n0 + TW].rearrange("(h p) n -> p h n", p=DV), yw[:], [K("yw")], [], K("styw"))
    P.barrier()


def hgrn_prep(P, nc, NB, T, zT_d, prm, S):
    TW = 512
    NW = T // TW
    NCW = TW // CH
    with ExitStack() as es:
        lbc = SB(es, nc, "hp_lbc", [128, 4, 8])
        nl = SB(es, nc, "hp_nl", [128, 8])
        ident = SB(es, nc, "hp_id", [128, 128]); rmask = SB(es, nc, "hp_rmask", [128, TW])
        q = SB(es, nc, "hp_q", [128, TW]); f = SB(es, nc, "hp_f", [128, TW]); iv = SB(es, nc, "hp_i", [128, TW]); g = SB(es, nc, "hp_g", [128, TW])
        k = SB(es, nc, "hp_k", [128, TW]); c = SB(es, nc, "hp_c", [128, TW]); ep = SB(es, nc, "hp_ep", [128, TW]); en = SB(es, nc, "hp_en", [128, TW])
        o_rt = SB(es, nc, "hp_ort", [128, TW]); o_kt = SB(es, nc, "hp_okt", [128, TW]); o_ktl = SB(es, nc, "hp_oktl", [128, TW])
        o_pc = SB(es, nc, "hp_opc", [128, NCW]); vtk = SB(es, nc, "hp_vtk", [128, 4, 128])
        ps6 = PS(es, nc, "hp_ps6", [128, 512])
        P.dma("pool", lbc[:, 0, :], prm["lb2"][0].rearrange("(h p) -> p h", p=128), [], ["hp_lbc"], "hp_c0", allow_slow_non_contiguous=True)
        P.dma("pool", lbc[:, 1, :], prm["lb2"][1].rearrange("(h p) -> p h", p=128), [], ["hp_lbc"], "hp_c0", allow_slow_non_contiguous=True)
        P.dma("pool", ident[:], prm["ident"], [], ["hp_id"], "hp_c0")
        P.dma("pool", rmask[:], prm["rmask"].partition_broadcast(128), [], ["hp_rmask"], "hp_c0")
        P.add("dve", lambda e: e.tensor_tensor(lbc[:, 2, :], lbc[:, 1, :], lbc[:, 0, :], ALU.subtract), ["hp_lbc"], ["hp_lbc"])
        P.add("act", lambda e: e.activation(lbc[:, 2, :], lbc[:, 2, :], AF.Sigmoid), ["hp_lbc"], ["hp_lbc"])
        P.add("dve", lambda e: e.tensor_scalar(lbc[:, 3, :], lbc[:, 2, :], -1.0, 1.0, ALU.mult, ALU.add), ["hp_lbc"], ["hp_lbc"])
        P.add("dve", lambda e: e.tensor_scalar(nl[:], lbc[:, 3, :], -1.0, None, ALU.mult), ["hp_lbc"], ["hp_nl"])
        for b in range(NB):
            for w in range(NW):
                n0 = b * T + w * TW
                for h in range(8):
                    fs = slice(h * 128, (h + 1) * 128)
                    for j, (dst, key) in enumerate(((q, "hp_q"), (f, "hp_f"), (iv, "hp_i"), (g, "hp_g"))):
                        P.dma("sp" if j % 2 == 0 else "act", dst[:], zT_d[j * 1024 + h * 128:j * 1024 + (h + 1) * 128, n0:n0 + TW], [], [key], key)
                    P.add("act", lambda e: e.activation(f[:], f[:], AF.Sigmoid), ["hp_f"], ["hp_f"])
                    P.add("dve", lambda e, h=h: e.tensor_scalar(k[:], f[:], nl[:, h:h + 1], lbc[:, 3, h:h + 1], ALU.mult, ALU.add),
                          ["hp_f", "hp_nl", "hp_lbc"], ["hp_k"])
                    P.add("dve", lambda e, h=h: e.tensor_scalar(f[:], f[:], lbc[:, 3, h:h + 1], lbc[:, 2, h:h + 1], ALU.mult, ALU.add),
                          ["hp_f", "hp_lbc", "hp_k"], ["hp_f"])
                    P.add("act", lambda e: e.activation(f[:], f[:], AF.Ln), ["hp_f"], ["hp_f"])
                    P.add("act", lambda e: e.activation(q[:], q[:], AF.Silu), ["hp_q"], ["hp_q"])
                    P.add("act", lambda e: e.activation(g[:], g[:], AF.Silu), ["hp_g"], ["hp_g"])
                    P.dma("pool", S["g"][fs, n0:n0 + TW], g[:], ["hp_g"], [], "hp_stg")
                    P.add("dve", lambda e: e.tensor_tensor_scan(c[:], rmask[:], f[:], 0.0, ALU.mult, ALU.add), ["hp_rmask", "hp_f"], ["hp_c"])
                    P.add("act", lambda e: e.activation(ep[:], c[:], AF.Exp), ["hp_c"], ["hp_ep"])
                    P.add("act", lambda e: e.activation(en[:], c[:], AF.Exp, scale=-1.0), ["hp_c"], ["hp_en"])
                    P.add("dve", lambda e: e.tensor_tensor(o_rt[:], q[:], ep[:], ALU.mult), ["hp_q", "hp_ep"], ["hp_ort"])
                    P.add("pool", lambda e: e.tensor_tensor(o_kt[:], k[:], en[:], ALU.mult), ["hp_k", "hp_en"], ["hp_okt"])
                    P.add("dve", lambda e: e.tensor_copy(o_pc[:], ep[:].rearrange("p (n c) -> p n c", c=CH)[:, :, CH - 1]), ["hp_ep"], ["hp_opc"])
                    pcb = o_pc[:].unsqueeze(2).to_broadcast([128, NCW, CH])
                    P.add("dve", lambda e, pcb=pcb: e.tensor_tensor(o_ktl[:].rearrange("p (n c) -> p n c", c=CH),
                                                                     o_kt[:].rearrange("p (n c) -> p n c", c=CH), pcb, ALU.mult),
                          ["hp_okt", "hp_opc"], ["hp_oktl"])
                    for sub in range(TW // 128):
                        P.tr(ps6[:, sub * 128:(sub + 1) * 128], iv[:, sub * 128:(sub + 1) * 128], ident[:], ["hp_i", "hp_id"], ["hp_ps6"])
                    P.add("act", lambda e: e.activation(vtk[:], ps6[:].rearrange("p (s f) -> p s f", f=128), AF.Copy), ["hp_ps6"], ["hp_vtk"])
                    P.dma("pool", S["vtok"][n0:n0 + TW, fs].rearrange("(s p) f -> p s f", p=128), vtk[:], ["hp_vtk"], [], "hp_stv")
                    for key, tl, dd in (("hp_ort", o_rt, "rt"), ("hp_okt", o_kt, "kt"), ("hp_oktl", o_ktl, "ktl")):
                        P.dma("sp", S[dd][fs, n0:n0 + TW], tl[:], [key], [], "st" + key)
                    P.dma("sp", S["pc"][fs, n0 // CH:n0 // CH + NCW], o_pc[:], ["hp_opc"], [], "sthp_opc")
    P.barrier()


R0 = 1792
NEG = -30000.0
PI = math.pi


def nsa_prep(P, nc, NB, T, zT_d, pos_d, cst, S):
    TW = 512
    NW = T // TW
    with ExitStack() as es:
        invf = SB(es, nc, "np_invf", [128, 1]); rot = SB(es, nc, "np_rot", [128, 128]); ident = SB(es, nc, "np_id", [128, 128])
        posi = SB(es, nc, "np_posi", [128, TW], I32); ang = SB(es, nc, "np_ang", [128, TW])
        tt = SB(es, nc, "np_tt", [128, TW]); ki = SB(es, nc, "np_ki", [128, TW], I32); kf = SB(es, nc, "np_kf", [128, TW])
        sinF = SB(es, nc, "np_sin", [128, TW]); cosF = SB(es, nc, "np_cos", [128, TW])
        x = SB(es, nc, "np_x", [128, TW]); xr = SB(es, nc, "np_xr", [128, TW]); xq = SB(es, nc, "np_xq", [128, TW]); t2 = SB(es, nc, "np_t2", [128, TW])
        vtk = SB(es, nc, "np_vtk", [128, 4, 128]); gl = SB(es, nc, "np_gl", [24, TW]); gtk = SB(es, nc, "np_gtk", [128, 4, 24])
        ps1 = PS(es, nc, "np_ps1", [128, 512]); ps2 = PS(es, nc, "np_ps2", [128, 512]); ps3 = PS(es, nc, "np_ps3", [128, 512])
        P.dma("pool", invf[:], cst["invf"], [], ["np_c"], "np_c0")
        P.dma("pool", rot[:], cst["rot"], [], ["np_c"], "np_c0")
        P.dma("pool", ident[:], cst["ident"], [], ["np_c"], "np_c0")

        def reduce_sin(dst, shift, kd):
            P.add("dve", lambda e: e.tensor_scalar(tt[:], ang[:], shift, 1.0 / (2 * PI), ALU.add, ALU.mult), ["np_ang"], ["np_tt"])
            P.add("dve", lambda e: e.tensor_copy(ki[:], tt[:]), ["np_tt"], ["np_ki"])
            P.add("dve", lambda e: e.tensor_copy(kf[:], ki[:]), ["np_ki"], ["np_kf"])
            P.add("dve", lambda e: e.tensor_scalar(tt[:], ang[:], shift, None, ALU.add), ["np_ang", "np_ki"], ["np_tt"])
            P.add("dve", lambda e: e.scalar_tensor_tensor(tt[:], kf[:], -2 * PI, tt[:], ALU.mult, ALU.add), ["np_kf", "np_tt"], ["np_tt"])
            P.add("dve", lambda e: e.tensor_scalar(kf[:], tt[:], PI, -2 * PI, ALU.is_gt, ALU.mult), ["np_tt"], ["np_kf"])
            P.add("dve", lambda e: e.tensor_tensor(tt[:], tt[:], kf[:], ALU.add), ["np_tt", "np_kf"], ["np_tt"])
            P.add("dve", lambda e: e.tensor_scalar(kf[:], tt[:], -PI, 2 * PI, ALU.is_lt, ALU.mult), ["np_tt"], ["np_kf"])
            P.add("dve", lambda e: e.tensor_tensor(tt[:], tt[:], kf[:], ALU.add), ["np_tt", "np_kf"], ["np_tt"])
            P.add("dve", lambda e: e.tensor_scalar(tt[:], tt[:], PI, -PI, ALU.min, ALU.max), ["np_tt"], ["np_tt"])
            P.add("act", lambda e: e.activation(dst[:], tt[:], AF.Sin), ["np_tt"], [kd])

        for b in range(NB):
            for w in range(NW):
                t0 = w * TW
                n0 = b * T + t0
                P.dma("sp", posi[:], pos_d[b, t0:t0 + TW].partition_broadcast(128), [], ["np_posi"], "np_posi")
                P.add("dve", lambda e: e.tensor_copy(ang[:], posi[:]), ["np_posi"], ["np_ang"])
                P.add("dve", lambda e: e.tensor_scalar(ang[:], ang[:], invf[:, 0:1], None, ALU.mult), ["np_ang", "np_c"], ["np_ang"])
                reduce_sin(sinF, 0.0, "np_sin")
                reduce_sin(cosF, PI / 2, "np_cos")
                for ti, (row, dname, drow, sc, raw) in enumerate(
                        [(128 * i, "qrot", 128 * i, 0.125, True) for i in range(4)] + [(768, "ksr", 0, 1.0, False), (1024, "kwr", 0, 1.0, False)]):
                    P.dma("sp", x[:], zT_d[R0 + row:R0 + row + 128, n0:n0 + TW], [], ["np_x"], "np_x")
                    P.mm(ps1[:, 0:TW], rot[:], x[:], True, True, ["np_c", "np_x"], ["np_ps1"])
                    P.add("dve", lambda e: e.tensor_tensor(t2[:], ps1[:, 0:TW], sinF[:], ALU.mult), ["np_ps1", "np_sin"], ["np_t2"])
                    P.add("pool", lambda e: e.tensor_tensor(xr[:], x[:], cosF[:], ALU.mult), ["np_x", "np_cos"], ["np_xr"])
                    P.add("dve", lambda e, sc=sc: e.scalar_tensor_tensor(xr[:], xr[:], 1.0, t2[:], ALU.mult, ALU.add), ["np_xr", "np_t2"], ["np_xr"])
                    if sc != 1.0:
                        P.add("pool", lambda e, sc=sc: e.tensor_scalar(xr[:], xr[:], sc, None, ALU.mult), ["np_xr"], ["np_xr"])
                    P.dma("pool", S[dname][drow:drow + 128, n0:n0 + TW], xr[:], ["np_xr"], [], "np_stxr")
                    if raw:
                        P.add("act", lambda e, sc=sc: e.activation(xq[:], x[:], AF.Copy, scale=sc), ["np_x"], ["np_xq"])
                        P.dma("pool", S["qraw"][drow:drow + 128, n0:n0 + TW], xq[:], ["np_xq"], [], "np_stxq")
                for row, dname in ((896, "vs_tok"), (1152, "vw_tok")):
                    P.dma("sp", x[:], zT_d[R0 + row:R0 + row + 128, n0:n0 + TW], [], ["np_x"], "np_x")
                    for sub in range(4):
                        P.tr(ps2[:, sub * 128:(sub + 1) * 128], x[:, sub * 128:(sub + 1) * 128], ident[:], ["np_x", "np_c"], ["np_ps2"])
                    P.add("act", lambda e: e.activation(vtk[:], ps2[:].rearrange("p (s f) -> p s f", f=128), AF.Copy), ["np_ps2"], ["np_vtk"])
                    P.dma("pool", S[dname][n0:n0 + TW, :].rearrange("(s p) f -> p s f", p=128), vtk[:], ["np_vtk"], [], "np_stv")
                P.dma("sp", gl[:], zT_d[R0 + 1280:R0 + 1304, n0:n0 + TW], [], ["np_gl"], "np_gl")
                P.add("act", lambda e: e.activation(gl[:], gl[:], AF.Sigmoid), ["np_gl"], ["np_gl"])
                for sub in range(4):
                    P.tr(ps3[:, sub * 24:(sub + 1) * 24], gl[:, sub * 128:(sub + 1) * 128], ident[0:24, 0:24], ["np_gl", "np_c"], ["np_ps3"])
                P.add("act", lambda e: e.activation(gtk[:], ps3[:, 0:96].rearrange("p (s f) -> p s f", f=24), AF.Copy), ["np_ps3"], ["np_gtk"])
                P.dma("pool", S["gat_tok"][n0:n0 + TW, :].rearrange("(s p) f -> p s f", p=128), gtk[:], ["np_gtk"], [], "np_stg")
    P.barrier()


def nsa_cmp(P, nc, NB, T, zT_d, w1_d, w2_d, cpos_d, S):
    ncmp = (T - 32) // 16 + 1
    nch = (ncmp + 127) // 128
    C0 = math.sqrt(2.0 / math.pi)
    with ExitStack() as es:
        w1 = SB(es, nc, "nc_w1", [64, 2, 32, 128]); w2 = SB(es, nc, "nc_w2", [128, 2, 64]); cpos = SB(es, nc, "nc_cpos", [64, 2, 32])
        bias = SB(es, nc, "nc_bias", [128, 2]); kc = SB(es, nc, "nc_kc", [64, T])
        hx = SB(es, nc, "nc_hx", [128, 256]); h2 = SB(es, nc, "nc_h2", [128, 256]); h3 = SB(es, nc, "nc_h3", [128, 256])
        ko = SB(es, nc, "nc_ko", [64, 256]); vo = SB(es, nc, "nc_vo", [128, 2, 64])
        psb = PS(es, nc, "nc_psb", [128, 512]); psh = PS(es, nc, "nc_psh", [128, 512]); pso = PS(es, nc, "nc_pso", [128, 512])
        for z in range(2):
            P.dma("sp", w1[:, z, :, :], w1_d[z].rearrange("(l d) o -> d l o", d=64), [], ["nc_w1"], "nc_c0")
            P.dma("sp", w2[:, z, :], w2_d[z], [], ["nc_w2"], "nc_c0")
            P.dma("sp", cpos[:, z, :], cpos_d[z].rearrange("l d -> d l"), [], ["nc_cpos"], "nc_c0", allow_slow_non_contiguous=True)
        for z in range(2):
            for l in range(32):
                P.mm(psb[:, z:z + 1], w1[:, z, l, :], cpos[:, z, l:l + 1], l == 0, l == 31, ["nc_w1", "nc_cpos"], ["nc_psb"])
        P.add("dve", lambda e: e.tensor_copy(bias[:], psb[:, 0:2]), ["nc_psb"], ["nc_bias"])
        for b in range(NB):
            for z in range(2):
                for hk in range(2):
                    row = R0 + 512 + z * 128 + hk * 64
                    P.dma("sp", kc[:], zT_d[row:row + 64, b * T:(b + 1) * T], [], ["nc_kc"], "nc_kc")
                    for l in range(32):
                        P.mm(psh[:, 0:ncmp], w1[:, z, l, :], kc[:, l:l + 16 * (ncmp - 1) + 1:16], l == 0, l == 31, ["nc_w1", "nc_kc"], ["nc_psh"])
                    P.add("act", lambda e, z=z: e.activation(hx[:, 0:ncmp], psh[:, 0:ncmp], AF.Identity, bias=bias[:, z:z + 1], scale=1.0),
                          ["nc_psh", "nc_bias"], ["nc_hx"])
                    P.add("act", lambda e: e.activation(h2[:, 0:ncmp], hx[:, 0:ncmp], AF.Square), ["nc_hx"], ["nc_h2"])
                    P.add("dve", lambda e: e.tensor_scalar(h2[:, 0:ncmp], h2[:, 0:ncmp], 0.044715, 1.0, ALU.mult, ALU.add), ["nc_h2"], ["nc_h2"])
                    P.add("dve", lambda e: e.tensor_tensor(h2[:, 0:ncmp], h2[:, 0:ncmp], hx[:, 0:ncmp], ALU.mult), ["nc_h2", "nc_hx"], ["nc_h2"])
                    P.add("act", lambda e: e.activation(h3[:, 0:ncmp], h2[:, 0:ncmp], AF.Tanh, scale=C0), ["nc_h2"], ["nc_h3"])
                    P.add("dve", lambda e: e.tensor_scalar(h3[:, 0:ncmp], h3[:, 0:ncmp], 0.5, 0.5, ALU.mult, ALU.add), ["nc_h3"], ["nc_h3"])
                    P.add("dve", lambda e: e.tensor_tensor(h3[:, 0:ncmp], h3[:, 0:ncmp], hx[:, 0:ncmp], ALU.mult), ["nc_h3", "nc_hx"], ["nc_h3"])
                    r = (b * 2 + hk)
                    if z == 0:
                        P.mm(pso[0:64, 0:ncmp], w2[:, 0, :], h3[:, 0:ncmp], True, True, ["nc_w2", "nc_h3"], ["nc_pso"])
                        P.add("act", lambda e: e.activation(ko[:, 0:ncmp], pso[0:64, 0:ncmp], AF.Copy), ["nc_pso"], ["nc_ko"])
                        P.dma("pool", S["kcmpT"][r * 64:(r + 1) * 64, 0:ncmp], ko[:, 0:ncmp], ["nc_ko"], [], "nc_stko")
                    else:
                        for c in range(nch):
                            nn = min(128, ncmp - c * 128)
                            P.mm(pso[0:nn, c * 64:(c + 1) * 64], h3[:, c * 128:c * 128 + nn], w2[:, 1, :], True, True, ["nc_w2", "nc_h3"], ["nc_pso"])
                            P.add("act", lambda e, c=c, nn=nn: e.activation(vo[0:nn, c, :], pso[0:nn, c * 64:(c + 1) * 64], AF.Copy), ["nc_pso"], ["nc_vo"])
                            P.dma("pool", S["vcmp"][r, c * 128:c * 128 + nn, :], vo[0:nn, c, :], ["nc_vo"], [], "nc_stvo")
    P.barrier()


def nsa_attn_a(P, nc, NB, T, cst, S):
    ncmp = (T - 32) // 16 + 1
    nch = (ncmp + 127) // 128
    NT = T // 128
    NS = T // 64
    with ExitStack() as es:
        tab = SB(es, nc, "na_tab", [128, 2048 + T]); ident = SB(es, nc, "na_id", [128, 128])
        keepw = SB(es, nc, "na_keepw", [128, 2 * NS]); addw = SB(es, nc, "na_addw", [128, 2 * NS])
        kcm = SB(es, nc, "na_kcm", [64, nch * 128]); vx = SB(es, nc, "na_vx", [128, nch, 129])
        qr = SB(es, nc, "na_qr", [64, 4, T])
        ee = [SB(es, nc, "na_e%d" % i, [128, 512]) for i in range(2)]
        o1 = SB(es, nc, "na_o1", [128, 4, 65]); o2 = SB(es, nc, "na_o2", [128, 4, NS]); rden = SB(es, nc, "na_rden", [128, 4])
        oc = [SB(es, nc, "na_oc%d" % i, [128, 4, 64]) for i in range(2)]
        imp = SB(es, nc, "na_imp", [128, NS]); imp2 = SB(es, nc, "na_imp2", [128, NS]); m8 = SB(es, nc, "na_m8", [128, 16])
        selb = SB(es, nc, "na_selb", [128, NS]); sbT = SB(es, nc, "na_sbT", [64, T])
        psS = [PS(es, nc, "na_psS%d" % i, [128, 512]) for i in range(2)]
        psO1 = PS(es, nc, "na_psO1", [128, 512]); psO2 = PS(es, nc, "na_psO2", [128, 512]); psT = PS(es, nc, "na_psT", [128, 512])
        P.dma("sp", tab[:], cst["tab"], [], ["na_c"], "na_c0")
        P.dma("sp", ident[:], cst["ident"], [], ["na_c"], "na_c0")
        P.dma("sp", keepw[:], cst["keepw"], [], ["na_c"], "na_c0")
        P.dma("sp", addw[:], cst["addw"], [], ["na_c"], "na_c0")
        for c in range(nch):
            nn = min(128, ncmp - c * 128)
            P.dma("sp", vx[0:nn, c, 64:65 + NS], cst["ovx"][c * 128:c * 128 + nn, :], [], ["na_vxc"], "na_c0")
        for b in range(NB):
            for hk in range(2):
                r = b * 2 + hk
                P.dma("sp", kcm[:, 0:ncmp], S["kcmpT"][r * 64:(r + 1) * 64, 0:ncmp], [], ["na_kcm"], "na_kcm")
                for c in range(nch):
                    nn = min(128, ncmp - c * 128)
                    P.dma("sp", vx[0:nn, c, 0:64], S["vcmp"][r, c * 128:c * 128 + nn, :], [], ["na_vx"], "na_vx")
                P.dma("act", qr[:], S["qraw"][hk * 256:(hk + 1) * 256, b * T:(b + 1) * T].rearrange("(g d) n -> d g n", d=64), [], ["na_qr"], "na_qr")
                for j in range(NT):
                    chunks = [c for c in range(nch) if 16 * 128 * c + 31 <= 128 * j + 127]
                    s_ = j % 2
                    koc = "na_oc%d" % s_
                    if not chunks:
                        P.add("dve", lambda e, s_=s_: e.memset(oc[s_][:], 0.0), [], [koc])
                        P.add("dve", lambda e: e.memset(imp[:], 0.0), [], ["na_imp"])
                    else:
                        for ci, c in enumerate(chunks):
                            nn = min(128, ncmp - c * 128)
                            off = 2048 + 128 * j - 2048 * c
                            ps = psS[c % 2]
                            P.mm(ps[0:nn, :], kcm[:, c * 128:c * 128 + nn], qr[:, :, j * 128:(j + 1) * 128], True, False,
                                 ["na_kcm", "na_qr"], ["na_psS%d" % (c % 2)])
                            P.mm(ps[0:nn, :], ident[0:nn, 0:nn], tab[0:nn, off:off + 128].unsqueeze(1).to_broadcast([nn, 4, 128]), False, True,
                                 ["na_c"], ["na_psS%d" % (c % 2)])
                            P.add("act", lambda e, c=c, nn=nn, ps=ps: e.activation(ee[c % 2][0:nn, :], ps[0:nn, :], AF.Exp),
                                  ["na_psS%d" % (c % 2)], ["na_e%d" % (c % 2)])
                        for g in range(4):
                            for ci, c in enumerate(chunks):
                                nn = min(128, ncmp - c * 128)
                                P.mm(psO1[:, g * 65:(g + 1) * 65], ee[c % 2][0:nn, g * 128:(g + 1) * 128], vx[0:nn, c, 0:65],
                                     ci == 0, ci == len(chunks) - 1, ["na_e%d" % (c % 2), "na_vx", "na_vxc"], ["na_psO1"])
                        for g in range(4):
                            for ci, c in enumerate(chunks):
                                nn = min(128, ncmp - c * 128)
                                P.mm(psO2[:, g * NS:(g + 1) * NS], ee[c % 2][0:nn, g * 128:(g + 1) * 128], vx[0:nn, c, 65:65 + NS],
                                     ci == 0, ci == len(chunks) - 1, ["na_e%d" % (c % 2), "na_vx", "na_vxc"], ["na_psO2"])
                        P.add("act", lambda e: e.activation(o1[:], psO1[:, 0:260].rearrange("p (g d) -> p g d", d=65), AF.Copy), ["na_psO1"], ["na_o1"])
                        P.add("dve", lambda e: e.tensor_copy(o2[:], psO2[:, 0:4 * NS].rearrange("p (g d) -> p g d", d=NS)), ["na_psO2"], ["na_o2"])
                        P.add("dve", lambda e: e.tensor_scalar(rden[:], o1[:, :, 64], 1e-30, None, ALU.max), ["na_o1"], ["na_rden"])
                        P.add("dve", lambda e: e.reciprocal(rden[:], rden[:]), ["na_rden"], ["na_rden"])
                        P.add("dve", lambda e, s_=s_: e.tensor_tensor(oc[s_][:], o1[:, :, 0:64], rden[:].unsqueeze(2).to_broadcast([128, 4, 64]), ALU.mult),
                              ["na_o1", "na_rden"], [koc])
                        P.add("dve", lambda e: e.tensor_tensor(o2[:], o2[:], rden[:].unsqueeze(2).to_broadcast([128, 4, NS]), ALU.mult),
                              ["na_o2", "na_rden"], ["na_o2"])
                        P.add("dve", lambda e: e.reduce_sum(imp[:], o2[:].rearrange("p g s -> p s g"), AX.X), ["na_o2"], ["na_imp"])
                    P.dma("pool", S["oc_tok"][b * T + j * 128:b * T + (j + 1) * 128, hk * 256:(hk + 1) * 256],
                          oc[s_][:].rearrange("p g d -> p (g d)"), [koc], [], "st" + koc)
                    o2_ = NS - 2 * j
                    P.add("dve", lambda e, o2_=o2_: e.tensor_tensor(imp[:], imp[:], keepw[:, o2_:o2_ + NS], ALU.mult), ["na_imp", "na_c"], ["na_imp"])
                    P.add("dve", lambda e, o2_=o2_: e.tensor_tensor(imp[:], imp[:], addw[:, o2_:o2_ + NS], ALU.add), ["na_imp", "na_c"], ["na_imp"])
                    P.add("dve", lambda e: e.memset(imp[:, 0:1], 1e30), ["na_imp"], ["na_imp"])
                    P.add("dve", lambda e: e.max(m8[:, 0:8], imp[:]), ["na_imp"], ["na_m8"])
                    P.add("dve", lambda e: e.match_replace(imp2[:], m8[:, 0:8], imp[:], -3e38), ["na_imp", "na_m8"], ["na_imp2"])
                    P.add("dve", lambda e: e.max(m8[:, 8:16], imp2[:]), ["na_imp2"], ["na_m8"])
                    P.add("dve", lambda e: e.tensor_scalar(selb[:], imp[:], m8[:, 15:16], None, ALU.is_ge), ["na_imp", "na_m8"], ["na_selb"])
                    P.add("dve", lambda e: e.tensor_scalar(selb[:], selb[:], -1.0, -NEG, ALU.add, ALU.mult), ["na_selb"], ["na_selb"])
                    P.tr(psT[0:NS, 0:128], selb[:], ident[:], ["na_selb", "na_c"], ["na_psT"])
                    P.add("act", lambda e, j=j: e.activation(sbT[0:NS, j * 128:(j + 1) * 128], psT[0:NS, 0:128], AF.Copy), ["na_psT"], ["na_sbT"])
                P.dma("sp", S["selbT"][r * 64:r * 64 + NS, :], sbT[0:NS, :], ["na_sbT"], [], "na_stsb")
    P.barrier()


def nsa_attn_b(P, nc, NB, T, cst, S, yT_d, yrow0):
    NT = T // 128
    NS = T // 64
    WT = 4
    with ExitStack() as es:
        stg = SB(es, nc, "nb_stg", [64, 4, 512]); stg2 = SB(es, nc, "nb_stg2", [128, 512])
        qb = SB(es, nc, "nb_qb", [64, 4, T], BF16); ks = SB(es, nc, "nb_ks", [64, T], BF16); kw = SB(es, nc, "nb_kw", [64, T], BF16)
        sb = SB(es, nc, "nb_sb", [64, T], BF16); eall = SB(es, nc, "nb_eall", [64, T], BF16)
        caus = SB(es, nc, "nb_caus", [128, 128], BF16); anti = SB(es, nc, "nb_anti", [128, 128], BF16); idb = SB(es, nc, "nb_idb", [128, 128], BF16)
        ident = SB(es, nc, "nb_id", [128, 128])
        vsx = SB(es, nc, "nb_vsx", [128, NT, 65], BF16); vwx = SB(es, nc, "nb_vwx", [128, NT, 65], BF16)
        vst = SB(es, nc, "nb_vst", [128, NT, 64])
        gat = SB(es, nc, "nb_gat", [128, NT, 24])
        eall_s = SB(es, nc, "nb_eas", [128, NT, 512], BF16); eall_w = SB(es, nc, "nb_eaw", [128, WT + 1, 512], BF16)
        oct_ = SB(es, nc, "nb_oct", [128, 4, 64]); os_ = SB(es, nc, "nb_os", [128, 4, 65]); ow_ = SB(es, nc, "nb_ow", [128, 4, 65])
        wsw = SB(es, nc, "nb_wsw", [128, 2, 4]); acc = SB(es, nc, "nb_acc", [128, 4, 64]); tmp = SB(es, nc, "nb_tmp", [128, 4, 64])
        yw = SB(es, nc, "nb_yw", [128, 2, T])
        psS = [PS(es, nc, "nb_psS%d" % i, [128, 512]) for i in range(4)]
        psOs = PS(es, nc, "nb_psOs", [128, 512]); psOw = PS(es, nc, "nb_psOw", [128, 512]); psT = PS(es, nc, "nb_psT", [128, 512])

        def load_cast(dst, src, shape_p, keyd, wide):
            P.dma("sp", stg2[0:shape_p, 0:wide], src, [], ["nb_stg2"], "nb_stg2")
            P.add("act", lambda e: e.activation(dst, stg2[0:shape_p, 0:wide], AF.Copy), ["nb_stg2"], [keyd])
        for c0 in range(0, T, 512):
            load_cast(eall[:, c0:c0 + 512], cst["eall"][:, c0:c0 + 512], 64, "nb_eall", 512)
        load_cast(caus[:], cst["caus"], 128, "nb_cm", 128)
        load_cast(anti[:], cst["anti"], 128, "nb_cm", 128)
        load_cast(idb[:], cst["ident"], 128, "nb_cm", 128)
        P.dma("sp", ident[:], cst["ident"], [], ["nb_id"], "nb_c0")
        for b in range(NB):
            P.dma("sp", gat[:], S["gat_tok"][b * T:(b + 1) * T, :].rearrange("(j p) f -> p j f", p=128), [], ["nb_gat"], "nb_gat")
            for hk in range(2):
                r = b * 2 + hk
                for c0 in range(0, T, 512):
                    P.dma("sp", stg[:], S["qrot"][hk * 256:(hk + 1) * 256, b * T + c0:b * T + c0 + 512].rearrange("(g d) n -> d g n", d=64),
                          [], ["nb_stg"], "nb_stg")
                    P.add("act", lambda e, c0=c0: e.activation(qb[:, :, c0:c0 + 512], stg[:], AF.Copy), ["nb_stg"], ["nb_qb"])
                    load_cast(ks[:, c0:c0 + 512], S["ksr"][hk * 64:(hk + 1) * 64, b * T + c0:b * T + c0 + 512], 64, "nb_ks", 512)
                    load_cast(kw[:, c0:c0 + 512], S["kwr"][hk * 64:(hk + 1) * 64, b * T + c0:b * T + c0 + 512], 64, "nb_kw", 512)
                    load_cast(sb[0:NS, c0:c0 + 512], S["selbT"][r * 64:r * 64 + NS, c0:c0 + 512], NS, "nb_sb", 512)
                for vx_, nm, kk_ in ((vsx, "vs_tok", "nb_vsx"), (vwx, "vw_tok", "nb_vwx")):
                    P.dma("sp", vst[:], S[nm][b * T:(b + 1) * T, hk * 64:(hk + 1) * 64].rearrange("(j p) d -> p j d", p=128), [], ["nb_vst"], "nb_vst")
                    P.add("dve", lambda e, vx_=vx_: e.tensor_copy(vx_[:, :, 0:64], vst[:]), ["nb_vst"], [kk_])
                    P.add("dve", lambda e, vx_=vx_: e.memset(vx_[:, :, 64:65], 1.0), [], [kk_])
                k = 0
                for j in range(NT):
                    qs = qb[:, :, j * 128:(j + 1) * 128]
                    for br, (kk_b, vx_, pso, kps) in enumerate(((ks, vsx, psOs, "nb_psOs"), (kw, vwx, psOw, "nb_psOw"))):
                        kts = list(range(0, j + 1)) if br == 0 else list(range(max(0, j - WT), j + 1))
                        ea = eall_s if br == 0 else eall_w
                        kea = "nb_eas" if br == 0 else "nb_eaw"
                        for ki_, kt in enumerate(kts):
                            ps = psS[k % 4]; kp = "nb_psS%d" % (k % 4)
                            k += 1
                            extra = []
                            if br == 0 and NS > 16 and (2 * j + 1) >= 16:
                                extra.append((eall[0:NS, kt * 128:(kt + 1) * 128], sb[0:NS, j * 128:(j + 1) * 128], NS, ["nb_eall", "nb_sb"]))
                            if kt == j:
                                extra.append((idb[:], caus[:], 128, ["nb_cm"]))
                            if br == 1 and kt == j - WT:
                                extra.append((idb[:], anti[:], 128, ["nb_cm"]))
                            P.mm(ps[:], kk_b[:, kt * 128:(kt + 1) * 128], qs, True, not extra, ["nb_ks", "nb_kw", "nb_qb"], [kp])
                            for xi, (l_, r_, pp, rk) in enumerate(extra):
                                P.mm(ps[:], l_, r_.unsqueeze(1).to_broadcast([pp, 4, 128]), False, xi == len(extra) - 1, rk, [kp])
                            P.add("act", lambda e, ea=ea, ki_=ki_, ps=ps: e.activation(ea[:, ki_, :], ps[:], AF.Exp), [kp], [kea])
                        for g in range(4):
                            for ki_, kt in enumerate(kts):
                                P.mm(pso[:, g * 65:(g + 1) * 65], ea[:, ki_, g * 128:(g + 1) * 128], vx_[:, kt, :], ki_ == 0, ki_ == len(kts) - 1,
                                     [kea, "nb_vsx", "nb_vwx"], [kps])
                    P.dma("sp", oct_[:].rearrange("p g d -> p (g d)"), S["oc_tok"][b * T + j * 128:b * T + (j + 1) * 128, hk * 256:(hk + 1) * 256],
                          [], ["nb_oct"], "nb_oct")
                    P.add("act", lambda e: e.activation(os_[:], psOs[:, 0:260].rearrange("p (g d) -> p g d", d=65), AF.Copy), ["nb_psOs"], ["nb_os"])
                    P.add("dve", lambda e: e.tensor_copy(ow_[:], psOw[:, 0:260].rearrange("p (g d) -> p g d", d=65)), ["nb_psOw"], ["nb_ow"])
                    gv = gat[:, j, hk * 12:(hk + 1) * 12].rearrange("p (g t) -> p g t", t=3)
                    P.add("dve", lambda e: e.tensor_scalar(wsw[:, 0, :], os_[:, :, 64], 1e-30, None, ALU.max), ["nb_os"], ["nb_wsw"])
                    P.add("dve", lambda e: e.tensor_scalar(wsw[:, 1, :], ow_[:, :, 64], 1e-30, None, ALU.max), ["nb_ow"], ["nb_wsw"])
                    P.add("dve", lambda e: e.reciprocal(wsw[:], wsw[:]), ["nb_wsw"], ["nb_wsw"])
                    P.add("dve", lambda e, gv=gv: e.tensor_tensor(wsw[:, 0, :], wsw[:, 0, :], gv[:, :, 1], ALU.mult), ["nb_wsw", "nb_gat"], ["nb_wsw"])
                    P.add("dve", lambda e, gv=gv: e.tensor_tensor(wsw[:, 1, :], wsw[:, 1, :], gv[:, :, 2], ALU.mult), ["nb_wsw", "nb_gat"], ["nb_wsw"])
                    P.add("dve", lambda e, gv=gv: e.tensor_tensor(acc[:], oct_[:], gv[:, :, 0:1].to_broadcast([128, 4, 64]), ALU.mult),
                          ["nb_oct", "nb_gat"], ["nb_acc"])
                    P.add("pool", lambda e: e.tensor_tensor(tmp[:], os_[:, :, 0:64], wsw[:, 0, :].unsqueeze(2).to_broadcast([128, 4, 64]), ALU.mult),
                          ["nb_os", "nb_wsw"], ["nb_tmp"])
                    P.add("dve", lambda e: e.tensor_tensor(acc[:], acc[:], tmp[:], ALU.add), ["nb_acc", "nb_tmp"], ["nb_acc"])
                    P.add("pool", lambda e: e.tensor_tensor(tmp[:], ow_[:, :, 0:64], wsw[:, 1, :].unsqueeze(2).to_broadcast([128, 4, 64]), ALU.mult),
                          ["nb_ow", "nb_wsw", "nb_acc"], ["nb_tmp"])
                    P.add("dve", lambda e: e.tensor_tensor(acc[:], acc[:], tmp[:], ALU.add), ["nb_acc", "nb_tmp"], ["nb_acc"])
                    for c in range(2):
                        P.tr(psT[:, c * 128:(c + 1) * 128], acc[:, 2 * c:2 * c + 2, :].rearrange("p g d -> p (g d)"), ident[:], ["nb_acc", "nb_id"], ["nb_psT"])
                    P.add("act", lambda e, j=j: e.activation(yw[:, :, j * 128:(j + 1) * 128], psT[:, 0:256].rearrange("p (c n) -> p c n", c=2), AF.Copy),
                          ["nb_psT"], ["nb_yw"])
                P.dma("pool", yT_d[yrow0 + hk * 256:yrow0 + (hk + 1) * 256, b * T:(b + 1) * T].rearrange("(c p) n -> p c n", p=128), yw[:], ["nb_yw"], [], "nb_styw")
    P.barrier()

def make_consts(T):
    NS = T // 64
    ncmp = (T - 32) // 16 + 1
    NCP = (ncmp + 127) // 128 * 128
    c = {}
    c["ident"] = np.eye(128, dtype=np.float32)
    c["id64"] = np.eye(64, dtype=np.float32)
    b = np.zeros((128, 128), np.float32); b[:64, :64] = 1; b[64:, 64:] = 1
    c["blk1"] = b
    rm = np.ones(512, np.float32); rm[::64] = 0
    c["rmask"] = rm
    ms = np.triu(np.ones((64, 64), np.float32), 1); ml = np.tril(np.ones((64, 64), np.float32), -1); mi = np.triu(np.ones((64, 64), np.float32), 0)
    c["masks"] = np.concatenate([-ms, -ml, ms, mi, -mi], 1)
    c["tri"] = np.concatenate([np.triu(np.ones((128, 128), np.float32), 1), np.ones((128, 128), np.float32)], 1)
    c["iop"] = np.arange(128, dtype=np.float32).reshape(128, 1)
    inv = (1.0 / (10000.0 ** (np.arange(0, 64, 2, dtype=np.float32) / 64))).astype(np.float32)
    c["invf"] = inv[(np.arange(128) % 32)].reshape(128, 1).astype(np.float32)
    R = np.zeros((64, 64), np.float32)
    for m in range(32):
        R[m, m + 32] = -1.0; R[m + 32, m] = 1.0
    rot = np.zeros((128, 128), np.float32); rot[:64, :64] = R.T; rot[64:, 64:] = R.T
    c["rot"] = rot
    n = np.arange(128)[:, None]; xp = np.arange(2048 + T)[None, :]
    c["tab"] = np.where(16 * n + 31 <= xp - 2048, 0.0, -30000.0).astype(np.float32)
    cs = np.arange(NCP)[:, None] * 16; ss = np.arange(NS)[None, :] * 64
    ov = np.clip(np.minimum(cs + 32, ss + 64) - np.maximum(cs, ss), 0, None) / 32.0
    ov[ncmp:] = 0
    c["ovx"] = np.concatenate([np.ones((NCP, 1)), ov], 1).astype(np.float32)
    tl = np.arange(128)[:, None]; x = np.arange(2 * NS)[None, :]
    rel = x - NS; cur = tl // 64
    c["keepw"] = (rel < cur - 1).astype(np.float32)
    c["addw"] = np.where((rel == cur) | (rel == cur - 1), 1e30, np.where(rel > cur, -1e30, 0.0)).astype(np.float32)
    ea = np.zeros((64, T), np.float32); ea[np.arange(T) // 64, np.arange(T)] = 1.0
    c["eall"] = ea
    k = np.arange(128)[:, None]; t = np.arange(128)[None, :]
    c["caus"] = np.where(k <= t, 0.0, -30000.0).astype(np.float32)
    c["anti"] = np.where(k > t, 0.0, -30000.0).astype(np.float32)
    return c


def build(NB, T, debug=False, same_engine_sync=True):
    N = NB * T
    NS = T // 64
    ncmp = (T - 32) // 16 + 1
    NCP = (ncmp + 127) // 128 * 128
    NBLK = (2 * N) // BLK + NEXP
    NT = N // 128
    Cc = make_consts(T)
    nc = bass.Bass("TRN2", target_bir_lowering=False)
    din = lambda name, shape, dt=F32: nc.dram_tensor(name, list(shape), dt, kind="ExternalInput").ap()
    scr = lambda name, shape: nc.dram_tensor(name, list(shape), F32, kind=("ExternalOutput" if debug else "Internal")).ap()
    x = din("x", [N, 1024]); p = din("p", [2, N, 256]); pos = din("pos", [NB, T], I32)
    ev_w_in = din("ev_w_in", [1024, 3096]); ev_w_out = din("ev_w_out", [1024, 1024])
    od_w_in = din("od_w_in", [1024, 4096]); od_w_out = din("od_w_out", [1024, 1024])
    rw = dict(mu=din("rw_mu", [1792]), w0=din("rw_w0", [512]), w2=din("rw_w2", [64, 512]), a0=din("rw_a0", [512]),
              a2=din("rw_a2", [64, 512]), g2=din("rw_g2", [128, 512]), k_k=din("rw_k_k", [512]), k_a=din("rw_k_a", [512]),
              r_k=din("rw_r_k", [512]))
    gng = din("rw_gn_g", [512]); gnb = din("rw_gn_b", [512])
    cpos = din("nsa_cmp_pos", [2, 32, 64]); cw1 = din("nsa_cmp_w1", [2, 2048, 128]); cw2 = din("nsa_cmp_w2", [2, 128, 64])
    hg_lb = din("hg_lb", [2, 1024]); hg_ng = din("hg_norm_g", [128])
    wrg = din("moe_w_rg", [2, 1024, 4]); brg = din("moe_b_rg", [2, 4]); wre = din("moe_w_re", [2, 1024, 32]); bre = din("moe_b_re", [2, 32])
    mw1 = [din("moe_w1_%d" % i, [4096, 4096]) for i in range(2)]; mw3 = [din("moe_w3_%d" % i, [4096, 4096]) for i in range(2)]
    mw2 = [din("moe_w2_%d" % i, [4096, 4096]) for i in range(2)]
    ln_g = din("ln_g", [2, 2, 1024]); ln_b = din("ln_b", [2, 2, 1024])
    ple_w = din("ple_w", [2, 256, 1024]); ple_gw = din("ple_gate_w", [2, 1024, 1024])
    cst = {k: din("c_" + k, Cc[k].shape) for k in Cc}
    thr = din("c_thr", [NBLK])
    out = nc.dram_tensor("out", [N, 1024], F32, kind="ExternalOutput").ap()
    zT = scr("zT", [4096, N]); yT = scr("yT0", [1024, N]); yT1 = scr("yT1", [1024, N]); h1 = scr("h1", [N, 1024]); x1 = scr("x1", [N, 1024])
    xbuf = nc.dram_tensor("xbuf", [NBLK * BLK, 1024], F32).ap(); ybuf = nc.dram_tensor("ybuf", [NBLK * BLK, 1024], F32).ap()
    SR = {k: scr("SR_" + k, [512, N]) for k in ("rt", "kt", "kp", "bt", "ktl", "btl", "g", "bon")}
    SR["pc"] = scr("SR_pc", [512, N // 64]); SR["vtok"] = scr("SR_vtok", [N, 512])
    SH = {k: scr("SH_" + k, [1024, N]) for k in ("rt", "kt", "ktl", "g")}
    SH["pc"] = scr("SH_pc", [1024, N // 64]); SH["vtok"] = scr("SH_vtok", [N, 1024])
    SN = dict(qraw=scr("SN_qraw", [512, N]), qrot=scr("SN_qrot", [512, N]), ksr=scr("SN_ksr", [128, N]), kwr=scr("SN_kwr", [128, N]),
              vs_tok=scr("SN_vs", [N, 128]), vw_tok=scr("SN_vw", [N, 128]), gat_tok=scr("SN_gat", [N, 24]),
              kcmpT=scr("SN_kcmpT", [NB * 2 * 64, NCP]), vcmp=scr("SN_vcmp", [NB * 2, NCP, 64]),
              oc_tok=scr("SN_oc", [N, 512]), selbT=scr("SN_selbT", [NB * 2 * 64, T]))
    R = {}
    R["OH"] = nc.alloc_sbuf_tensor("r_OH", [128, NT, 2, 32], F32)
    R["rank"] = nc.alloc_sbuf_tensor("r_rank", [128, NT, 2], F32)
    R["gates"] = nc.alloc_sbuf_tensor("r_gates", [128, NT, 2], F32)
    R["base"] = nc.alloc_sbuf_tensor("r_base", [128, 32], F32)
    R["dest_i"] = nc.alloc_sbuf_tensor("r_dest_i", [128, NT * 2], I32)
    R["widx"] = nc.alloc_sbuf_tensor("r_widx", [128, NBLK], I32)
    P = Prog(nc, same_engine_sync=same_engine_sync)
    ident = cst["ident"]
    gemm_fm(P, nc, N, x, ev_w_in, 3096, zT, ident, "g0")
    prm = dict(rw); prm.update(blk1=cst["blk1"], ident=ident, rmask=cst["rmask"])
    rwkv_prep(P, nc, NB, T, zT, prm, SR)
    D = dict(SR); D.update(masks=cst["masks"], id64=cst["id64"], ident=ident, gng=gng, gnb=gnb)
    chunk_scan(P, nc, NB, T, D, True, 64, 64, 8, yT, 0, "rw", gn_eps=64e-5)
    nsa_prep(P, nc, NB, T, zT, pos, cst, SN)
    nsa_cmp(P, nc, NB, T, zT, cw1, cw2, cpos, SN)
    nsa_attn_a(P, nc, NB, T, cst, SN)
    nsa_attn_b(P, nc, NB, T, cst, SN, yT, 512)
    phase_t1(P, nc, N, yT, x, ev_w_out, ln_g[0, 0], ln_b[0, 0], wrg[0], wre[0], brg[0], bre[0], ident, cst["tri"], h1, R)
    phase_t2(P, nc, N, h1, xbuf, thr, cst["iop"], R)
    phase_t3(P, nc, N, xbuf, ybuf, mw1[0], mw3[0], mw2[0], ident, R)
    phase_t4(P, nc, N, h1, ybuf, p[0], ln_g[0, 1], ln_b[0, 1], ple_gw[0], ple_w[0], ident, x1, R)
    gemm_fm(P, nc, N, x1, od_w_in, 4096, zT, ident, "g1")
    hgrn_prep(P, nc, NB, T, zT, dict(lb2=hg_lb, ident=ident, rmask=cst["rmask"]), SH)
    D = dict(SH); D.update(masks=cst["masks"], id64=cst["id64"], ident=ident, normg=hg_ng)
    chunk_scan(P, nc, NB, T, D, False, 128, 128, 8, yT1, 0, "hg", gn_eps=1e-5)
    phase_t1(P, nc, N, yT1, x1, od_w_out, ln_g[1, 0], ln_b[1, 0], wrg[1], wre[1], brg[1], bre[1], ident, cst["tri"], h1, R)
    phase_t2(P, nc, N, h1, xbuf, thr, cst["iop"], R)
    phase_t3(P, nc, N, xbuf, ybuf, mw1[1], mw3[1], mw2[1], ident, R)
    phase_t4(P, nc, N, h1, ybuf, p[1], ln_g[1, 1], ln_b[1, 1], ple_gw[1], ple_w[1], ident, out, R)
    P.finalize(); P.emit()
    return nc, Cc, len(P.ops)


def make_in_map(inp, b0, NB, T, Cc):
    N = NB * T
    NBLK = (2 * N) // BLK + NEXP
    f = lambda a: np.ascontiguousarray(np.asarray(a))
    m = {}
    m["x"] = f(inp["x"][b0:b0 + NB, :T]).reshape(N, 1024)
    m["p"] = f(inp["p"][:, b0:b0 + NB, :T]).reshape(2, N, 256)
    m["pos"] = f(inp["positions"][b0:b0 + NB, :T]).astype(np.int32)
    m["ev_w_in"] = f(inp["ev_w_in"][0]); m["ev_w_out"] = f(inp["ev_w_out"][0])
    m["od_w_in"] = f(inp["od_w_in"][0]); m["od_w_out"] = f(inp["od_w_out"][0])
    for k in ("mu", "w0", "w2", "a0", "a2", "g2", "k_k", "k_a", "gn_g", "gn_b"):
        m["rw_" + k] = f(inp["rw_" + k][0])
    m["rw_r_k"] = f(inp["rw_r_k"][0]).reshape(512)
    m["nsa_cmp_pos"] = f(inp["nsa_cmp_pos"][0]); m["nsa_cmp_w1"] = f(inp["nsa_cmp_w1"][0]); m["nsa_cmp_w2"] = f(inp["nsa_cmp_w2"][0])
    m["hg_lb"] = f(inp["hg_lb"]); m["hg_norm_g"] = f(inp["hg_norm_g"][0])
    for k in ("moe_w_rg", "moe_b_rg", "moe_w_re", "moe_b_re", "ln_g", "ln_b", "ple_w", "ple_gate_w"):
        m[k] = f(inp[k])
    for k in ("moe_w1", "moe_w3", "moe_w2"):
        for li in range(2):
            m[k + "_%d" % li] = f(inp[k][li]).reshape(4096, 4096)
    for k in Cc:
        m["c_" + k] = Cc[k]
    m["c_thr"] = (np.arange(NBLK) * BLK).astype(np.float32)
    return m


def kernel(**inputs):
    NB, T = 2, 4096
    nc, Cc, nops = build(NB, T)
    in_maps = [make_in_map(inputs, c * NB, NB, T, Cc) for c in range(8)]
    res = run_bass_kernel_spmd(nc, in_maps, core_ids=list(range(8)))
    out = np.concatenate([np.asarray(r["out"]).reshape(NB, T, 1024) for r in res.results], 0)
    return out.astype(np.float32)
```

```python
from contextlib import ExitStack
import math, os
import numpy as np
import concourse.bass as bass
import concourse.mybir as mybir
from concourse.bass_utils import run_bass_kernel_spmd

F32 = mybir.dt.float32
BF16 = mybir.dt.bfloat16
I32 = mybir.dt.int32
U32 = mybir.dt.uint32
AF = mybir.ActivationFunctionType
ALU = mybir.AluOpType
AX = mybir.AxisListType

EPOCH = 12000
DMA_CAP = 1800


class _Op:
    __slots__ = ("eng", "fn", "reads", "writes", "dma", "semkey", "deps", "waits",
                 "inc", "sem", "val", "barrier", "seq")


class Prog:
    ENGS = ("pe", "act", "dve", "pool", "sp")

    def __init__(self, nc, same_engine_sync=True):
        self.nc = nc
        self.ops = []
        self.same_engine_sync = same_engine_sync
        self._sems = []
        self._keep = []

    def add(self, eng, fn, reads=(), writes=(), dma=False, semkey=None):
        o = _Op()
        o.eng = eng; o.fn = fn
        o.reads = tuple(reads); o.writes = tuple(writes)
        o.dma = dma; o.semkey = semkey
        o.deps = []; o.waits = []; o.inc = False; o.sem = None; o.val = 0
        o.barrier = False; o.seq = len(self.ops)
        if dma and semkey is None:
            raise ValueError("dma op needs semkey")
        self.ops.append(o)
        return o

    def barrier(self):
        for e in self.ENGS:
            o = self.add(e, None)
            o.barrier = True

    def dma(self, q, out, in_, reads, writes, semkey, **kw):
        return self.add(q, lambda e: e.dma_start(out=out, in_=in_, **kw), reads, writes,
                        dma=True, semkey=semkey)

    def mm(self, out, lhsT, rhs, start, stop, reads, writes):
        return self.add("pe", lambda e: e.matmul(out, lhsT, rhs, start=start, stop=stop),
                        reads, writes)

    def tr(self, out, in_, ident, reads, writes):
        return self.add("pe", lambda e: e.transpose(out, in_, ident), reads, writes)

    def _new_sem(self, name):
        h = self.nc.alloc_semaphore(name=name)
        self._sems.append(h)
        return h

    def finalize(self):
        ops = self.ops
        last_w = {}
        readers = {}
        last_of_eng = {}
        for o in ops:
            if o.barrier:
                continue
            deps = set()
            for k in o.reads:
                w = last_w.get(k)
                if w is not None:
                    deps.add(w)
            for k in o.writes:
                w = last_w.get(k)
                if w is not None:
                    deps.add(w)
                for r in readers.get(k, ()):
                    deps.add(r)
            deps.discard(o.seq)
            for d in deps:
                od = ops[d]
                if od.dma:
                    o.deps.append(d)
                elif od.eng != o.eng or o.dma:
                    o.deps.append(d)
                else:
                    if self.same_engine_sync and o.eng != "pe":
                        o.deps.append(d)
            for k in o.reads:
                readers.setdefault(k, []).append(o.seq)
            for k in o.writes:
                last_w[k] = o.seq
                readers[k] = []
        for o in ops:
            for d in o.deps:
                ops[d].inc = True
        eng_sem = {}
        dma_sem = {}
        last_inc_op = {}
        cur_last = {}
        for o in ops:
            if o.barrier:
                if o.eng == self.ENGS[0]:
                    for e, lo in cur_last.items():
                        lo.inc = True
                continue
            if not o.dma:
                cur_last[o.eng] = o
        waited = {e: {} for e in self.ENGS}
        released = set()
        all_dma_state = {}
        eng_state = {}
        nsem = 0
        for o in ops:
            if o.barrier:
                if o.eng == self.ENGS[-1]:
                    pass
                for e, (s, c) in eng_state.items():
                    if e != o.eng and c > 0:
                        o.waits.append((s, c))
                for k, (s, c) in all_dma_state.items():
                    if c > 0:
                        o.waits.append((s, c * 16))
                if o.eng == self.ENGS[-1]:
                    for v in dma_sem.values():
                        released.add(id(v))
                    dma_sem = {}
                continue
            for d in o.deps:
                od = ops[d]
                o.waits.append((od.sem, od.val))
            if o.dma:
                st = dma_sem.get(o.semkey)
                if st is None or st[1] >= DMA_CAP:
                    used = set(id(v) for v in dma_sem.values())
                    cands = [v for v in self._keep if id(v) not in used and id(v) in released and v[1] < DMA_CAP - 300]
                    if cands:
                        st = min(cands, key=lambda v: v[1])
                        released.discard(id(st))
                    else:
                        st = [self._new_sem("d%d" % nsem), 0]; nsem += 1
                        self._keep.append(st)
                    dma_sem[o.semkey] = st
                st[1] += 1
                o.sem = st[0]; o.val = st[1] * 16; o.inc = True
                all_dma_state[id(st)] = (st[0], st[1])
            elif o.inc:
                st = eng_sem.get(o.eng)
                if st is None or st[1] >= EPOCH:
                    st = [self._new_sem("e%d" % nsem), 0]; nsem += 1
                    eng_sem[o.eng] = st
                st[1] += 1
                o.sem = st[0]; o.val = st[1]
                eng_state[o.eng] = (st[0], st[1])
        for o in ops:
            w = waited[o.eng]
            best = {}
            for (s, v) in o.waits:
                key = id(s)
                if w.get(key, 0) >= v:
                    continue
                if key not in best or best[key][1] < v:
                    best[key] = (s, v)
            o.waits = list(best.values())
            for key, (s, v) in best.items():
                w[key] = v
        self.nsem = nsem
        return self

    def emit(self):
        nc = self.nc
        per = {e: [o for o in self.ops if o.eng == e] for e in self.ENGS}

        def run(engine, lst):
            for o in lst:
                for (s, v) in o.waits:
                    engine.wait_ge(s, v)
                if o.fn is None:
                    continue
                ins = o.fn(engine)
                if o.inc:
                    ins.then_inc(o.sem, 16 if o.dma else 1)

        with nc.Block() as block:
            @block.tensor
            def _(e):
                run(e, per["pe"])

            @block.scalar
            def _(e):
                run(e, per["act"])

            @block.vector
            def _(e):
                run(e, per["dve"])

            @block.gpsimd
            def _(e):
                run(e, per["pool"])

            @block.sync
            def _(e):
                run(e, per["sp"])


ALPHA = float((2 * 2) ** 0.25)
LN_EPS = 1e-5
BLK = 512
NEXP = 32


_UNIQ = [0]


def SB(es, nc, name, shape, dt=F32):
    _UNIQ[0] += 1
    return es.enter_context(nc.sbuf_tensor("s%d_%s" % (_UNIQ[0], name), list(shape), dt))


def PS(es, nc, name, shape, dt=F32):
    _UNIQ[0] += 1
    return es.enter_context(nc.psum_tensor("p%d_%s" % (_UNIQ[0], name), list(shape), dt))


def layer_norm_tile(P, u, out, g_t, b_t, st, mv, sd, rs, tag, eng2="pool"):
    ku, ko = tag + "u", tag + "o"
    P.add("dve", lambda e: e.bn_stats(st[:, 0, :], u[:, 0:512]), [ku], [tag + "st0"])
    P.add("dve", lambda e: e.bn_stats(st[:, 1, :], u[:, 512:1024]), [ku], [tag + "st1"])
    P.add("dve", lambda e: e.bn_aggr(mv[:], st[:].rearrange("p a b -> p (a b)")),
          [tag + "st0", tag + "st1"], [tag + "mv"])
    P.add("act", lambda e: e.activation(sd[:], mv[:, 1:2], AF.Sqrt, bias=LN_EPS, scale=1.0),
          [tag + "mv"], [tag + "sd"])
    P.add("dve", lambda e: e.reciprocal(rs[:], sd[:]), [tag + "sd"], [tag + "rs"])
    P.add("dve", lambda e: e.tensor_scalar(out, u, mv[:, 0:1], rs[:, 0:1], ALU.subtract, ALU.mult),
          [ku, tag + "mv", tag + "rs"], [ko])
    P.add(eng2, lambda e: e.tensor_tensor(out, out, g_t, ALU.mult), [ko, tag + "g"], [ko])
    P.add(eng2, lambda e: e.tensor_tensor(out, out, b_t, ALU.add), [ko, tag + "b"], [ko])


def phase_t1(P, nc, N, yT_d, x_d, wout_d, lng_d, lnb_d, wrg_d, wre_d, brg_d, bre_d,
             ident_d, tri_d, h1_d, R):
    NT = N // 128
    with ExitStack() as es:
        wout_b = SB(es, nc, "wout_b", [128, 8, 1024], BF16)
        wst = SB(es, nc, "wst", [128, 8, 512], F32)
        g_t = SB(es, nc, "g_t", [128, 1024]); b_t = SB(es, nc, "b_t", [128, 1024])
        wr = SB(es, nc, "wr", [128, 8, 36]); br = SB(es, nc, "br", [128, 36])
        ident = SB(es, nc, "ident", [128, 128]); tri = SB(es, nc, "tri", [128, 256])
        yt = [SB(es, nc, "yt%d" % i, [128, 8, 128]) for i in range(2)]
        ytb = [SB(es, nc, "ytb%d" % i, [128, 8, 128], BF16) for i in range(2)]
        xt = [SB(es, nc, "xt%d" % i, [128, 1024]) for i in range(2)]
        u = SB(es, nc, "u", [128, 1024]); h1 = [SB(es, nc, "h1_%d" % i, [128, 1024]) for i in range(2)]
        h1T = SB(es, nc, "h1T", [128, 8, 128])
        st = SB(es, nc, "st", [128, 2, 6]); mv = SB(es, nc, "mv", [128, 2])
        sd = SB(es, nc, "sd", [128, 1]); rs = SB(es, nc, "rs", [128, 1])
        lg = SB(es, nc, "lg", [128, 36]); sm = SB(es, nc, "sm", [128, 16])
        ohg = SB(es, nc, "ohg", [128, 4]); eg = SB(es, nc, "eg", [128, 4])
        le = SB(es, nc, "le", [128, 8]); top8 = SB(es, nc, "top8", [128, 8])
        m12 = SB(es, nc, "m12", [128, 2, 8]); t12 = SB(es, nc, "t12", [128, 32]); junk = SB(es, nc, "junk", [128, 32])
        psA = [PS(es, nc, "psA%d" % i, [128, 512]) for i in range(2)]
        psT = [PS(es, nc, "psT%d" % i, [128, 512]) for i in range(2)]
        psL = PS(es, nc, "psL", [128, 512]); psR = PS(es, nc, "psR", [128, 512])
        OH, rank, gates, base = R["OH"], R["rank"], R["gates"], R["base"]

        for hh in range(2):
            P.dma("sp", wst[:], wout_d[:, hh * 512:(hh + 1) * 512].rearrange("(c p) n -> p c n", p=128),
                  [], ["wst"], "wst")
            P.add("act", lambda e, hh=hh: e.activation(wout_b[:, :, hh * 512:(hh + 1) * 512], wst[:], AF.Copy),
                  ["wst"], ["wout_b"])
        P.dma("sp", g_t[:], lng_d.partition_broadcast(128), [], ["L1g"], "cst1")
        P.dma("sp", b_t[:], lnb_d.partition_broadcast(128), [], ["L1b"], "cst1")
        P.dma("pool", wr[:, :, 0:4], wrg_d.rearrange("(c p) e -> p c e", p=128), [], ["wr"], "cst2")
        P.dma("pool", wr[:, :, 4:36], wre_d.rearrange("(c p) e -> p c e", p=128), [], ["wr"], "cst2")
        P.dma("pool", br[:, 0:4], brg_d.partition_broadcast(128), [], ["br"], "cst2")
        P.dma("pool", br[:, 4:36], bre_d.partition_broadcast(128), [], ["br"], "cst2")
        P.dma("pool", ident[:], ident_d, [], ["ident"], "cst2")
        P.dma("pool", tri[:], tri_d, [], ["tri"], "cst2")
        P.add("dve", lambda e: e.memset(base[:], 0.0), [], ["base"])

        for i in range(NT):
            s = i % 2
            n0 = i * 128
            kyt, kytb, kxt, kh1 = "yt%d" % s, "ytb%d" % s, "xt%d" % s, "h1_%d" % s
            P.dma("sp", yt[s][:], yT_d[:, n0:n0 + 128].rearrange("(c p) n -> p c n", p=128), [], [kyt], kyt)
            P.dma("sp", xt[s][:], x_d[n0:n0 + 128, :], [], [kxt], kxt)
            P.add("act", lambda e, s=s: e.activation(ytb[s][:], yt[s][:], AF.Copy), [kyt], [kytb])
            for hh in range(2):
                for c in range(8):
                    P.mm(psA[hh][:], ytb[s][:, c, :], wout_b[:, c, hh * 512:(hh + 1) * 512], c == 0, c == 7,
                         [kytb, "wout_b"], ["psA%d" % hh])
                P.add("dve", lambda e, s=s, hh=hh: e.scalar_tensor_tensor(
                    u[:, hh * 512:(hh + 1) * 512], xt[s][:, hh * 512:(hh + 1) * 512], ALPHA, psA[hh][:],
                    ALU.mult, ALU.add), [kxt, "psA%d" % hh], ["L1u"])
            P.add("dve", lambda e: e.bn_stats(st[:, 0, :], u[:, 0:512]), ["L1u"], ["L1st0"])
            P.add("dve", lambda e: e.bn_stats(st[:, 1, :], u[:, 512:1024]), ["L1u"], ["L1st1"])
            P.add("dve", lambda e: e.bn_aggr(mv[:], st[:].rearrange("p a b -> p (a b)")), ["L1st0", "L1st1"], ["L1mv"])
            P.add("act", lambda e: e.activation(sd[:], mv[:, 1:2], AF.Sqrt, bias=LN_EPS, scale=1.0), ["L1mv"], ["L1sd"])
            P.add("dve", lambda e: e.reciprocal(rs[:], sd[:]), ["L1sd"], ["L1rs"])
            P.add("dve", lambda e, s=s: e.tensor_scalar(h1[s][:], u[:], mv[:, 0:1], rs[:, 0:1], ALU.subtract, ALU.mult),
                  ["L1u", "L1mv", "L1rs"], [kh1])
            P.add("pool", lambda e, s=s: e.tensor_tensor(h1[s][:], h1[s][:], g_t[:], ALU.mult), [kh1, "L1g"], [kh1])
            P.add("pool", lambda e, s=s: e.tensor_tensor(h1[s][:], h1[s][:], b_t[:], ALU.add), [kh1, "L1b"], [kh1])
            P.dma("sp", h1_d[n0:n0 + 128, :], h1[s][:], [kh1], [], "st" + kh1)
            for c in range(8):
                P.tr(psT[c // 4][:, (c % 4) * 128:(c % 4 + 1) * 128], h1[s][:, c * 128:(c + 1) * 128], ident[:],
                     [kh1, "ident"], ["psT%d" % (c // 4)])
            for hh in range(2):
                P.add("act", lambda e, hh=hh: e.activation(
                    h1T[:, hh * 4:(hh + 1) * 4, :], psT[hh][:].rearrange("p (c n) -> p c n", c=4), AF.Copy),
                    ["psT%d" % hh], ["h1T"])
            for c in range(8):
                P.mm(psL[:, 0:36], h1T[:, c, :], wr[:, c, :], c == 0, c == 7, ["h1T", "wr"], ["psL"])
            P.add("dve", lambda e: e.tensor_tensor(lg[:], psL[:, 0:36], br[:], ALU.add), ["psL", "br"], ["lg"])
            P.add("dve", lambda e: e.reduce_max(sm[:, 0:1], lg[:, 0:4], AX.X), ["lg"], ["sm0"])
            P.add("dve", lambda e: e.tensor_scalar(ohg[:], lg[:, 0:4], sm[:, 0:1], None, ALU.is_equal), ["lg", "sm0"], ["ohg"])
            P.add("dve", lambda e: e.tensor_scalar(sm[:, 1:2], sm[:, 0:1], -1.0, None, ALU.mult), ["sm0"], ["sm1"])
            P.add("act", lambda e: e.activation(eg[:], lg[:, 0:4], AF.Exp, bias=sm[:, 1:2], scale=1.0, accum_out=sm[:, 2:3]),
                  ["lg", "sm1"], ["sm2", "eg"])
            P.add("dve", lambda e: e.reciprocal(sm[:, 3:4], sm[:, 2:3]), ["sm2"], ["sm3"])
            P.add("dve", lambda e: e.tensor_scalar(le[:], lg[:, 4:12], ohg[:, 0:1], None, ALU.mult), ["lg", "ohg"], ["le"])
            for g in range(1, 4):
                P.add("dve", lambda e, g=g: e.scalar_tensor_tensor(le[:], lg[:, 4 + 8 * g:12 + 8 * g], ohg[:, g:g + 1], le[:],
                                                                   ALU.mult, ALU.add), ["lg", "ohg", "le"], ["le"])
            P.add("dve", lambda e: e.max(top8[:], le[:]), ["le"], ["top8"])
            P.add("dve", lambda e: e.tensor_scalar(m12[:, 0, :], le[:], top8[:, 0:1], None, ALU.is_equal), ["le", "top8"], ["m1"])
            P.add("dve", lambda e: e.tensor_scalar(m12[:, 1, :], le[:], top8[:, 1:2], None, ALU.is_equal), ["le", "top8"], ["m2"])
            P.add("dve", lambda e: e.tensor_tensor(sm[:, 4:5], top8[:, 1:2], top8[:, 0:1], ALU.subtract), ["top8"], ["sm4"])
            P.add("act", lambda e: e.activation(sm[:, 5:6], sm[:, 4:5], AF.Exp), ["sm4"], ["sm5"])
            P.add("dve", lambda e: e.tensor_scalar(sm[:, 6:7], sm[:, 5:6], 1.0, None, ALU.add), ["sm5"], ["sm6"])
            P.add("dve", lambda e: e.reciprocal(sm[:, 7:8], sm[:, 6:7]), ["sm6"], ["sm7"])
            P.add("dve", lambda e, i=i: e.tensor_tensor(gates[:, i, 0:1], sm[:, 7:8], sm[:, 3:4], ALU.mult), ["sm7", "sm3"], ["gates"])
            P.add("dve", lambda e, i=i: e.tensor_tensor(gates[:, i, 1:2], gates[:, i, 0:1], sm[:, 5:6], ALU.mult), ["gates", "sm5"], ["gates"])
            for j in range(2):
                for g in range(4):
                    P.add("dve", lambda e, i=i, j=j, g=g: e.tensor_scalar(
                        OH[:, i, j, 8 * g:8 * g + 8], m12[:, j, :], ohg[:, g:g + 1], None, ALU.mult),
                        ["m1", "m2", "ohg"], ["OH"])
            P.mm(psR[:, 0:32], tri[:, 0:128], OH[:, i, 0, :], True, True, ["tri", "OH"], ["psR"])
            P.mm(psR[:, 32:64], tri[:, 128:256], OH[:, i, 0, :], True, True, ["tri", "OH"], ["psR"])
            P.mm(psR[:, 64:96], tri[:, 0:128], OH[:, i, 1, :], True, True, ["tri", "OH"], ["psR"])
            P.mm(psR[:, 96:128], tri[:, 128:256], OH[:, i, 1, :], True, True, ["tri", "OH"], ["psR"])
            for j in range(2):
                P.add("dve", lambda e, j=j: e.tensor_tensor(t12[:], base[:], psR[:, 64 * j:64 * j + 32], ALU.add),
                      ["base", "psR"], ["t12"])
                P.add("dve", lambda e, i=i, j=j: e.tensor_tensor(junk[:], OH[:, i, j, :], t12[:], ALU.mult),
                      ["OH", "t12"], ["junk"])
                P.add("dve", lambda e, i=i, j=j: e.reduce_sum(rank[:, i, j:j + 1], junk[:], AX.X), ["junk"], ["rank"])
                P.add("dve", lambda e, j=j: e.tensor_tensor(base[:], base[:], psR[:, 64 * j + 32:64 * j + 64], ALU.add),
                      ["base", "psR"], ["base"])
    P.barrier()


def phase_t2(P, nc, N, h1_d, xbuf_d, thr_d, iop_d, R):
    NT = N // 128
    NBLK = (2 * N) // BLK + NEXP
    OH, rank, base = R["OH"], R["rank"], R["base"]
    dest_i, widx = R["dest_i"], R["widx"]
    sh = BLK.bit_length() - 1
    with ExitStack() as es:
        ci = SB(es, nc, "ci", [128, 32], I32); padded = SB(es, nc, "padded", [128, 32])
        ones = SB(es, nc, "ones32", [128, 32]); ends = SB(es, nc, "ends", [128, 32]); start = SB(es, nc, "start", [128, 32])
        tmp = SB(es, nc, "tmpd", [128, NT * 2, 32]); dsum = SB(es, nc, "dsum", [128, NT * 2])
        thr = SB(es, nc, "thr", [128, NBLK]); iop = SB(es, nc, "iop", [128, 1])
        cmp = SB(es, nc, "cmp", [128, NBLK, 32]); be = SB(es, nc, "be", [128, NBLK])
        hx = [SB(es, nc, "hx%d" % i, [128, 1024]) for i in range(2)]
        P.dma("sp", thr[:], thr_d.partition_broadcast(128), [], ["thr"], "c2a")
        P.dma("sp", iop[:], iop_d, [], ["iop"], "c2a")
        P.add("dve", lambda e: e.tensor_copy(ci[:], base[:]), ["base"], ["ci"])
        P.add("dve", lambda e: e.tensor_scalar(ci[:], ci[:], BLK - 1, None, ALU.add), ["ci"], ["ci"])
        P.add("dve", lambda e: e.tensor_scalar(ci[:], ci[:], sh, None, ALU.arith_shift_right), ["ci"], ["ci"])
        P.add("dve", lambda e: e.tensor_scalar(ci[:], ci[:], sh, None, ALU.logical_shift_left), ["ci"], ["ci"])
        P.add("dve", lambda e: e.tensor_copy(padded[:], ci[:]), ["ci"], ["padded"])
        P.add("dve", lambda e: e.memset(ones[:], 1.0), [], ["ones32"])
        P.add("dve", lambda e: e.tensor_tensor_scan(ends[:], ones[:], padded[:], 0.0, ALU.mult, ALU.add),
              ["ones32", "padded"], ["ends"])
        P.add("dve", lambda e: e.tensor_tensor(start[:], ends[:], padded[:], ALU.subtract), ["ends", "padded"], ["start"])
        P.add("dve", lambda e: e.tensor_tensor(
            tmp[:], OH[:].rearrange("p i j e -> p (i j) e"), start[:].unsqueeze(1).to_broadcast([128, NT * 2, 32]), ALU.mult),
            ["OH", "start"], ["tmpd"])
        P.add("dve", lambda e: e.reduce_sum(dsum[:], tmp[:], AX.X), ["tmpd"], ["dsum"])
        P.add("dve", lambda e: e.tensor_tensor(dsum[:], dsum[:], rank[:].rearrange("p i j -> p (i j)"), ALU.add),
              ["dsum", "rank"], ["dsum"])
        P.add("dve", lambda e: e.tensor_copy(dest_i[:], dsum[:]), ["dsum"], ["dest_i"])
        P.add("dve", lambda e: e.tensor_tensor(
            cmp[:], ends[:].unsqueeze(1).to_broadcast([128, NBLK, 32]), thr[:].unsqueeze(2).to_broadcast([128, NBLK, 32]),
            ALU.is_le), ["ends", "thr"], ["cmp"])
        P.add("dve", lambda e: e.reduce_sum(be[:], cmp[:], AX.X), ["cmp"], ["be"])
        P.add("dve", lambda e: e.tensor_scalar(be[:], be[:], float(NEXP - 1), 128.0, ALU.min, ALU.mult), ["be"], ["be"])
        P.add("dve", lambda e: e.tensor_scalar(be[:], be[:], iop[:, 0:1], None, ALU.add), ["be", "iop"], ["be"])
        P.add("dve", lambda e: e.tensor_copy(widx[:], be[:]), ["be"], ["widx"])
        for i in range(NT):
            s = i % 2
            k = "hx%d" % s
            P.dma("sp", hx[s][:], h1_d[i * 128:(i + 1) * 128, :], [], [k], k)
            for j in range(2):
                P.add("pool", lambda e, i=i, j=j, s=s: e.indirect_dma_start(
                    xbuf_d, bass.IndirectOffsetOnAxis(dest_i[:, 2 * i + j:2 * i + j + 1], 0), hx[s][:], None),
                    [k, "dest_i"], [], dma=True, semkey="sc%d_%d" % (s, j))
    P.barrier()


def phase_t3(P, nc, N, xbuf_d, ybuf_d, w1_d, w3_d, w2_d, ident_d, R):
    NBLK = (2 * N) // BLK + NEXP
    widx = R["widx"]
    NS = BLK // 128
    with ExitStack() as es:
        ident = SB(es, nc, "ident3", [128, 128])
        ws = [SB(es, nc, "ws%d" % i, [128, 4096]) for i in range(3)]
        w1b = [SB(es, nc, "w1b%d" % i, [128, 8, 4, 128], BF16) for i in range(2)]
        w3b = [SB(es, nc, "w3b%d" % i, [128, 8, 4, 128], BF16) for i in range(2)]
        w2b = [SB(es, nc, "w2b%d" % i, [128, 4, 1024], BF16) for i in range(2)]
        xb = SB(es, nc, "xb", [128, NS, 1024])
        xbT = SB(es, nc, "xbT", [128, 8, BLK], BF16)
        a1 = SB(es, nc, "a1", [128, BLK])
        hact = SB(es, nc, "hact", [128, 4, BLK], BF16)
        ysb = [SB(es, nc, "ysb%d" % i, [128, 1024]) for i in range(2)]
        psT = [PS(es, nc, "ps3T%d" % i, [128, 512]) for i in range(2)]
        psH1 = PS(es, nc, "psH1", [128, 512]); psH3 = PS(es, nc, "psH3", [128, 512])
        psY = [PS(es, nc, "psY%d" % i, [128, 512]) for i in range(2)]
        P.dma("sp", ident[:], ident_d, [], ["ident3"], "c3")
        for b in range(NBLK):
            s = b % 2
            for wi, wd in enumerate((w1_d, w3_d, w2_d)):
                P.add("pool", lambda e, wi=wi, wd=wd, b=b: e.indirect_dma_start(
                    ws[wi][:], None, wd, bass.IndirectOffsetOnAxis(widx[:, b:b + 1], 0)),
                    ["widx"], ["ws%d" % wi], dma=True, semkey="ws%d" % wi)
            P.add("act", lambda e, s=s: e.activation(
                w1b[s][:], ws[0][:].rearrange("p (c q h) -> p c h q", c=8, q=128, h=4), AF.Copy), ["ws0"], ["w1b%d" % s])
            P.add("dve", lambda e, s=s: e.tensor_copy(
                w3b[s][:], ws[1][:].rearrange("p (c q h) -> p c h q", c=8, q=128, h=4)), ["ws1"], ["w3b%d" % s])
            P.add("dve", lambda e, s=s: e.tensor_copy(
                w2b[s][:], ws[2][:].rearrange("p (c n) -> p c n", c=4)), ["ws2"], ["w2b%d" % s])
            P.dma("sp", xb[:], xbuf_d[b * BLK:(b + 1) * BLK, :].rearrange("(s p) d -> p s d", p=128), [], ["xb"], "xb")
            for sub in range(NS):
                for c in range(8):
                    P.tr(psT[c // 4][:, (c % 4) * 128:(c % 4 + 1) * 128],
                         xb[:, sub, :].rearrange("p (q c) -> p c q", c=8)[:, c, :], ident[:],
                         ["xb", "ident3"], ["ps3T%d" % (c // 4)])
                for hh in range(2):
                    eng = "act" if hh == 0 else "dve"
                    if eng == "act":
                        P.add("act", lambda e, hh=hh, sub=sub: e.activation(
                            xbT[:, hh * 4:(hh + 1) * 4, sub * 128:(sub + 1) * 128],
                            psT[hh][:].rearrange("p (c n) -> p c n", c=4), AF.Copy), ["ps3T%d" % hh], ["xbT"])
                    else:
                        P.add("dve", lambda e, hh=hh, sub=sub: e.tensor_copy(
                            xbT[:, hh * 4:(hh + 1) * 4, sub * 128:(sub + 1) * 128],
                            psT[hh][:].rearrange("p (c n) -> p c n", c=4)), ["ps3T%d" % hh], ["xbT"])
            for hc in range(4):
                for c in range(8):
                    P.mm(psH1[:, 0:BLK], w1b[s][:, c, hc, :], xbT[:, c, :], c == 0, c == 7, ["w1b%d" % s, "xbT"], ["psH1"])
                for c in range(8):
                    P.mm(psH3[:, 0:BLK], w3b[s][:, c, hc, :], xbT[:, c, :], c == 0, c == 7, ["w3b%d" % s, "xbT"], ["psH3"])
                P.add("act", lambda e: e.activation(a1[:], psH1[:, 0:BLK], AF.Silu), ["psH1"], ["a1"])
                P.add("dve", lambda e, hc=hc: e.tensor_tensor(hact[:, hc, :], a1[:], psH3[:, 0:BLK], ALU.mult),
                      ["a1", "psH3"], ["hact"])
            for sub in range(NS):
                ys = sub % 2
                for hh in range(2):
                    for hc in range(4):
                        P.mm(psY[hh][:], hact[:, hc, sub * 128:(sub + 1) * 128], w2b[s][:, hc, hh * 512:(hh + 1) * 512],
                             hc == 0, hc == 3, ["hact", "w2b%d" % s], ["psY%d" % hh])
                P.add("act", lambda e, ys=ys: e.activation(ysb[ys][:, 0:512], psY[0][:], AF.Copy), ["psY0"], ["ysb%d" % ys])
                P.add("dve", lambda e, ys=ys: e.tensor_copy(ysb[ys][:, 512:1024], psY[1][:]), ["psY1"], ["ysb%d" % ys])
                r0 = b * BLK + sub * 128
                P.dma("sp", ybuf_d[r0:r0 + 128, :], ysb[ys][:], ["ysb%d" % ys], [], "stysb%d" % ys)
    P.barrier()


def phase_t4(P, nc, N, h1_d, ybuf_d, p_d, lng_d, lnb_d, wg_d, wp_d, ident_d, out_d, R):
    NT = N // 128
    dest_i, gates = R["dest_i"], R["gates"]
    with ExitStack() as es:
        wg_b = SB(es, nc, "wg_b", [128, 8, 1024], BF16); wp_b = SB(es, nc, "wp_b", [128, 2, 1024], BF16)
        wst = SB(es, nc, "wst4", [128, 8, 512], F32)
        g_t = SB(es, nc, "g_t4", [128, 1024]); b_t = SB(es, nc, "b_t4", [128, 1024])
        ident = SB(es, nc, "ident4", [128, 128])
        y1 = [SB(es, nc, "y1_%d" % i, [128, 1024]) for i in range(2)]
        y2 = [SB(es, nc, "y2_%d" % i, [128, 1024]) for i in range(2)]
        hh1 = [SB(es, nc, "hh1_%d" % i, [128, 1024]) for i in range(2)]
        pt = [SB(es, nc, "pt%d" % i, [128, 256]) for i in range(2)]
        u = SB(es, nc, "u4", [128, 1024]); h2 = SB(es, nc, "h2", [128, 1024])
        h2T = SB(es, nc, "h2T", [128, 8, 128], BF16); pT = SB(es, nc, "pT", [128, 2, 128], BF16)
        sg = SB(es, nc, "sg", [128, 1024]); ot = [SB(es, nc, "ot%d" % i, [128, 1024]) for i in range(2)]
        st = SB(es, nc, "st4", [128, 2, 6]); mv = SB(es, nc, "mv4", [128, 2])
        sd = SB(es, nc, "sd4", [128, 1]); rs = SB(es, nc, "rs4", [128, 1])
        psT = [PS(es, nc, "ps4T%d" % i, [128, 512]) for i in range(2)]
        psG = [PS(es, nc, "psG%d" % i, [128, 512]) for i in range(2)]
        psP = [PS(es, nc, "psP%d" % i, [128, 512]) for i in range(2)]
        psPT = PS(es, nc, "psPT", [128, 512])
        for hh in range(2):
            P.dma("sp", wst[:], wg_d[:, hh * 512:(hh + 1) * 512].rearrange("(c p) n -> p c n", p=128), [], ["wst4"], "wst4")
            P.add("act", lambda e, hh=hh: e.activation(wg_b[:, :, hh * 512:(hh + 1) * 512], wst[:], AF.Copy), ["wst4"], ["wg_b"])
        for hh in range(2):
            P.dma("sp", wst[:, 0:2, :], wp_d[:, hh * 512:(hh + 1) * 512].rearrange("(c p) n -> p c n", p=128), [], ["wst4"], "wst4")
            P.add("act", lambda e, hh=hh: e.activation(wp_b[:, :, hh * 512:(hh + 1) * 512], wst[:, 0:2, :], AF.Copy), ["wst4"], ["wp_b"])
        P.dma("sp", g_t[:], lng_d.partition_broadcast(128), [], ["L2g"], "c4")
        P.dma("sp", b_t[:], lnb_d.partition_broadcast(128), [], ["L2b"], "c4")
        P.dma("sp", ident[:], ident_d, [], ["ident4"], "c4")
        for i in range(NT):
            s = i % 2
            n0 = i * 128
            ky1, ky2, kh, kp, ko = "y1_%d" % s, "y2_%d" % s, "hh1_%d" % s, "pt%d" % s, "ot%d" % s
            P.add("pool", lambda e, i=i, s=s: e.indirect_dma_start(
                y1[s][:], None, ybuf_d, bass.IndirectOffsetOnAxis(dest_i[:, 2 * i:2 * i + 1], 0)),
                ["dest_i"], [ky1], dma=True, semkey=ky1)
            P.add("pool", lambda e, i=i, s=s: e.indirect_dma_start(
                y2[s][:], None, ybuf_d, bass.IndirectOffsetOnAxis(dest_i[:, 2 * i + 1:2 * i + 2], 0)),
                ["dest_i"], [ky2], dma=True, semkey=ky2)
            P.dma("sp", hh1[s][:], h1_d[n0:n0 + 128, :], [], [kh], kh)
            P.dma("sp", pt[s][:], p_d[n0:n0 + 128, :], [], [kp], kp)
            P.add("dve", lambda e, i=i, s=s: e.tensor_scalar(y1[s][:], y1[s][:], gates[:, i, 0:1], None, ALU.mult),
                  [ky1, "gates"], [ky1])
            P.add("dve", lambda e, i=i, s=s: e.scalar_tensor_tensor(y1[s][:], y2[s][:], gates[:, i, 1:2], y1[s][:], ALU.mult, ALU.add),
                  [ky1, ky2, "gates"], [ky1])
            P.add("dve", lambda e, s=s: e.scalar_tensor_tensor(u[:], hh1[s][:], ALPHA, y1[s][:], ALU.mult, ALU.add),
                  [kh, ky1], ["L2u"])
            P.add("dve", lambda e: e.bn_stats(st[:, 0, :], u[:, 0:512]), ["L2u"], ["L2st0"])
            P.add("dve", lambda e: e.bn_stats(st[:, 1, :], u[:, 512:1024]), ["L2u"], ["L2st1"])
            P.add("dve", lambda e: e.bn_aggr(mv[:], st[:].rearrange("p a b -> p (a b)")), ["L2st0", "L2st1"], ["L2mv"])
            P.add("act", lambda e: e.activation(sd[:], mv[:, 1:2], AF.Sqrt, bias=LN_EPS, scale=1.0), ["L2mv"], ["L2sd"])
            P.add("dve", lambda e: e.reciprocal(rs[:], sd[:]), ["L2sd"], ["L2rs"])
            P.add("dve", lambda e: e.tensor_scalar(h2[:], u[:], mv[:, 0:1], rs[:, 0:1], ALU.subtract, ALU.mult),
                  ["L2u", "L2mv", "L2rs"], ["h2"])
            P.add("pool", lambda e: e.tensor_tensor(h2[:], h2[:], g_t[:], ALU.mult), ["h2", "L2g"], ["h2"])
            P.add("pool", lambda e: e.tensor_tensor(h2[:], h2[:], b_t[:], ALU.add), ["h2", "L2b"], ["h2"])
            for c in range(8):
                P.tr(psT[c // 4][:, (c % 4) * 128:(c % 4 + 1) * 128], h2[:, c * 128:(c + 1) * 128], ident[:],
                     ["h2", "ident4"], ["ps4T%d" % (c // 4)])
            for hh in range(2):
                P.add("act", lambda e, hh=hh: e.activation(
                    h2T[:, hh * 4:(hh + 1) * 4, :], psT[hh][:].rearrange("p (c n) -> p c n", c=4), AF.Copy),
                    ["ps4T%d" % hh], ["h2T"])
            for c in range(2):
                P.tr(psPT[:, c * 128:(c + 1) * 128], pt[s][:, c * 128:(c + 1) * 128], ident[:], [kp, "ident4"], ["psPT"])
            P.add("dve", lambda e: e.tensor_copy(pT[:], psPT[:, 0:256].rearrange("p (c n) -> p c n", c=2)), ["psPT"], ["pT"])
            for hh in range(2):
                for c in range(8):
                    P.mm(psG[hh][:], h2T[:, c, :], wg_b[:, c, hh * 512:(hh + 1) * 512], c == 0, c == 7, ["h2T", "wg_b"], ["psG%d" % hh])
                for c in range(2):
                    P.mm(psP[hh][:], pT[:, c, :], wp_b[:, c, hh * 512:(hh + 1) * 512], c == 0, c == 1, ["pT", "wp_b"], ["psP%d" % hh])
                P.add("act", lambda e, hh=hh: e.activation(sg[:, hh * 512:(hh + 1) * 512], psG[hh][:], AF.Sigmoid), ["psG%d" % hh], ["sg"])
                P.add("dve", lambda e, hh=hh: e.tensor_tensor(sg[:, hh * 512:(hh + 1) * 512], sg[:, hh * 512:(hh + 1) * 512], psP[hh][:], ALU.mult),
                      ["sg", "psP%d" % hh], ["sg"])
            P.add("pool", lambda e, s=s: e.tensor_tensor(ot[s][:], sg[:], h2[:], ALU.add), ["sg", "h2"], [ko])
            P.dma("sp", out_d[n0:n0 + 128, :], ot[s][:], [ko], [], "st" + ko)
    P.barrier()


CH = 64
STAGE = int(os.environ.get('STAGE', '9'))


def gemm_fm(P, nc, N, x_d, w_d, C, out_d, ident_d, tag):
    NCC = (C + 127) // 128
    with ExitStack() as es:
        wb = SB(es, nc, tag + "wb", [128, 8, C], BF16)
        wst = [SB(es, nc, tag + "wst%d" % i, [128, 8, 512]) for i in range(2)]
        ident = SB(es, nc, tag + "id", [128, 128])
        xt = SB(es, nc, tag + "xt", [128, 4, 1024])
        xT = SB(es, nc, tag + "xT", [128, 8, 512], BF16)
        ot = [SB(es, nc, tag + "ot%d" % i, [128, 512]) for i in range(3)]
        psT = [PS(es, nc, tag + "psT%d" % i, [128, 512]) for i in range(2)]
        psO = [PS(es, nc, tag + "psO%d" % i, [128, 512]) for i in range(2)]
        P.dma("sp", ident[:], ident_d, [], [tag + "id"], tag + "id")
        npc = (C + 511) // 512
        for pc in range(npc):
            s = pc % 2
            c0 = pc * 512
            cw = min(512, C - c0)
            P.dma("sp" if s == 0 else "pool", wst[s][:, :, 0:cw], w_d[:, c0:c0 + cw].rearrange("(c p) n -> p c n", p=128),
                  [], [tag + "wst%d" % s], tag + "wst%d" % s)
            if s == 0:
                P.add("act", lambda e, s=s, c0=c0, cw=cw: e.activation(wb[:, :, c0:c0 + cw], wst[s][:, :, 0:cw], AF.Copy),
                      [tag + "wst%d" % s], [tag + "wb"])
            else:
                P.add("dve", lambda e, s=s, c0=c0, cw=cw: e.tensor_copy(wb[:, :, c0:c0 + cw], wst[s][:, :, 0:cw]),
                      [tag + "wst%d" % s], [tag + "wb"])
        k = 0
        for tt in range(N // 512):
            n0 = tt * 512
            P.dma("sp", xt[:], x_d[n0:n0 + 512, :].rearrange("(s p) d -> p s d", p=128), [], [tag + "xt"], tag + "xt")
            for c in range(8):
                b = c % 2
                for sub in range(4):
                    P.tr(psT[b][:, sub * 128:(sub + 1) * 128], xt[:, sub, c * 128:(c + 1) * 128], ident[:],
                         [tag + "xt", tag + "id"], [tag + "psT%d" % b])
                if b == 0:
                    P.add("act", lambda e, c=c, b=b: e.activation(xT[:, c, :], psT[b][:], AF.Copy), [tag + "psT%d" % b], [tag + "xT"])
                else:
                    P.add("dve", lambda e, c=c, b=b: e.tensor_copy(xT[:, c, :], psT[b][:]), [tag + "psT%d" % b], [tag + "xT"])
            for cc in range(NCC):
                M = min(128, C - cc * 128)
                b = cc % 2
                o = k % 3
                k += 1
                for c in range(8):
                    P.mm(psO[b][0:M, :], wb[:, c, cc * 128:cc * 128 + M], xT[:, c, :], c == 0, c == 7,
                         [tag + "wb", tag + "xT"], [tag + "psO%d" % b])
                if b == 0:
                    P.add("act", lambda e, b=b, o=o, M=M: e.activation(ot[o][0:M, :], psO[b][0:M, :], AF.Copy),
                          [tag + "psO%d" % b], [tag + "ot%d" % o])
                else:
                    P.add("dve", lambda e, b=b, o=o, M=M: e.tensor_copy(ot[o][0:M, :], psO[b][0:M, :]),
                          [tag + "psO%d" % b], [tag + "ot%d" % o])
                P.dma("sp" if cc % 2 == 0 else "pool", out_d[cc * 128:cc * 128 + M, n0:n0 + 512], ot[o][0:M, :],
                      [tag + "ot%d" % o], [], tag + "sto%d" % o)
    P.barrier()


def rwkv_prep(P, nc, NB, T, zT_d, prm, S):
    TW = 512
    NW = T // TW
    NCW = TW // CH
    C1 = -math.exp(-0.5)
    with ExitStack() as es:
        col = SB(es, nc, "rp_col", [128, 4, 8])
        mul = SB(es, nc, "rp_mul", [128, 2])
        w2 = SB(es, nc, "rp_w2", [128, 512])
        g2 = SB(es, nc, "rp_g2", [128, 512])
        blk1 = SB(es, nc, "rp_blk1", [128, 128]); ident = SB(es, nc, "rp_id", [128, 128])
        rmask = SB(es, nc, "rp_rmask", [128, TW])
        lo = [SB(es, nc, "rp_lo%d" % i, [128, TW + 1]) for i in range(2)]
        lm = [SB(es, nc, "rp_lm%d" % i, [128, TW]) for i in range(2)]
        raw = [SB(es, nc, "rp_raw%d" % i, [128, TW + 1]) for i in range(3)]
        r = SB(es, nc, "rp_r", [128, TW]); k = SB(es, nc, "rp_k", [128, TW]); v = SB(es, nc, "rp_v", [128, TW])
        ld = SB(es, nc, "rp_ld", [128, TW]); a = SB(es, nc, "rp_a", [128, TW]); g = SB(es, nc, "rp_g", [128, TW])
        kk = SB(es, nc, "rp_kk", [128, TW]); t1 = SB(es, nc, "rp_t1", [128, TW]); t2 = SB(es, nc, "rp_t2", [128, TW])
        km = SB(es, nc, "rp_km", [128, TW]); be = SB(es, nc, "rp_be", [128, TW])
        c = SB(es, nc, "rp_c", [128, TW]); ep = SB(es, nc, "rp_ep", [128, TW]); en = SB(es, nc, "rp_en", [128, TW])
        ex = SB(es, nc, "rp_ex", [128, TW])
        o_rt = SB(es, nc, "rp_ort", [128, TW]); o_kt = SB(es, nc, "rp_okt", [128, TW]); o_kp = SB(es, nc, "rp_okp", [128, TW])
        o_bt = SB(es, nc, "rp_obt", [128, TW]); o_ktl = SB(es, nc, "rp_oktl", [128, TW]); o_btl = SB(es, nc, "rp_obtl", [128, TW])
        o_bon = SB(es, nc, "rp_obon", [128, TW]); o_pc = SB(es, nc, "rp_opc", [128, NCW])
        vtk = SB(es, nc, "rp_vtk", [128, 4, 128])
        ps1 = PS(es, nc, "rp_ps1", [128, 512]); ps2 = PS(es, nc, "rp_ps2", [128, 512]); ps3 = PS(es, nc, "rp_ps3", [128, 512])
        ps4 = PS(es, nc, "rp_ps4", [128, 512]); ps5 = PS(es, nc, "rp_ps5", [128, 512]); ps6 = PS(es, nc, "rp_ps6", [128, 512])

        def colload(j, src, fcmul=True):
            P.dma("pool", col[:, :, j:j + 1], src.rearrange("(f p o) -> p f o", p=128, o=1), [], ["rp_col"], "rp_c0", allow_slow_non_contiguous=True)
        colload(0, prm["mu"][0:512]); colload(1, prm["mu"][512:1024]); colload(2, prm["mu"][1024:1536])
        colload(3, prm["w0"]); colload(4, prm["a0"]); colload(5, prm["k_k"]); colload(6, prm["k_a"]); colload(7, prm["r_k"])
        P.dma("pool", mul[:], prm["mu"][1536:1792].rearrange("(f p) -> p f", p=128), [], ["rp_mul"], "rp_c0", allow_slow_non_contiguous=True)
        P.dma("pool", w2[0:64, :], prm["w2"], [], ["rp_w2"], "rp_c0")
        P.dma("pool", w2[64:128, :], prm["a2"], [], ["rp_w2"], "rp_c0")
        P.dma("pool", g2[:], prm["g2"], [], ["rp_g2"], "rp_c0")
        P.dma("pool", blk1[:], prm["blk1"], [], ["rp_blk1"], "rp_c0")
        P.dma("pool", ident[:], prm["ident"], [], ["rp_id"], "rp_c0")
        P.dma("pool", rmask[:], prm["rmask"].partition_broadcast(128), [], ["rp_rmask"], "rp_c0")

        def load_shift(dst, row0, n0, t0, key, q):
            if t0 == 0:
                P.add("pool", lambda e: e.memset(dst[:, 0:1], 0.0), [], [key])
                P.dma(q, dst[:, 1:TW + 1], zT_d[row0:row0 + 128, n0:n0 + TW], [], [key], key)
            else:
                P.dma(q, dst[:, :], zT_d[row0:row0 + 128, n0 - 1:n0 + TW], [], [key], key)

        def mix(dst, src, mu_ap, key_src, key_dst, eng="dve"):
            P.add("pool", lambda e: e.tensor_tensor(t2[:], src[:, 0:TW], src[:, 1:TW + 1], ALU.subtract), [key_src], ["rp_t2"])
            P.add("dve", lambda e: e.scalar_tensor_tensor(dst, t2[:], mu_ap, src[:, 1:TW + 1], ALU.mult, ALU.add),
                  ["rp_t2", key_src, "rp_col", "rp_mul"], [key_dst])

        for b in range(NB):
            for w in range(NW):
                t0 = w * TW
                n0 = b * T + t0
                for i in range(2):
                    load_shift(lo[i], 1536 + 128 * i, n0, t0, "rp_lo%d" % i, "sp")
                    mix(lm[i][:], lo[i], mul[:, i:i + 1], "rp_lo%d" % i, "rp_lm%d" % i)
                P.add("act", lambda e: e.activation(lm[0][0:64, :], lm[0][0:64, :], AF.Tanh), ["rp_lm0"], ["rp_lm0"])
                P.add("act", lambda e: e.activation(lm[1][:], lm[1][:], AF.Sigmoid), ["rp_lm1"], ["rp_lm1"])
                for fc in range(4):
                    for j, dst in enumerate((r, k, v)):
                        load_shift(raw[j], j * 512 + fc * 128, n0, t0, "rp_raw%d" % j, "sp")
                        mix(dst[:], raw[j], col[:, fc, j:j + 1], "rp_raw%d" % j, ("rp_r", "rp_k", "rp_v")[j])
                    fs = slice(fc * 128, (fc + 1) * 128)
                    P.mm(ps1[:, 0:TW], w2[0:64, fs], lm[0][0:64, :], True, True, ["rp_w2", "rp_lm0"], ["rp_ps1"])
                    P.add("act", lambda e, fc=fc: e.activation(ld[:], ps1[:, 0:TW], AF.Sigmoid, bias=col[:, fc, 3:4], scale=1.0),
                          ["rp_ps1", "rp_col"], ["rp_ld"])
                    P.add("pool", lambda e: e.tensor_scalar(ld[:], ld[:], C1, None, ALU.mult), ["rp_ld"], ["rp_ld"])
                    P.mm(ps2[:, 0:TW], w2[64:128, fs], lm[0][64:128, :], True, True, ["rp_w2", "rp_lm0"], ["rp_ps2"])
                    P.add("act", lambda e, fc=fc: e.activation(a[:], ps2[:, 0:TW], AF.Sigmoid, bias=col[:, fc, 4:5], scale=1.0),
                          ["rp_ps2", "rp_col"], ["rp_a"])
                    P.mm(ps3[:, 0:TW], g2[:, fs], lm[1][:], True, True, ["rp_g2", "rp_lm1"], ["rp_ps3"])
                    P.add("act", lambda e: e.activation(g[:], ps3[:, 0:TW], AF.Copy), ["rp_ps3"], ["rp_g"])
                    P.dma("pool", S["g"][fs, n0:n0 + TW], g[:], ["rp_g"], [], "rp_stg")
                    P.add("dve", lambda e, fc=fc: e.tensor_scalar(kk[:], k[:], col[:, fc, 5:6], None, ALU.mult), ["rp_k", "rp_col"], ["rp_kk"])
                    P.add("act", lambda e: e.activation(t1[:], kk[:], AF.Square), ["rp_kk"], ["rp_t1"])
                    P.mm(ps4[:, 0:TW], blk1[:], t1[:], True, True, ["rp_blk1", "rp_t1"], ["rp_ps4"])
                    P.add("act", lambda e: e.activation(t1[:], ps4[:, 0:TW], AF.Sqrt), ["rp_ps4"], ["rp_t1"])
                    P.add("dve", lambda e: e.tensor_scalar(t1[:], t1[:], 1e-12, None, ALU.max), ["rp_t1"], ["rp_t1"])
                    P.add("dve", lambda e: e.reciprocal(t1[:], t1[:]), ["rp_t1"], ["rp_t1"])
                    P.add("dve", lambda e: e.tensor_tensor(kk[:], kk[:], t1[:], ALU.mult), ["rp_kk", "rp_t1"], ["rp_kk"])
                    P.add("dve", lambda e, fc=fc: e.tensor_scalar(km[:], a[:], -1.0, col[:, fc, 6:7], ALU.add, ALU.mult), ["rp_a", "rp_col"], ["rp_km"])
                    P.add("dve", lambda e: e.scalar_tensor_tensor(km[:], km[:], 1.0, k[:], ALU.add, ALU.mult), ["rp_km", "rp_k"], ["rp_km"])
                    P.add("pool", lambda e: e.tensor_tensor(be[:], kk[:], a[:], ALU.mult), ["rp_kk", "rp_a"], ["rp_be"])
                    P.add("dve", lambda e: e.tensor_tensor_scan(c[:], rmask[:], ld[:], 0.0, ALU.mult, ALU.add), ["rp_rmask", "rp_ld"], ["rp_c"])
                    P.add("act", lambda e: e.activation(ep[:], c[:], AF.Exp), ["rp_c"], ["rp_ep"])
                    P.add("act", lambda e: e.activation(en[:], c[:], AF.Exp, scale=-1.0), ["rp_c"], ["rp_en"])
                    P.add("pool", lambda e: e.tensor_tensor(t2[:], c[:], ld[:], ALU.subtract), ["rp_c", "rp_ld"], ["rp_t2"])
                    P.add("act", lambda e: e.activation(ex[:], t2[:], AF.Exp), ["rp_t2"], ["rp_ex"])
                    P.add("dve", lambda e: e.tensor_tensor(o_rt[:], r[:], ep[:], ALU.mult), ["rp_r", "rp_ep"], ["rp_ort"])
                    P.add("pool", lambda e: e.tensor_tensor(o_kp[:], kk[:], ex[:], ALU.mult), ["rp_kk", "rp_ex"], ["rp_okp"])
                    P.add("dve", lambda e: e.tensor_tensor(o_kt[:], km[:], en[:], ALU.mult), ["rp_km", "rp_en"], ["rp_okt"])
                    P.add("pool", lambda e: e.tensor_tensor(o_bt[:], be[:], en[:], ALU.mult), ["rp_be", "rp_en"], ["rp_obt"])
                    P.add("dve", lambda e: e.tensor_copy(o_pc[:], ep[:].rearrange("p (n c) -> p n c", c=CH)[:, :, CH - 1]), ["rp_ep"], ["rp_opc"])
                    pcb = o_pc[:].unsqueeze(2).to_broadcast([128, NCW, CH])
                    P.add("dve", lambda e, pcb=pcb: e.tensor_tensor(o_ktl[:].rearrange("p (n c) -> p n c", c=CH),
                                                                     o_kt[:].rearrange("p (n c) -> p n c", c=CH), pcb, ALU.mult),
                          ["rp_okt", "rp_opc"], ["rp_oktl"])
                    P.add("dve", lambda e, pcb=pcb: e.scalar_tensor_tensor(o_btl[:].rearrange("p (n c) -> p n c", c=CH),
                                                                            o_bt[:].rearrange("p (n c) -> p n c", c=CH), -1.0, pcb,
                                                                            ALU.mult, ALU.mult),
                          ["rp_obt", "rp_opc"], ["rp_obtl"])
                    P.add("dve", lambda e, fc=fc: e.scalar_tensor_tensor(t1[:], r[:], col[:, fc, 7:8], km[:], ALU.mult, ALU.mult),
                          ["rp_r", "rp_km", "rp_col", "rp_t1"], ["rp_t1"])
                    P.mm(ps5[:, 0:TW], blk1[:], t1[:], True, True, ["rp_blk1", "rp_t1"], ["rp_ps5"])
                    P.add("dve", lambda e: e.tensor_tensor(o_bon[:], ps5[:, 0:TW], v[:], ALU.mult), ["rp_ps5", "rp_v"], ["rp_obon"])
                    for sub in range(TW // 128):
                        P.tr(ps6[:, sub * 128:(sub + 1) * 128], v[:, sub * 128:(sub + 1) * 128], ident[:], ["rp_v", "rp_id"], ["rp_ps6"])
                    P.add("act", lambda e: e.activation(vtk[:], ps6[:].rearrange("p (s f) -> p s f", f=128), AF.Copy), ["rp_ps6"], ["rp_vtk"])
                    P.dma("pool", S["vtok"][n0:n0 + TW, fs].rearrange("(s p) f -> p s f", p=128), vtk[:], ["rp_vtk"], [], "rp_stv")
                    for key, tl, dd in (("rp_ort", o_rt, "rt"), ("rp_okp", o_kp, "kp"), ("rp_okt", o_kt, "kt"), ("rp_obt", o_bt, "bt"),
                                        ("rp_oktl", o_ktl, "ktl"), ("rp_obtl", o_btl, "btl"), ("rp_obon", o_bon, "bon")):
                        P.dma("sp", S[dd][fs, n0:n0 + TW], tl[:], [key], [], "st" + key)
                    P.dma("sp", S["pc"][fs, n0 // CH:n0 // CH + NCW], o_pc[:], ["rp_opc"], [], "strp_opc")
    P.barrier()


def chunk_scan(P, nc, NB, T, D, delta, DK, DV, H, yT_d, yrow0, tag, gn_eps=0.0):
    TW = 256
    NCW = TW // CH
    NW = T // TW
    HV = H * DV
    names = ["rt", "kt", "ktl", "g"] + (["kp", "bt", "btl", "bon"] if delta else [])
    K = lambda s: tag + s
    with ExitStack() as es:
        A = {n: SB(es, nc, tag + "A" + n, [DK if n not in ("g", "bon") else DV, H, TW]) for n in names}
        vt = SB(es, nc, tag + "vt", [CH, NCW, HV])
        pcw = SB(es, nc, tag + "pcw", [DK, H, NCW])
        Hs = SB(es, nc, tag + "H", [DK, H, DV])
        masks = SB(es, nc, tag + "masks", [64, 320]); id64 = SB(es, nc, tag + "id64", [64, 64])
        ident = SB(es, nc, tag + "ident", [128, 128])
        am = SB(es, nc, tag + "am", [64, H, 320 if delta else 64])
        ktok = SB(es, nc, tag + "ktok", [64, H, 2 if delta else 1, DK])
        yw = SB(es, nc, tag + "yw", [DV, H, TW])
        on = SB(es, nc, tag + "on", [64, H, DV]); sq = SB(es, nc, tag + "sq", [64, H, DV]); oc = SB(es, nc, tag + "oc", [64, H, DV])
        s12 = SB(es, nc, tag + "s12", [64, 4, H])
        if delta:
            R = [SB(es, nc, tag + "R%d" % i, [64, H, 64]) for i in range(2)]
            pw = [SB(es, nc, tag + "pw%d" % i, [64, H, 2, 64]) for i in range(2)]
            rhs = SB(es, nc, tag + "rhs", [64, H, DV]); us = SB(es, nc, tag + "us", [64, H, DV])
            gng = SB(es, nc, tag + "gng", [DV, H]); gnb = SB(es, nc, tag + "gnb", [DV, H])
        else:
            normg = SB(es, nc, tag + "normg", [DV, 1])
        nbO = (H * DV * 4 + 2047) // 2048
        psA = [PS(es, nc, tag + "psA%d" % i, [128, 512]) for i in range(2)]
        psB = [PS(es, nc, tag + "psB%d" % i, [128, 512]) for i in range(2)]
        psC = PS(es, nc, tag + "psC", [128, 512])
        psD = PS(es, nc, tag + "psD", [128, 512])
        psO = PS(es, nc, tag + "psO", [128, 512])
        psT = PS(es, nc, tag + "psT", [128, 512])
        P.dma("pool", masks[:], D["masks"], [], [K("masks")], K("c0"))
        P.dma("pool", id64[:], D["id64"], [], [K("id64")], K("c0"))
        P.dma("pool", ident[:], D["ident"], [], [K("ident")], K("c0"))
        if delta:
            P.dma("pool", gng[:], D["gng"].rearrange("(h p) -> p h", p=DV), [], [K("gn")], K("c0"), allow_slow_non_contiguous=True)
            P.dma("pool", gnb[:], D["gnb"].rearrange("(h p) -> p h", p=DV), [], [K("gn")], K("c0"), allow_slow_non_contiguous=True)
        else:
            P.dma("pool", normg[:], D["normg"].rearrange("(p o) -> p o", o=1), [], [K("gn")], K("c0"))

        def psO_h(h):
            if DV == 64:
                return psO[0:64, h * 64:(h + 1) * 64]
            return (psB[h // 4])[0:64, (h % 4) * 128:(h % 4 + 1) * 128]

        def psH_h(h):
            if DV == 64:
                return psD[0:DK, h * 64:(h + 1) * 64]
            return (psC if h < 4 else psD)[0:DK, (h % 4) * 128:(h % 4 + 1) * 128]
        kO = [K("psO")] if DV == 64 else [K("psB0"), K("psB1")]
        kH = [K("psD")] if DV == 64 else [K("psC"), K("psD")]

        for b in range(NB):
            P.add("dve", lambda e: e.memset(Hs[:], 0.0), [], [K("H")])
            for w in range(NW):
                n0 = b * T + w * TW
                for i, n in enumerate(names):
                    p_ = DK if n not in ("g", "bon") else DV
                    P.dma("sp" if i % 2 == 0 else "act", A[n][:], D[n][:, n0:n0 + TW].rearrange("(h p) n -> p h n", p=p_),
                          [], [K("A" + n)], K("A" + n))
                P.dma("sp", vt[:], D["vtok"][n0:n0 + TW, :].rearrange("(c s) f -> s c f", s=CH), [], [K("vt")], K("vt"))
                P.dma("sp", pcw[:], D["pc"][:, n0 // CH:n0 // CH + NCW].rearrange("(h p) c -> p h c", p=DK), [], [K("pcw")], K("pcw"))
                for n in range(NCW):
                    cs = slice(n * CH, (n + 1) * CH)
                    for h in range(H):
                        pa = psA[h % 2]
                        kpa = K("psA%d" % (h % 2))
                        if delta:
                            pairs = (("bt", "kp"), ("kp", "bt"), ("kt", "kp"), ("kt", "rt"), ("bt", "rt"))
                        else:
                            pairs = (("kt", "rt"),)
                        for q, (l, r_) in enumerate(pairs):
                            P.mm(pa[0:64, q * 64:(q + 1) * 64], A[l][:, h, cs], A[r_][:, h, cs], True, True,
                                 [K("A" + l), K("A" + r_)], [kpa])
                        if delta:
                            P.add("dve", lambda e, h=h, pa=pa: e.tensor_tensor(am[:, h, :], pa[0:64, 0:320], masks[:], ALU.mult),
                                  [kpa, K("masks")], [K("am")])
                        else:
                            P.add("dve", lambda e, h=h, pa=pa: e.tensor_tensor(am[:, h, :], pa[0:64, 0:64], masks[:, 192:256], ALU.mult),
                                  [kpa, K("masks")], [K("am")])
                    if STAGE < 2: continue
                    nk = 2 if delta else 1
                    for h in range(H):
                        for q, nm in enumerate(("ktl", "btl")[:nk]):
                            if DK == 64:
                                dst = psA[(h * nk + q) // 8][0:64, ((h * nk + q) % 8) * 64:((h * nk + q) % 8 + 1) * 64]
                                kd = K("psA%d" % ((h * nk + q) // 8))
                            else:
                                dst = psA[h // 4][0:64, (h % 4) * 128:(h % 4 + 1) * 128]
                                kd = K("psA%d" % (h // 4))
                            P.tr(dst, A[nm][:, h, cs], ident[0:DK, 0:DK], [K("A" + nm), K("ident"), K("am")], [kd])
                    for half in range(2):
                        hs = slice(half * (H // 2), (half + 1) * (H // 2))
                        src = psA[half][0:64, 0:(H // 2) * nk * DK].rearrange("p (h q d) -> p h q d", q=nk, d=DK)
                        P.add("act", lambda e, hs=hs, src=src: e.activation(ktok[:, hs, :, :], src, AF.Copy),
                              [K("psA%d" % half)], [K("ktok")])
                    if STAGE < 3: continue
                    if delta:
                        P.add("dve", lambda e: e.tensor_tensor(R[0][:], am[:, :, 0:64], id64[:].unsqueeze(1).to_broadcast([64, H, 64]), ALU.add),
                              [K("am"), K("id64")], [K("R0")])
                        cur = 0
                        for j in range(1, 6):
                            src_p = (lambda h: am[:, h, 0:64]) if j == 1 else (lambda h, pwp=pw[(j - 1) % 2]: pwp[:, h, 0, :])
                            src_pt = (lambda h: am[:, h, 64:128]) if j == 1 else (lambda h, pwp=pw[(j - 1) % 2]: pwp[:, h, 1, :])
                            ksrc = K("am") if j == 1 else K("pw%d" % ((j - 1) % 2))
                            for h in range(H):
                                pb = psB[h // 4]
                                o0 = (h % 4) * 128
                                if j < 5:
                                    P.mm(pb[0:64, o0:o0 + 64], src_pt(h), src_p(h), True, True, [ksrc], [K("psB%d" % (h // 4))])
                                P.mm(pb[0:64, o0 + 64:o0 + 128], src_p(h), src_pt(h), True, True, [ksrc], [K("psB%d" % (h // 4))])
                            for half in range(2):
                                hs = slice(half * 4, half * 4 + 4)
                                eng = "act" if half == 0 else "dve"
                                srcv = psB[half][0:64, :].rearrange("p (h q d) -> p h q d", q=2, d=64)
                                if j < 5:
                                    if eng == "act":
                                        P.add("act", lambda e, hs=hs, srcv=srcv, j=j: e.activation(pw[j % 2][:, hs, :, :], srcv, AF.Copy),
                                              [K("psB%d" % half)], [K("pw%d" % (j % 2))])
                                    else:
                                        P.add("dve", lambda e, hs=hs, srcv=srcv, j=j: e.tensor_copy(pw[j % 2][:, hs, :, :], srcv),
                                              [K("psB%d" % half)], [K("pw%d" % (j % 2))])
                                else:
                                    if eng == "act":
                                        P.add("act", lambda e, hs=hs, srcv=srcv, j=j: e.activation(pw[j % 2][:, hs, 1, :], srcv[:, :, 1, :], AF.Copy),
                                              [K("psB%d" % half)], [K("pw%d" % (j % 2))])
                                    else:
                                        P.add("dve", lambda e, hs=hs, srcv=srcv, j=j: e.tensor_copy(pw[j % 2][:, hs, 1, :], srcv[:, :, 1, :]),
                                              [K("psB%d" % half)], [K("pw%d" % (j % 2))])
                            for h in range(H):
                                P.mm(psC[0:64, h * 64:(h + 1) * 64], pw[j % 2][:, h, 1, :], R[cur][:, h, :], True, True,
                                     [K("pw%d" % (j % 2)), K("R%d" % cur)], [K("psC")])
                            P.add("dve", lambda e, cur=cur: e.tensor_tensor(R[1 - cur][:], R[cur][:], psC[0:64, :].rearrange("p (h d) -> p h d", d=64), ALU.add),
                                  [K("R%d" % cur), K("psC")], [K("R%d" % (1 - cur))])
                            cur = 1 - cur
                        if STAGE < 4: continue
                        for h in range(H):
                            P.mm(psD[0:64, h * 64:(h + 1) * 64], A["kp"][:, h, cs], Hs[:, h, :], True, False, [K("Akp"), K("H")], [K("psD")])
                            P.mm(psD[0:64, h * 64:(h + 1) * 64], am[:, h, 128:192], vt[:, n, h * DV:(h + 1) * DV], False, True,
                                 [K("am"), K("vt")], [K("psD")])
                        P.add("act", lambda e: e.activation(rhs[:], psD[0:64, :].rearrange("p (h d) -> p h d", d=64), AF.Copy), [K("psD")], [K("rhs")])
                        for h in range(H):
                            P.mm(psC[0:64, h * 64:(h + 1) * 64], R[cur][:, h, :], rhs[:, h, :], True, True, [K("R%d" % cur), K("rhs")], [K("psC")])
                        P.add("dve", lambda e: e.tensor_copy(us[:], psC[0:64, :].rearrange("p (h d) -> p h d", d=64)), [K("psC")], [K("us")])
                    if STAGE < 5: continue
                    for h in range(H):
                        a3 = am[:, h, 192:256] if delta else am[:, h, :]
                        P.mm(psO_h(h), A["rt"][:, h, cs], Hs[:, h, :], True, False, [K("Art"), K("H")], kO)
                        P.mm(psO_h(h), a3, vt[:, n, h * DV:(h + 1) * DV], False, not delta, [K("am"), K("vt")], kO)
                        if delta:
                            P.mm(psO_h(h), am[:, h, 256:320], us[:, h, :], False, True, [K("am"), K("us")], kO)
                    for h in range(H):
                        P.mm(psH_h(h), ktok[:, h, 0, :], vt[:, n, h * DV:(h + 1) * DV], True, not delta, [K("ktok"), K("vt")], kH)
                        if delta:
                            P.mm(psH_h(h), ktok[:, h, 1, :], us[:, h, :], False, True, [K("ktok"), K("us")], kH)
                    if STAGE < 6: continue
                    if DV == 64:
                        o3p = psO[0:64, :].rearrange("p (h d) -> p h d", d=64)
                        P.add("act", lambda e, o3p=o3p: e.activation(oc[:], o3p, AF.Copy), kO, [K("oc")])
                        o3 = oc[:]
                        kO_ = [K("oc")]
                        P.add("dve", lambda e, o3=o3: e.reduce_sum(s12[:, 0, :], o3, AX.X), kO_, [K("s12")])
                        P.add("act", lambda e, o3=o3: e.activation(sq[:], o3, AF.Square), kO_, [K("sq")])
                        P.add("dve", lambda e: e.reduce_sum(s12[:, 1, :], sq[:], AX.X), [K("sq")], [K("s12")])
                        P.add("dve", lambda e: e.tensor_scalar(s12[:, 0, :], s12[:, 0, :], 1.0 / DV, None, ALU.mult), [K("s12")], [K("s12")])
                        P.add("dve", lambda e: e.tensor_tensor(s12[:, 2, :], s12[:, 0, :], s12[:, 0, :], ALU.mult), [K("s12")], [K("s12")])
                        P.add("dve", lambda e: e.scalar_tensor_tensor(s12[:, 1, :], s12[:, 1, :], 1.0 / DV, s12[:, 2, :], ALU.mult, ALU.subtract),
                              [K("s12")], [K("s12")])
                        P.add("act", lambda e: e.activation(s12[:, 1, :], s12[:, 1, :], AF.Sqrt, bias=gn_eps, scale=1.0), [K("s12")], [K("s12")])
                        P.add("dve", lambda e: e.reciprocal(s12[:, 1, :], s12[:, 1, :]), [K("s12")], [K("s12")])
                        P.add("dve", lambda e, o3=o3: e.tensor_tensor(on[:], o3, s12[:, 0, :].unsqueeze(2).to_broadcast([64, H, DV]), ALU.subtract),
                              kO_ + [K("s12")], [K("on")])
                        P.add("dve", lambda e: e.tensor_tensor(on[:], on[:], s12[:, 1, :].unsqueeze(2).to_broadcast([64, H, DV]), ALU.mult),
                              [K("on"), K("s12")], [K("on")])
                    else:
                        for half in range(2):
                            o3 = psB[half][0:64, :].rearrange("p (h d) -> p h d", d=128)
                            hs = slice(half * 4, half * 4 + 4)
                            P.add("act", lambda e, o3=o3, hs=hs: e.activation(sq[:, hs, :], o3, AF.Square), [K("psB%d" % half)], [K("sq")])
                        P.add("dve", lambda e: e.reduce_sum(s12[:, 1, :], sq[:], AX.X), [K("sq")], [K("s12")])
                        P.add("act", lambda e: e.activation(s12[:, 1, :], s12[:, 1, :], AF.Sqrt, bias=gn_eps, scale=1.0 / DV), [K("s12")], [K("s12")])
                        P.add("dve", lambda e: e.reciprocal(s12[:, 1, :], s12[:, 1, :]), [K("s12")], [K("s12")])
                        for half in range(2):
                            o3 = psB[half][0:64, :].rearrange("p (h d) -> p h d", d=128)
                            hs = slice(half * 4, half * 4 + 4)
                            P.add("dve", lambda e, o3=o3, hs=hs: e.tensor_tensor(on[:, hs, :], o3, s12[:, 1, hs].unsqueeze(2).to_broadcast([64, 4, DV]), ALU.mult),
                                  [K("psB%d" % half), K("s12")], [K("on")])
                    if DV == 64:
                        h3 = psD[0:DK, :].rearrange("p (h d) -> p h d", d=64)
                        P.add("dve", lambda e, n=n: e.tensor_tensor(Hs[:], Hs[:], pcw[:, :, n:n + 1].to_broadcast([DK, H, DV]), ALU.mult),
                              [K("H"), K("pcw")], [K("H")])
                        P.add("dve", lambda e, h3=h3: e.tensor_tensor(Hs[:], Hs[:], h3, ALU.add), [K("H")] + kH, [K("H")])
                    else:
                        P.add("dve", lambda e, n=n: e.tensor_tensor(Hs[:], Hs[:], pcw[:, :, n:n + 1].to_broadcast([DK, H, DV]), ALU.mult),
                              [K("H"), K("pcw")], [K("H")])
                        for half, pp in enumerate((psC, psD)):
                            hs = slice(half * 4, half * 4 + 4)
                            h3 = pp[0:DK, :].rearrange("p (h d) -> p h d", d=128)
                            P.add("dve", lambda e, h3=h3, hs=hs: e.tensor_tensor(Hs[:, hs, :], Hs[:, hs, :], h3, ALU.add),
                                  [K("H"), kH[half]], [K("H")])
                    if STAGE < 7: continue
                    for h in range(H):
                        P.tr(psT[0:DV, h * 64:(h + 1) * 64], on[:, h, :], ident[0:64, 0:64], [K("on"), K("ident")], [K("psT")])
                    t3 = psT[0:DV, :].rearrange("p (h t) -> p h t", t=64)
                    ywc = yw[:, :, cs]
                    if delta:
                        P.add("dve", lambda e, t3=t3, ywc=ywc: e.tensor_tensor(ywc, t3, gng[:].unsqueeze(2).to_broadcast([DV, H, 64]), ALU.mult),
                              [K("psT"), K("gn")], [K("yw")])
                        P.add("pool", lambda e, ywc=ywc: e.tensor_tensor(ywc, ywc, gnb[:].unsqueeze(2).to_broadcast([DV, H, 64]), ALU.add),
                              [K("yw"), K("gn")], [K("yw")])
                        P.add("pool", lambda e, ywc=ywc, cs=cs: e.tensor_tensor(ywc, ywc, A["bon"][:, :, cs], ALU.add), [K("yw"), K("Abon")], [K("yw")])
                        P.add("pool", lambda e, ywc=ywc, cs=cs: e.tensor_tensor(ywc, ywc, A["g"][:, :, cs], ALU.mult), [K("yw"), K("Ag")], [K("yw")])
                    else:
                        P.add("dve", lambda e, t3=t3, ywc=ywc: e.tensor_scalar(ywc, t3, normg[:, 0:1], None, ALU.mult), [K("psT"), K("gn")], [K("yw")])
                        P.add("pool", lambda e, ywc=ywc, cs=cs: e.tensor_tensor(ywc, ywc, A["g"][:, :, cs], ALU.mult), [K("yw"), K("Ag")], [K("yw")])
                P.dma("pool", yT_d[yrow0:yrow0 + HV, n0:n0 + TW].rearrange("(h p) n -> p h n", p=DV), yw[:], [K("yw")], [], K("styw"))
    P.barrier()


def hgrn_prep(P, nc, NB, T, zT_d, prm, S):
    TW = 512
    NW = T // TW
    NCW = TW // CH
    with ExitStack() as es:
        lbc = SB(es, nc, "hp_lbc", [128, 4, 8])
        nl = SB(es, nc, "hp_nl", [128, 8])
        ident = SB(es, nc, "hp_id", [128, 128]); rmask = SB(es, nc, "hp_rmask", [128, TW])
        q = SB(es, nc, "hp_q", [128, TW]); f = SB(es, nc, "hp_f", [128, TW]); iv = SB(es, nc, "hp_i", [128, TW]); g = SB(es, nc, "hp_g", [128, TW])
        k = SB(es, nc, "hp_k", [128, TW]); c = SB(es, nc, "hp_c", [128, TW]); ep = SB(es, nc, "hp_ep", [128, TW]); en = SB(es, nc, "hp_en", [128, TW])
        o_rt = SB(es, nc, "hp_ort", [128, TW]); o_kt = SB(es, nc, "hp_okt", [128, TW]); o_ktl = SB(es, nc, "hp_oktl", [128, TW])
        o_pc = SB(es, nc, "hp_opc", [128, NCW]); vtk = SB(es, nc, "hp_vtk", [128, 4, 128])
        ps6 = PS(es, nc, "hp_ps6", [128, 512])
        P.dma("pool", lbc[:, 0, :], prm["lb2"][0].rearrange("(h p) -> p h", p=128), [], ["hp_lbc"], "hp_c0", allow_slow_non_contiguous=True)
        P.dma("pool", lbc[:, 1, :], prm["lb2"][1].rearrange("(h p) -> p h", p=128), [], ["hp_lbc"], "hp_c0", allow_slow_non_contiguous=True)
        P.dma("pool", ident[:], prm["ident"], [], ["hp_id"], "hp_c0")
        P.dma("pool", rmask[:], prm["rmask"].partition_broadcast(128), [], ["hp_rmask"], "hp_c0")
        P.add("dve", lambda e: e.tensor_tensor(lbc[:, 2, :], lbc[:, 1, :], lbc[:, 0, :], ALU.subtract), ["hp_lbc"], ["hp_lbc"])
        P.add("act", lambda e: e.activation(lbc[:, 2, :], lbc[:, 2, :], AF.Sigmoid), ["hp_lbc"], ["hp_lbc"])
        P.add("dve", lambda e: e.tensor_scalar(lbc[:, 3, :], lbc[:, 2, :], -1.0, 1.0, ALU.mult, ALU.add), ["hp_lbc"], ["hp_lbc"])
        P.add("dve", lambda e: e.tensor_scalar(nl[:], lbc[:, 3, :], -1.0, None, ALU.mult), ["hp_lbc"], ["hp_nl"])
        for b in range(NB):
            for w in range(NW):
                n0 = b * T + w * TW
                for h in range(8):
                    fs = slice(h * 128, (h + 1) * 128)
                    for j, (dst, key) in enumerate(((q, "hp_q"), (f, "hp_f"), (iv, "hp_i"), (g, "hp_g"))):
                        P.dma("sp" if j % 2 == 0 else "act", dst[:], zT_d[j * 1024 + h * 128:j * 1024 + (h + 1) * 128, n0:n0 + TW], [], [key], key)
                    P.add("act", lambda e: e.activation(f[:], f[:], AF.Sigmoid), ["hp_f"], ["hp_f"])
                    P.add("dve", lambda e, h=h: e.tensor_scalar(k[:], f[:], nl[:, h:h + 1], lbc[:, 3, h:h + 1], ALU.mult, ALU.add),
                          ["hp_f", "hp_nl", "hp_lbc"], ["hp_k"])
                    P.add("dve", lambda e, h=h: e.tensor_scalar(f[:], f[:], lbc[:, 3, h:h + 1], lbc[:, 2, h:h + 1], ALU.mult, ALU.add),
                          ["hp_f", "hp_lbc", "hp_k"], ["hp_f"])
                    P.add("act", lambda e: e.activation(f[:], f[:], AF.Ln), ["hp_f"], ["hp_f"])
                    P.add("act", lambda e: e.activation(q[:], q[:], AF.Silu), ["hp_q"], ["hp_q"])
                    P.add("act", lambda e: e.activation(g[:], g[:], AF.Silu), ["hp_g"], ["hp_g"])
                    P.dma("pool", S["g"][fs, n0:n0 + TW], g[:], ["hp_g"], [], "hp_stg")
                    P.add("dve", lambda e: e.tensor_tensor_scan(c[:], rmask[:], f[:], 0.0, ALU.mult, ALU.add), ["hp_rmask", "hp_f"], ["hp_c"])
                    P.add("act", lambda e: e.activation(ep[:], c[:], AF.Exp), ["hp_c"], ["hp_ep"])
                    P.add("act", lambda e: e.activation(en[:], c[:], AF.Exp, scale=-1.0), ["hp_c"], ["hp_en"])
                    P.add("dve", lambda e: e.tensor_tensor(o_rt[:], q[:], ep[:], ALU.mult), ["hp_q", "hp_ep"], ["hp_ort"])
                    P.add("pool", lambda e: e.tensor_tensor(o_kt[:], k[:], en[:], ALU.mult), ["hp_k", "hp_en"], ["hp_okt"])
                    P.add("dve", lambda e: e.tensor_copy(o_pc[:], ep[:].rearrange("p (n c) -> p n c", c=CH)[:, :, CH - 1]), ["hp_ep"], ["hp_opc"])
                    pcb = o_pc[:].unsqueeze(2).to_broadcast([128, NCW, CH])
                    P.add("dve", lambda e, pcb=pcb: e.tensor_tensor(o_ktl[:].rearrange("p (n c) -> p n c", c=CH),
                                                                     o_kt[:].rearrange("p (n c) -> p n c", c=CH), pcb, ALU.mult),
                          ["hp_okt", "hp_opc"], ["hp_oktl"])
                    for sub in range(TW // 128):
                        P.tr(ps6[:, sub * 128:(sub + 1) * 128], iv[:, sub * 128:(sub + 1) * 128], ident[:], ["hp_i", "hp_id"], ["hp_ps6"])
                    P.add("act", lambda e: e.activation(vtk[:], ps6[:].rearrange("p (s f) -> p s f", f=128), AF.Copy), ["hp_ps6"], ["hp_vtk"])
                    P.dma("pool", S["vtok"][n0:n0 + TW, fs].rearrange("(s p) f -> p s f", p=128), vtk[:], ["hp_vtk"], [], "hp_stv")
                    for key, tl, dd in (("hp_ort", o_rt, "rt"), ("hp_okt", o_kt, "kt"), ("hp_oktl", o_ktl, "ktl")):
                        P.dma("sp", S[dd][fs, n0:n0 + TW], tl[:], [key], [], "st" + key)
                    P.dma("sp", S["pc"][fs, n0 // CH:n0 // CH + NCW], o_pc[:], ["hp_opc"], [], "sthp_opc")
    P.barrier()


R0 = 1792
NEG = -30000.0
PI = math.pi


def nsa_prep(P, nc, NB, T, zT_d, pos_d, cst, S):
    TW = 512
    NW = T // TW
    with ExitStack() as es:
        invf = SB(es, nc, "np_invf", [128, 1]); rot = SB(es, nc, "np_rot", [128, 128]); ident = SB(es, nc, "np_id", [128, 128])
        posi = SB(es, nc, "np_posi", [128, TW], I32); ang = SB(es, nc, "np_ang", [128, TW])
        tt = SB(es, nc, "np_tt", [128, TW]); ki = SB(es, nc, "np_ki", [128, TW], I32); kf = SB(es, nc, "np_kf", [128, TW])
        sinF = SB(es, nc, "np_sin", [128, TW]); cosF = SB(es, nc, "np_cos", [128, TW])
        x = SB(es, nc, "np_x", [128, TW]); xr = SB(es, nc, "np_xr", [128, TW]); xq = SB(es, nc, "np_xq", [128, TW]); t2 = SB(es, nc, "np_t2", [128, TW])
        vtk = SB(es, nc, "np_vtk", [128, 4, 128]); gl = SB(es, nc, "np_gl", [24, TW]); gtk = SB(es, nc, "np_gtk", [128, 4, 24])
        ps1 = PS(es, nc, "np_ps1", [128, 512]); ps2 = PS(es, nc, "np_ps2", [128, 512]); ps3 = PS(es, nc, "np_ps3", [128, 512])
        P.dma("pool", invf[:], cst["invf"], [], ["np_c"], "np_c0")
        P.dma("pool", rot[:], cst["rot"], [], ["np_c"], "np_c0")
        P.dma("pool", ident[:], cst["ident"], [], ["np_c"], "np_c0")

        def reduce_sin(dst, shift, kd):
            P.add("dve", lambda e: e.tensor_scalar(tt[:], ang[:], shift, 1.0 / (2 * PI), ALU.add, ALU.mult), ["np_ang"], ["np_tt"])
            P.add("dve", lambda e: e.tensor_copy(ki[:], tt[:]), ["np_tt"], ["np_ki"])
            P.add("dve", lambda e: e.tensor_copy(kf[:], ki[:]), ["np_ki"], ["np_kf"])
            P.add("dve", lambda e: e.tensor_scalar(tt[:], ang[:], shift, None, ALU.add), ["np_ang", "np_ki"], ["np_tt"])
            P.add("dve", lambda e: e.scalar_tensor_tensor(tt[:], kf[:], -2 * PI, tt[:], ALU.mult, ALU.add), ["np_kf", "np_tt"], ["np_tt"])
            P.add("dve", lambda e: e.tensor_scalar(kf[:], tt[:], PI, -2 * PI, ALU.is_gt, ALU.mult), ["np_tt"], ["np_kf"])
            P.add("dve", lambda e: e.tensor_tensor(tt[:], tt[:], kf[:], ALU.add), ["np_tt", "np_kf"], ["np_tt"])
            P.add("dve", lambda e: e.tensor_scalar(kf[:], tt[:], -PI, 2 * PI, ALU.is_lt, ALU.mult), ["np_tt"], ["np_kf"])
            P.add("dve", lambda e: e.tensor_tensor(tt[:], tt[:], kf[:], ALU.add), ["np_tt", "np_kf"], ["np_tt"])
            P.add("dve", lambda e: e.tensor_scalar(tt[:], tt[:], PI, -PI, ALU.min, ALU.max), ["np_tt"], ["np_tt"])
            P.add("act", lambda e: e.activation(dst[:], tt[:], AF.Sin), ["np_tt"], [kd])

        for b in range(NB):
            for w in range(NW):
                t0 = w * TW
                n0 = b * T + t0
                P.dma("sp", posi[:], pos_d[b, t0:t0 + TW].partition_broadcast(128), [], ["np_posi"], "np_posi")
                P.add("dve", lambda e: e.tensor_copy(ang[:], posi[:]), ["np_posi"], ["np_ang"])
                P.add("dve", lambda e: e.tensor_scalar(ang[:], ang[:], invf[:, 0:1], None, ALU.mult), ["np_ang", "np_c"], ["np_ang"])
                reduce_sin(sinF, 0.0, "np_sin")
                reduce_sin(cosF, PI / 2, "np_cos")
                for ti, (row, dname, drow, sc, raw) in enumerate(
                        [(128 * i, "qrot", 128 * i, 0.125, True) for i in range(4)] + [(768, "ksr", 0, 1.0, False), (1024, "kwr", 0, 1.0, False)]):
                    P.dma("sp", x[:], zT_d[R0 + row:R0 + row + 128, n0:n0 + TW], [], ["np_x"], "np_x")
                    P.mm(ps1[:, 0:TW], rot[:], x[:], True, True, ["np_c", "np_x"], ["np_ps1"])
                    P.add("dve", lambda e: e.tensor_tensor(t2[:], ps1[:, 0:TW], sinF[:], ALU.mult), ["np_ps1", "np_sin"], ["np_t2"])
                    P.add("pool", lambda e: e.tensor_tensor(xr[:], x[:], cosF[:], ALU.mult), ["np_x", "np_cos"], ["np_xr"])
                    P.add("dve", lambda e, sc=sc: e.scalar_tensor_tensor(xr[:], xr[:], 1.0, t2[:], ALU.mult, ALU.add), ["np_xr", "np_t2"], ["np_xr"])
                    if sc != 1.0:
                        P.add("pool", lambda e, sc=sc: e.tensor_scalar(xr[:], xr[:], sc, None, ALU.mult), ["np_xr"], ["np_xr"])
                    P.dma("pool", S[dname][drow:drow + 128, n0:n0 + TW], xr[:], ["np_xr"], [], "np_stxr")
                    if raw:
                        P.add("act", lambda e, sc=sc: e.activation(xq[:], x[:], AF.Copy, scale=sc), ["np_x"], ["np_xq"])
                        P.dma("pool", S["qraw"][drow:drow + 128, n0:n0 + TW], xq[:], ["np_xq"], [], "np_stxq")
                for row, dname in ((896, "vs_tok"), (1152, "vw_tok")):
                    P.dma("sp", x[:], zT_d[R0 + row:R0 + row + 128, n0:n0 + TW], [], ["np_x"], "np_x")
                    for sub in range(4):
                        P.tr(ps2[:, sub * 128:(sub + 1) * 128], x[:, sub * 128:(sub + 1) * 128], ident[:], ["np_x", "np_c"], ["np_ps2"])
                    P.add("act", lambda e: e.activation(vtk[:], ps2[:].rearrange("p (s f) -> p s f", f=128), AF.Copy), ["np_ps2"], ["np_vtk"])
                    P.dma("pool", S[dname][n0:n0 + TW, :].rearrange("(s p) f -> p s f", p=128), vtk[:], ["np_vtk"], [], "np_stv")
                P.dma("sp", gl[:], zT_d[R0 + 1280:R0 + 1304, n0:n0 + TW], [], ["np_gl"], "np_gl")
                P.add("act", lambda e: e.activation(gl[:], gl[:], AF.Sigmoid), ["np_gl"], ["np_gl"])
                for sub in range(4):
                    P.tr(ps3[:, sub * 24:(sub + 1) * 24], gl[:, sub * 128:(sub + 1) * 128], ident[0:24, 0:24], ["np_gl", "np_c"], ["np_ps3"])
                P.add("act", lambda e: e.activation(gtk[:], ps3[:, 0:96].rearrange("p (s f) -> p s f", f=24), AF.Copy), ["np_ps3"], ["np_gtk"])
                P.dma("pool", S["gat_tok"][n0:n0 + TW, :].rearrange("(s p) f -> p s f", p=128), gtk[:], ["np_gtk"], [], "np_stg")
    P.barrier()


def nsa_cmp(P, nc, NB, T, zT_d, w1_d, w2_d, cpos_d, S):
    ncmp = (T - 32) // 16 + 1
    nch = (ncmp + 127) // 128
    C0 = math.sqrt(2.0 / math.pi)
    with ExitStack() as es:
        w1 = SB(es, nc, "nc_w1", [64, 2, 32, 128]); w2 = SB(es, nc, "nc_w2", [128, 2, 64]); cpos = SB(es, nc, "nc_cpos", [64, 2, 32])
        bias = SB(es, nc, "nc_bias", [128, 2]); kc = SB(es, nc, "nc_kc", [64, T])
        hx = SB(es, nc, "nc_hx", [128, 256]); h2 = SB(es, nc, "nc_h2", [128, 256]); h3 = SB(es, nc, "nc_h3", [128, 256])
        ko = SB(es, nc, "nc_ko", [64, 256]); vo = SB(es, nc, "nc_vo", [128, 2, 64])
        psb = PS(es, nc, "nc_psb", [128, 512]); psh = PS(es, nc, "nc_psh", [128, 512]); pso = PS(es, nc, "nc_pso", [128, 512])
        for z in range(2):
            P.dma("sp", w1[:, z, :, :], w1_d[z].rearrange("(l d) o -> d l o", d=64), [], ["nc_w1"], "nc_c0")
            P.dma("sp", w2[:, z, :], w2_d[z], [], ["nc_w2"], "nc_c0")
            P.dma("sp", cpos[:, z, :], cpos_d[z].rearrange("l d -> d l"), [], ["nc_cpos"], "nc_c0", allow_slow_non_contiguous=True)
        for z in range(2):
            for l in range(32):
                P.mm(psb[:, z:z + 1], w1[:, z, l, :], cpos[:, z, l:l + 1], l == 0, l == 31, ["nc_w1", "nc_cpos"], ["nc_psb"])
        P.add("dve", lambda e: e.tensor_copy(bias[:], psb[:, 0:2]), ["nc_psb"], ["nc_bias"])
        for b in range(NB):
            for z in range(2):
                for hk in range(2):
                    row = R0 + 512 + z * 128 + hk * 64
                    P.dma("sp", kc[:], zT_d[row:row + 64, b * T:(b + 1) * T], [], ["nc_kc"], "nc_kc")
                    for l in range(32):
                        P.mm(psh[:, 0:ncmp], w1[:, z, l, :], kc[:, l:l + 16 * (ncmp - 1) + 1:16], l == 0, l == 31, ["nc_w1", "nc_kc"], ["nc_psh"])
                    P.add("act", lambda e, z=z: e.activation(hx[:, 0:ncmp], psh[:, 0:ncmp], AF.Identity, bias=bias[:, z:z + 1], scale=1.0),
                          ["nc_psh", "nc_bias"], ["nc_hx"])
                    P.add("act", lambda e: e.activation(h2[:, 0:ncmp], hx[:, 0:ncmp], AF.Square), ["nc_hx"], ["nc_h2"])
                    P.add("dve", lambda e: e.tensor_scalar(h2[:, 0:ncmp], h2[:, 0:ncmp], 0.044715, 1.0, ALU.mult, ALU.add), ["nc_h2"], ["nc_h2"])
                    P.add("dve", lambda e: e.tensor_tensor(h2[:, 0:ncmp], h2[:, 0:ncmp], hx[:, 0:ncmp], ALU.mult), ["nc_h2", "nc_hx"], ["nc_h2"])
                    P.add("act", lambda e: e.activation(h3[:, 0:ncmp], h2[:, 0:ncmp], AF.Tanh, scale=C0), ["nc_h2"], ["nc_h3"])
                    P.add("dve", lambda e: e.tensor_scalar(h3[:, 0:ncmp], h3[:, 0:ncmp], 0.5, 0.5, ALU.mult, ALU.add), ["nc_h3"], ["nc_h3"])
                    P.add("dve", lambda e: e.tensor_tensor(h3[:, 0:ncmp], h3[:, 0:ncmp], hx[:, 0:ncmp], ALU.mult), ["nc_h3", "nc_hx"], ["nc_h3"])
                    r = (b * 2 + hk)
                    if z == 0:
                        P.mm(pso[0:64, 0:ncmp], w2[:, 0, :], h3[:, 0:ncmp], True, True, ["nc_w2", "nc_h3"], ["nc_pso"])
                        P.add("act", lambda e: e.activation(ko[:, 0:ncmp], pso[0:64, 0:ncmp], AF.Copy), ["nc_pso"], ["nc_ko"])
                        P.dma("pool", S["kcmpT"][r * 64:(r + 1) * 64, 0:ncmp], ko[:, 0:ncmp], ["nc_ko"], [], "nc_stko")
                    else:
                        for c in range(nch):
                            nn = min(128, ncmp - c * 128)
                            P.mm(pso[0:nn, c * 64:(c + 1) * 64], h3[:, c * 128:c * 128 + nn], w2[:, 1, :], True, True, ["nc_w2", "nc_h3"], ["nc_pso"])
                            P.add("act", lambda e, c=c, nn=nn: e.activation(vo[0:nn, c, :], pso[0:nn, c * 64:(c + 1) * 64], AF.Copy), ["nc_pso"], ["nc_vo"])
                            P.dma("pool", S["vcmp"][r, c * 128:c * 128 + nn, :], vo[0:nn, c, :], ["nc_vo"], [], "nc_stvo")
    P.barrier()


def nsa_attn_a(P, nc, NB, T, cst, S):
    ncmp = (T - 32) // 16 + 1
    nch = (ncmp + 127) // 128
    NT = T // 128
    NS = T // 64
    with ExitStack() as es:
        tab = SB(es, nc, "na_tab", [128, 2048 + T]); ident = SB(es, nc, "na_id", [128, 128])
        keepw = SB(es, nc, "na_keepw", [128, 2 * NS]); addw = SB(es, nc, "na_addw", [128, 2 * NS])
        kcm = SB(es, nc, "na_kcm", [64, nch * 128]); vx = SB(es, nc, "na_vx", [128, nch, 129])
        qr = SB(es, nc, "na_qr", [64, 4, T])
        ee = [SB(es, nc, "na_e%d" % i, [128, 512]) for i in range(2)]
        o1 = SB(es, nc, "na_o1", [128, 4, 65]); o2 = SB(es, nc, "na_o2", [128, 4, NS]); rden = SB(es, nc, "na_rden", [128, 4])
        oc = [SB(es, nc, "na_oc%d" % i, [128, 4, 64]) for i in range(2)]
        imp = SB(es, nc, "na_imp", [128, NS]); imp2 = SB(es, nc, "na_imp2", [128, NS]); m8 = SB(es, nc, "na_m8", [128, 16])
        selb = SB(es, nc, "na_selb", [128, NS]); sbT = SB(es, nc, "na_sbT", [64, T])
        psS = [PS(es, nc, "na_psS%d" % i, [128, 512]) for i in range(2)]
        psO1 = PS(es, nc, "na_psO1", [128, 512]); psO2 = PS(es, nc, "na_psO2", [128, 512]); psT = PS(es, nc, "na_psT", [128, 512])
        P.dma("sp", tab[:], cst["tab"], [], ["na_c"], "na_c0")
        P.dma("sp", ident[:], cst["ident"], [], ["na_c"], "na_c0")
        P.dma("sp", keepw[:], cst["keepw"], [], ["na_c"], "na_c0")
        P.dma("sp", addw[:], cst["addw"], [], ["na_c"], "na_c0")
        for c in range(nch):
            nn = min(128, ncmp - c * 128)
            P.dma("sp", vx[0:nn, c, 64:65 + NS], cst["ovx"][c * 128:c * 128 + nn, :], [], ["na_vxc"], "na_c0")
        for b in range(NB):
            for hk in range(2):
                r = b * 2 + hk
                P.dma("sp", kcm[:, 0:ncmp], S["kcmpT"][r * 64:(r + 1) * 64, 0:ncmp], [], ["na_kcm"], "na_kcm")
                for c in range(nch):
                    nn = min(128, ncmp - c * 128)
                    P.dma("sp", vx[0:nn, c, 0:64], S["vcmp"][r, c * 128:c * 128 + nn, :], [], ["na_vx"], "na_vx")
                P.dma("act", qr[:], S["qraw"][hk * 256:(hk + 1) * 256, b * T:(b + 1) * T].rearrange("(g d) n -> d g n", d=64), [], ["na_qr"], "na_qr")
                for j in range(NT):
                    chunks = [c for c in range(nch) if 16 * 128 * c + 31 <= 128 * j + 127]
                    s_ = j % 2
                    koc = "na_oc%d" % s_
                    if not chunks:
                        P.add("dve", lambda e, s_=s_: e.memset(oc[s_][:], 0.0), [], [koc])
                        P.add("dve", lambda e: e.memset(imp[:], 0.0), [], ["na_imp"])
                    else:
                        for ci, c in enumerate(chunks):
                            nn = min(128, ncmp - c * 128)
                            off = 2048 + 128 * j - 2048 * c
                            ps = psS[c % 2]
                            P.mm(ps[0:nn, :], kcm[:, c * 128:c * 128 + nn], qr[:, :, j * 128:(j + 1) * 128], True, False,
                                 ["na_kcm", "na_qr"], ["na_psS%d" % (c % 2)])
                            P.mm(ps[0:nn, :], ident[0:nn, 0:nn], tab[0:nn, off:off + 128].unsqueeze(1).to_broadcast([nn, 4, 128]), False, True,
                                 ["na_c"], ["na_psS%d" % (c % 2)])
                            P.add("act", lambda e, c=c, nn=nn, ps=ps: e.activation(ee[c % 2][0:nn, :], ps[0:nn, :], AF.Exp),
                                  ["na_psS%d" % (c % 2)], ["na_e%d" % (c % 2)])
                        for g in range(4):
                            for ci, c in enumerate(chunks):
                                nn = min(128, ncmp - c * 128)
                                P.mm(psO1[:, g * 65:(g + 1) * 65], ee[c % 2][0:nn, g * 128:(g + 1) * 128], vx[0:nn, c, 0:65],
                                     ci == 0, ci == len(chunks) - 1, ["na_e%d" % (c % 2), "na_vx", "na_vxc"], ["na_psO1"])
                        for g in range(4):
                            for ci, c in enumerate(chunks):
                                nn = min(128, ncmp - c * 128)
                                P.mm(psO2[:, g * NS:(g + 1) * NS], ee[c % 2][0:nn, g * 128:(g + 1) * 128], vx[0:nn, c, 65:65 + NS],
                                     ci == 0, ci == len(chunks) - 1, ["na_e%d" % (c % 2), "na_vx", "na_vxc"], ["na_psO2"])
                        P.add("act", lambda e: e.activation(o1[:], psO1[:, 0:260].rearrange("p (g d) -> p g d", d=65), AF.Copy), ["na_psO1"], ["na_o1"])
                        P.add("dve", lambda e: e.tensor_copy(o2[:], psO2[:, 0:4 * NS].rearrange("p (g d) -> p g d", d=NS)), ["na_psO2"], ["na_o2"])
                        P.add("dve", lambda e: e.tensor_scalar(rden[:], o1[:, :, 64], 1e-30, None, ALU.max), ["na_o1"], ["na_rden"])
                        P.add("dve", lambda e: e.reciprocal(rden[:], rden[:]), ["na_rden"], ["na_rden"])
                        P.add("dve", lambda e, s_=s_: e.tensor_tensor(oc[s_][:], o1[:, :, 0:64], rden[:].unsqueeze(2).to_broadcast([128, 4, 64]), ALU.mult),
                              ["na_o1", "na_rden"], [koc])
                        P.add("dve", lambda e: e.tensor_tensor(o2[:], o2[:], rden[:].unsqueeze(2).to_broadcast([128, 4, NS]), ALU.mult),
                              ["na_o2", "na_rden"], ["na_o2"])
                        P.add("dve", lambda e: e.reduce_sum(imp[:], o2[:].rearrange("p g s -> p s g"), AX.X), ["na_o2"], ["na_imp"])
                    P.dma("pool", S["oc_tok"][b * T + j * 128:b * T + (j + 1) * 128, hk * 256:(hk + 1) * 256],
                          oc[s_][:].rearrange("p g d -> p (g d)"), [koc], [], "st" + koc)
                    o2_ = NS - 2 * j
                    P.add("dve", lambda e, o2_=o2_: e.tensor_tensor(imp[:], imp[:], keepw[:, o2_:o2_ + NS], ALU.mult), ["na_imp", "na_c"], ["na_imp"])
                    P.add("dve", lambda e, o2_=o2_: e.tensor_tensor(imp[:], imp[:], addw[:, o2_:o2_ + NS], ALU.add), ["na_imp", "na_c"], ["na_imp"])
                    P.add("dve", lambda e: e.memset(imp[:, 0:1], 1e30), ["na_imp"], ["na_imp"])
                    P.add("dve", lambda e: e.max(m8[:, 0:8], imp[:]), ["na_imp"], ["na_m8"])
                    P.add("dve", lambda e: e.match_replace(imp2[:], m8[:, 0:8], imp[:], -3e38), ["na_imp", "na_m8"], ["na_imp2"])
                    P.add("dve", lambda e: e.max(m8[:, 8:16], imp2[:]), ["na_imp2"], ["na_m8"])
                    P.add("dve", lambda e: e.tensor_scalar(selb[:], imp[:], m8[:, 15:16], None, ALU.is_ge), ["na_imp", "na_m8"], ["na_selb"])
                    P.add("dve", lambda e: e.tensor_scalar(selb[:], selb[:], -1.0, -NEG, ALU.add, ALU.mult), ["na_selb"], ["na_selb"])
                    P.tr(psT[0:NS, 0:128], selb[:], ident[:], ["na_selb", "na_c"], ["na_psT"])
                    P.add("act", lambda e, j=j: e.activation(sbT[0:NS, j * 128:(j + 1) * 128], psT[0:NS, 0:128], AF.Copy), ["na_psT"], ["na_sbT"])
                P.dma("sp", S["selbT"][r * 64:r * 64 + NS, :], sbT[0:NS, :], ["na_sbT"], [], "na_stsb")
    P.barrier()


def nsa_attn_b(P, nc, NB, T, cst, S, yT_d, yrow0):
    NT = T // 128
    NS = T // 64
    WT = 4
    with ExitStack() as es:
        stg = SB(es, nc, "nb_stg", [64, 4, 512]); stg2 = SB(es, nc, "nb_stg2", [128, 512])
        qb = SB(es, nc, "nb_qb", [64, 4, T], BF16); ks = SB(es, nc, "nb_ks", [64, T], BF16); kw = SB(es, nc, "nb_kw", [64, T], BF16)
        sb = SB(es, nc, "nb_sb", [64, T], BF16); eall = SB(es, nc, "nb_eall", [64, T], BF16)
        caus = SB(es, nc, "nb_caus", [128, 128], BF16); anti = SB(es, nc, "nb_anti", [128, 128], BF16); idb = SB(es, nc, "nb_idb", [128, 128], BF16)
        ident = SB(es, nc, "nb_id", [128, 128])
        vsx = SB(es, nc, "nb_vsx", [128, NT, 65], BF16); vwx = SB(es, nc, "nb_vwx", [128, NT, 65], BF16)
        vst = SB(es, nc, "nb_vst", [128, NT, 64])
        gat = SB(es, nc, "nb_gat", [128, NT, 24])
        eall_s = SB(es, nc, "nb_eas", [128, NT, 512], BF16); eall_w = SB(es, nc, "nb_eaw", [128, WT + 1, 512], BF16)
        oct_ = SB(es, nc, "nb_oct", [128, 4, 64]); os_ = SB(es, nc, "nb_os", [128, 4, 65]); ow_ = SB(es, nc, "nb_ow", [128, 4, 65])
        wsw = SB(es, nc, "nb_wsw", [128, 2, 4]); acc = SB(es, nc, "nb_acc", [128, 4, 64]); tmp = SB(es, nc, "nb_tmp", [128, 4, 64])
        yw = SB(es, nc, "nb_yw", [128, 2, T])
        psS = [PS(es, nc, "nb_psS%d" % i, [128, 512]) for i in range(4)]
        psOs = PS(es, nc, "nb_psOs", [128, 512]); psOw = PS(es, nc, "nb_psOw", [128, 512]); psT = PS(es, nc, "nb_psT", [128, 512])

        def load_cast(dst, src, shape_p, keyd, wide):
            P.dma("sp", stg2[0:shape_p, 0:wide], src, [], ["nb_stg2"], "nb_stg2")
            P.add("act", lambda e: e.activation(dst, stg2[0:shape_p, 0:wide], AF.Copy), ["nb_stg2"], [keyd])
        for c0 in range(0, T, 512):
            load_cast(eall[:, c0:c0 + 512], cst["eall"][:, c0:c0 + 512], 64, "nb_eall", 512)
        load_cast(caus[:], cst["caus"], 128, "nb_cm", 128)
        load_cast(anti[:], cst["anti"], 128, "nb_cm", 128)
        load_cast(idb[:], cst["ident"], 128, "nb_cm", 128)
        P.dma("sp", ident[:], cst["ident"], [], ["nb_id"], "nb_c0")
        for b in range(NB):
            P.dma("sp", gat[:], S["gat_tok"][b * T:(b + 1) * T, :].rearrange("(j p) f -> p j f", p=128), [], ["nb_gat"], "nb_gat")
            for hk in range(2):
                r = b * 2 + hk
                for c0 in range(0, T, 512):
                    P.dma("sp", stg[:], S["qrot"][hk * 256:(hk + 1) * 256, b * T + c0:b * T + c0 + 512].rearrange("(g d) n -> d g n", d=64),
                          [], ["nb_stg"], "nb_stg")
                    P.add("act", lambda e, c0=c0: e.activation(qb[:, :, c0:c0 + 512], stg[:], AF.Copy), ["nb_stg"], ["nb_qb"])
                    load_cast(ks[:, c0:c0 + 512], S["ksr"][hk * 64:(hk + 1) * 64, b * T + c0:b * T + c0 + 512], 64, "nb_ks", 512)
                    load_cast(kw[:, c0:c0 + 512], S["kwr"][hk * 64:(hk + 1) * 64, b * T + c0:b * T + c0 + 512], 64, "nb_kw", 512)
                    load_cast(sb[0:NS, c0:c0 + 512], S["selbT"][r * 64:r * 64 + NS, c0:c0 + 512], NS, "nb_sb", 512)
                for vx_, nm, kk_ in ((vsx, "vs_tok", "nb_vsx"), (vwx, "vw_tok", "nb_vwx")):
                    P.dma("sp", vst[:], S[nm][b * T:(b + 1) * T, hk * 64:(hk + 1) * 64].rearrange("(j p) d -> p j d", p=128), [], ["nb_vst"], "nb_vst")
                    P.add("dve", lambda e, vx_=vx_: e.tensor_copy(vx_[:, :, 0:64], vst[:]), ["nb_vst"], [kk_])
                    P.add("dve", lambda e, vx_=vx_: e.memset(vx_[:, :, 64:65], 1.0), [], [kk_])
                k = 0
                for j in range(NT):
                    qs = qb[:, :, j * 128:(j + 1) * 128]
                    for br, (kk_b, vx_, pso, kps) in enumerate(((ks, vsx, psOs, "nb_psOs"), (kw, vwx, psOw, "nb_psOw"))):
                        kts = list(range(0, j + 1)) if br == 0 else list(range(max(0, j - WT), j + 1))
                        ea = eall_s if br == 0 else eall_w
                        kea = "nb_eas" if br == 0 else "nb_eaw"
                        for ki_, kt in enumerate(kts):
                            ps = psS[k % 4]; kp = "nb_psS%d" % (k % 4)
                            k += 1
                            extra = []
                            if br == 0 and NS > 16 and (2 * j + 1) >= 16:
                                extra.append((eall[0:NS, kt * 128:(kt + 1) * 128], sb[0:NS, j * 128:(j + 1) * 128], NS, ["nb_eall", "nb_sb"]))
                            if kt == j:
                                extra.append((idb[:], caus[:], 128, ["nb_cm"]))
                            if br == 1 and kt == j - WT:
                                extra.append((idb[:], anti[:], 128, ["nb_cm"]))
                            P.mm(ps[:], kk_b[:, kt * 128:(kt + 1) * 128], qs, True, not extra, ["nb_ks", "nb_kw", "nb_qb"], [kp])
                            for xi, (l_, r_, pp, rk) in enumerate(extra):
                                P.mm(ps[:], l_, r_.unsqueeze(1).to_broadcast([pp, 4, 128]), False, xi == len(extra) - 1, rk, [kp])
                            P.add("act", lambda e, ea=ea, ki_=ki_, ps=ps: e.activation(ea[:, ki_, :], ps[:], AF.Exp), [kp], [kea])
                        for g in range(4):
                            for ki_, kt in enumerate(kts):
                                P.mm(pso[:, g * 65:(g + 1) * 65], ea[:, ki_, g * 128:(g + 1) * 128], vx_[:, kt, :], ki_ == 0, ki_ == len(kts) - 1,
                                     [kea, "nb_vsx", "nb_vwx"], [kps])
                    P.dma("sp", oct_[:].rearrange("p g d -> p (g d)"), S["oc_tok"][b * T + j * 128:b * T + (j + 1) * 128, hk * 256:(hk + 1) * 256],
                          [], ["nb_oct"], "nb_oct")
                    P.add("act", lambda e: e.activation(os_[:], psOs[:, 0:260].rearrange("p (g d) -> p g d", d=65), AF.Copy), ["nb_psOs"], ["nb_os"])
                    P.add("dve", lambda e: e.tensor_copy(ow_[:], psOw[:, 0:260].rearrange("p (g d) -> p g d", d=65)), ["nb_psOw"], ["nb_ow"])
                    gv = gat[:, j, hk * 12:(hk + 1) * 12].rearrange("p (g t) -> p g t", t=3)
                    P.add("dve", lambda e: e.tensor_scalar(wsw[:, 0, :], os_[:, :, 64], 1e-30, None, ALU.max), ["nb_os"], ["nb_wsw"])
                    P.add("dve", lambda e: e.tensor_scalar(wsw[:, 1, :], ow_[:, :, 64], 1e-30, None, ALU.max), ["nb_ow"], ["nb_wsw"])
                    P.add("dve", lambda e: e.reciprocal(wsw[:], wsw[:]), ["nb_wsw"], ["nb_wsw"])
                    P.add("dve", lambda e, gv=gv: e.tensor_tensor(wsw[:, 0, :], wsw[:, 0, :], gv[:, :, 1], ALU.mult), ["nb_wsw", "nb_gat"], ["nb_wsw"])
                    P.add("dve", lambda e, gv=gv: e.tensor_tensor(wsw[:, 1, :], wsw[:, 1, :], gv[:, :, 2], ALU.mult), ["nb_wsw", "nb_gat"], ["nb_wsw"])
                    P.add("dve", lambda e, gv=gv: e.tensor_tensor(acc[:], oct_[:], gv[:, :, 0:1].to_broadcast([128, 4, 64]), ALU.mult),
                          ["nb_oct", "nb_gat"], ["nb_acc"])
                    P.add("pool", lambda e: e.tensor_tensor(tmp[:], os_[:, :, 0:64], wsw[:, 0, :].unsqueeze(2).to_broadcast([128, 4, 64]), ALU.mult),
                          ["nb_os", "nb_wsw"], ["nb_tmp"])
                    P.add("dve", lambda e: e.tensor_tensor(acc[:], acc[:], tmp[:], ALU.add), ["nb_acc", "nb_tmp"], ["nb_acc"])
                    P.add("pool", lambda e: e.tensor_tensor(tmp[:], ow_[:, :, 0:64], wsw[:, 1, :].unsqueeze(2).to_broadcast([128, 4, 64]), ALU.mult),
                          ["nb_ow", "nb_wsw", "nb_acc"], ["nb_tmp"])
                    P.add("dve", lambda e: e.tensor_tensor(acc[:], acc[:], tmp[:], ALU.add), ["nb_acc", "nb_tmp"], ["nb_acc"])
                    for c in range(2):
                        P.tr(psT[:, c * 128:(c + 1) * 128], acc[:, 2 * c:2 * c + 2, :].rearrange("p g d -> p (g d)"), ident[:], ["nb_acc", "nb_id"], ["nb_psT"])
                    P.add("act", lambda e, j=j: e.activation(yw[:, :, j * 128:(j + 1) * 128], psT[:, 0:256].rearrange("p (c n) -> p c n", c=2), AF.Copy),
                          ["nb_psT"], ["nb_yw"])
                P.dma("pool", yT_d[yrow0 + hk * 256:yrow0 + (hk + 1) * 256, b * T:(b + 1) * T].rearrange("(c p) n -> p c n", p=128), yw[:], ["nb_yw"], [], "nb_styw")
    P.barrier()

def make_consts(T):
    NS = T // 64
    ncmp = (T - 32) // 16 + 1
    NCP = (ncmp + 127) // 128 * 128
    c = {}
    c["ident"] = np.eye(128, dtype=np.float32)
    c["id64"] = np.eye(64, dtype=np.float32)
    b = np.zeros((128, 128), np.float32); b[:64, :64] = 1; b[64:, 64:] = 1
    c["blk1"] = b
    rm = np.ones(512, np.float32); rm[::64] = 0
    c["rmask"] = rm
    ms = np.triu(np.ones((64, 64), np.float32), 1); ml = np.tril(np.ones((64, 64), np.float32), -1); mi = np.triu(np.ones((64, 64), np.float32), 0)
    c["masks"] = np.concatenate([-ms, -ml, ms, mi, -mi], 1)
    c["tri"] = np.concatenate([np.triu(np.ones((128, 128), np.float32), 1), np.ones((128, 128), np.float32)], 1)
    c["iop"] = np.arange(128, dtype=np.float32).reshape(128, 1)
    inv = (1.0 / (10000.0 ** (np.arange(0, 64, 2, dtype=np.float32) / 64))).astype(np.float32)
    c["invf"] = inv[(np.arange(128) % 32)].reshape(128, 1).astype(np.float32)
    R = np.zeros((64, 64), np.float32)
    for m in range(32):
        R[m, m + 32] = -1.0; R[m + 32, m] = 1.0
    rot = np.zeros((128, 128), np.float32); rot[:64, :64] = R.T; rot[64:, 64:] = R.T
    c["rot"] = rot
    n = np.arange(128)[:, None]; xp = np.arange(2048 + T)[None, :]
    c["tab"] = np.where(16 * n + 31 <= xp - 2048, 0.0, -30000.0).astype(np.float32)
    cs = np.arange(NCP)[:, None] * 16; ss = np.arange(NS)[None, :] * 64
    ov = np.clip(np.minimum(cs + 32, ss + 64) - np.maximum(cs, ss), 0, None) / 32.0
    ov[ncmp:] = 0
    c["ovx"] = np.concatenate([np.ones((NCP, 1)), ov], 1).astype(np.float32)
    tl = np.arange(128)[:, None]; x = np.arange(2 * NS)[None, :]
    rel = x - NS; cur = tl // 64
    c["keepw"] = (rel < cur - 1).astype(np.float32)
    c["addw"] = np.where((rel == cur) | (rel == cur - 1), 1e30, np.where(rel > cur, -1e30, 0.0)).astype(np.float32)
    ea = np.zeros((64, T), np.float32); ea[np.arange(T) // 64, np.arange(T)] = 1.0
    c["eall"] = ea
    k = np.arange(128)[:, None]; t = np.arange(128)[None, :]
    c["caus"] = np.where(k <= t, 0.0, -30000.0).astype(np.float32)
    c["anti"] = np.where(k > t, 0.0, -30000.0).astype(np.float32)
    return c


def build(NB, T, debug=False, same_engine_sync=True):
    N = NB * T
    NS = T // 64
    ncmp = (T - 32) // 16 + 1
    NCP = (ncmp + 127) // 128 * 128
    NBLK = (2 * N) // BLK + NEXP
    NT = N // 128
    Cc = make_consts(T)
    nc = bass.Bass("TRN2", target_bir_lowering=False)
    din = lambda name, shape, dt=F32: nc.dram_tensor(name, list(shape), dt, kind="ExternalInput").ap()
    scr = lambda name, shape: nc.dram_tensor(name, list(shape), F32, kind=("ExternalOutput" if debug else "Internal")).ap()
    x = din("x", [N, 1024]); p = din("p", [2, N, 256]); pos = din("pos", [NB, T], I32)
    ev_w_in = din("ev_w_in", [1024, 3096]); ev_w_out = din("ev_w_out", [1024, 1024])
    od_w_in = din("od_w_in", [1024, 4096]); od_w_out = din("od_w_out", [1024, 1024])
    rw = dict(mu=din("rw_mu", [1792]), w0=din("rw_w0", [512]), w2=din("rw_w2", [64, 512]), a0=din("rw_a0", [512]),
              a2=din("rw_a2", [64, 512]), g2=din("rw_g2", [128, 512]), k_k=din("rw_k_k", [512]), k_a=din("rw_k_a", [512]),
              r_k=din("rw_r_k", [512]))
    gng = din("rw_gn_g", [512]); gnb = din("rw_gn_b", [512])
    cpos = din("nsa_cmp_pos", [2, 32, 64]); cw1 = din("nsa_cmp_w1", [2, 2048, 128]); cw2 = din("nsa_cmp_w2", [2, 128, 64])
    hg_lb = din("hg_lb", [2, 1024]); hg_ng = din("hg_norm_g", [128])
    wrg = din("moe_w_rg", [2, 1024, 4]); brg = din("moe_b_rg", [2, 4]); wre = din("moe_w_re", [2, 1024, 32]); bre = din("moe_b_re", [2, 32])
    mw1 = [din("moe_w1_%d" % i, [4096, 4096]) for i in range(2)]; mw3 = [din("moe_w3_%d" % i, [4096, 4096]) for i in range(2)]
    mw2 = [din("moe_w2_%d" % i, [4096, 4096]) for i in range(2)]
    ln_g = din("ln_g", [2, 2, 1024]); ln_b = din("ln_b", [2, 2, 1024])
    ple_w = din("ple_w", [2, 256, 1024]); ple_gw = din("ple_gate_w", [2, 1024, 1024])
    cst = {k: din("c_" + k, Cc[k].shape) for k in Cc}
    thr = din("c_thr", [NBLK])
    out = nc.dram_tensor("out", [N, 1024], F32, kind="ExternalOutput").ap()
    zT = scr("zT", [4096, N]); yT = scr("yT0", [1024, N]); yT1 = scr("yT1", [1024, N]); h1 = scr("h1", [N, 1024]); x1 = scr("x1", [N, 1024])
    xbuf = nc.dram_tensor("xbuf", [NBLK * BLK, 1024], F32).ap(); ybuf = nc.dram_tensor("ybuf", [NBLK * BLK, 1024], F32).ap()
    SR = {k: scr("SR_" + k, [512, N]) for k in ("rt", "kt", "kp", "bt", "ktl", "btl", "g", "bon")}
    SR["pc"] = scr("SR_pc", [512, N // 64]); SR["vtok"] = scr("SR_vtok", [N, 512])
    SH = {k: scr("SH_" + k, [1024, N]) for k in ("rt", "kt", "ktl", "g")}
    SH["pc"] = scr("SH_pc", [1024, N // 64]); SH["vtok"] = scr("SH_vtok", [N, 1024])
    SN = dict(qraw=scr("SN_qraw", [512, N]), qrot=scr("SN_qrot", [512, N]), ksr=scr("SN_ksr", [128, N]), kwr=scr("SN_kwr", [128, N]),
              vs_tok=scr("SN_vs", [N, 128]), vw_tok=scr("SN_vw", [N, 128]), gat_tok=scr("SN_gat", [N, 24]),
              kcmpT=scr("SN_kcmpT", [NB * 2 * 64, NCP]), vcmp=scr("SN_vcmp", [NB * 2, NCP, 64]),
              oc_tok=scr("SN_oc", [N, 512]), selbT=scr("SN_selbT", [NB * 2 * 64, T]))
    R = {}
    R["OH"] = nc.alloc_sbuf_tensor("r_OH", [128, NT, 2, 32], F32)
    R["rank"] = nc.alloc_sbuf_tensor("r_rank", [128, NT, 2], F32)
    R["gates"] = nc.alloc_sbuf_tensor("r_gates", [128, NT, 2], F32)
    R["base"] = nc.alloc_sbuf_tensor("r_base", [128, 32], F32)
    R["dest_i"] = nc.alloc_sbuf_tensor("r_dest_i", [128, NT * 2], I32)
    R["widx"] = nc.alloc_sbuf_tensor("r_widx", [128, NBLK], I32)
    P = Prog(nc, same_engine_sync=same_engine_sync)
    ident = cst["ident"]
    gemm_fm(P, nc, N, x, ev_w_in, 3096, zT, ident, "g0")
    prm = dict(rw); prm.update(blk1=cst["blk1"], ident=ident, rmask=cst["rmask"])
    rwkv_prep(P, nc, NB, T, zT, prm, SR)
    D = dict(SR); D.update(masks=cst["masks"], id64=cst["id64"], ident=ident, gng=gng, gnb=gnb)
    chunk_scan(P, nc, NB, T, D, True, 64, 64, 8, yT, 0, "rw", gn_eps=64e-5)
    nsa_prep(P, nc, NB, T, zT, pos, cst, SN)
    nsa_cmp(P, nc, NB, T, zT, cw1, cw2, cpos, SN)
    nsa_attn_a(P, nc, NB, T, cst, SN)
    nsa_attn_b(P, nc, NB, T, cst, SN, yT, 512)
    phase_t1(P, nc, N, yT, x, ev_w_out, ln_g[0, 0], ln_b[0, 0], wrg[0], wre[0], brg[0], bre[0], ident, cst["tri"], h1, R)
    phase_t2(P, nc, N, h1, xbuf, thr, cst["iop"], R)
    phase_t3(P, nc, N, xbuf, ybuf, mw1[0], mw3[0], mw2[0], ident, R)
    phase_t4(P, nc, N, h1, ybuf, p[0], ln_g[0, 1], ln_b[0, 1], ple_gw[0], ple_w[0], ident, x1, R)
    gemm_fm(P, nc, N, x1, od_w_in, 4096, zT, ident, "g1")
    hgrn_prep(P, nc, NB, T, zT, dict(lb2=hg_lb, ident=ident, rmask=cst["rmask"]), SH)
    D = dict(SH); D.update(masks=cst["masks"], id64=cst["id64"], ident=ident, normg=hg_ng)
    chunk_scan(P, nc, NB, T, D, False, 128, 128, 8, yT1, 0, "hg", gn_eps=1e-5)
    phase_t1(P, nc, N, yT1, x1, od_w_out, ln_g[1, 0], ln_b[1, 0], wrg[1], wre[1], brg[1], bre[1], ident, cst["tri"], h1, R)
    phase_t2(P, nc, N, h1, xbuf, thr, cst["iop"], R)
    phase_t3(P, nc, N, xbuf, ybuf, mw1[1], mw3[1], mw2[1], ident, R)
    phase_t4(P, nc, N, h1, ybuf, p[1], ln_g[1, 1], ln_b[1, 1], ple_gw[1], ple_w[1], ident, out, R)
    P.finalize(); P.emit()
    return nc, Cc, len(P.ops)


def make_in_map(inp, b0, NB, T, Cc):
    N = NB * T
    NBLK = (2 * N) // BLK + NEXP
    f = lambda a: np.ascontiguousarray(np.asarray(a))
    m = {}
    m["x"] = f(inp["x"][b0:b0 + NB, :T]).reshape(N, 1024)
    m["p"] = f(inp["p"][:, b0:b0 + NB, :T]).reshape(2, N, 256)
    m["pos"] = f(inp["positions"][b0:b0 + NB, :T]).astype(np.int32)
    m["ev_w_in"] = f(inp["ev_w_in"][0]); m["ev_w_out"] = f(inp["ev_w_out"][0])
    m["od_w_in"] = f(inp["od_w_in"][0]); m["od_w_out"] = f(inp["od_w_out"][0])
    for k in ("mu", "w0", "w2", "a0", "a2", "g2", "k_k", "k_a", "gn_g", "gn_b"):
        m["rw_" + k] = f(inp["rw_" + k][0])
    m["rw_r_k"] = f(inp["rw_r_k"][0]).reshape(512)
    m["nsa_cmp_pos"] = f(inp["nsa_cmp_pos"][0]); m["nsa_cmp_w1"] = f(inp["nsa_cmp_w1"][0]); m["nsa_cmp_w2"] = f(inp["nsa_cmp_w2"][0])
    m["hg_lb"] = f(inp["hg_lb"]); m["hg_norm_g"] = f(inp["hg_norm_g"][0])
    for k in ("moe_w_rg", "moe_b_rg", "moe_w_re", "moe_b_re", "ln_g", "ln_b", "ple_w", "ple_gate_w"):
        m[k] = f(inp[k])
    for k in ("moe_w1", "moe_w3", "moe_w2"):
        for li in range(2):
            m[k + "_%d" % li] = f(inp[k][li]).reshape(4096, 4096)
    for k in Cc:
        m["c_" + k] = Cc[k]
    m["c_thr"] = (np.arange(NBLK) * BLK).astype(np.float32)
    return m


def kernel(**inputs):
    NB, T = 2, 4096
    nc, Cc, nops = build(NB, T)
    in_maps = [make_in_map(inputs, c * NB, NB, T, Cc) for c in range(8)]
    res = run_bass_kernel_spmd(nc, in_maps, core_ids=list(range(8)))
    out = np.concatenate([np.asarray(r["out"]).reshape(NB, T, 1024) for r in res.results], 0)
    return out.astype(np.float32)
```
